# Optimizing a Trainium2 kernel written in Bass

```python
import math
import jax, jax.numpy as jnp
from jax import lax
import numpy as np

D_MODEL = 1024
BATCH = 4
SEQ = 8192
DEPTH = 2

MEM_LEN = 256
MLA_HEADS = 6
MLA_NOPE = 64
MLA_ROPE = 32
MLA_V = 64
MLA_Q_RANK = 256
MLA_KV_RANK = 128
ROPE_THETA = 10000.0
DIFF_HEADS = 6
DIFF_HD = 32
MOBA_HEADS = 4
MOBA_HD = 64
MOBA_BLOCK = 256
MOBA_TOPK = 3
MOBA_Q_CHUNK = 32
Q_BLOCK = 128
CROSS_HEADS = 4
CROSS_HD = D_MODEL // CROSS_HEADS
D_FF = 4 * D_MODEL
EPS = 1e-6
NEG = -1e30
N_ALIBI = DIFF_HEADS + MOBA_HEADS

C_Q = MLA_Q_RANK
C_KV = MLA_KV_RANK
C_KR = MLA_ROPE
C_DIFF = DIFF_HEADS * 2 * DIFF_HD
C_MOBA = MOBA_HEADS * MOBA_HD
D_IN = C_Q + C_KV + C_KR + 3 * C_DIFF + 3 * C_MOBA
D_MIX = MLA_HEADS * MLA_V + DIFF_HEADS * 2 * DIFF_HD + MOBA_HEADS * MOBA_HD

kernel_name = "hymba_style_mla_diff_moba_trunk"


def rmsnorm(x, g):
    xf = x.astype(jnp.float32)
    y = xf * lax.rsqrt(jnp.mean(xf * xf, axis=-1, keepdims=True) + EPS)
    return (y * g.astype(jnp.float32)).astype(x.dtype)


def alibi_slopes():
    return jnp.asarray([2.0 ** (-8.0 * (h + 1) / N_ALIBI) for h in range(N_ALIBI)], jnp.float32)


def apply_rope(x, pos):
    half = x.shape[-1] // 2
    inv = ROPE_THETA ** (-jnp.arange(half, dtype=jnp.float32) / half)
    ang = pos.astype(jnp.float32)[:, None, :, None] * inv
    cos, sin = jnp.cos(ang), jnp.sin(ang)
    xf = x.astype(jnp.float32)
    x1, x2 = xf[..., :half], xf[..., half:]
    return jnp.concatenate([x1 * cos - x2 * sin, x1 * sin + x2 * cos], axis=-1).astype(x.dtype)


def to_heads(t, hd):
    B, S, _ = t.shape
    return t.reshape(B, S, -1, hd).transpose(0, 2, 1, 3)


def from_heads(t):
    B, H, S, d = t.shape
    return t.transpose(0, 2, 1, 3).reshape(B, S, H * d)


def sweep_query_blocks(body, B, H, S, dv):
    out = lax.map(body, jnp.arange(S // Q_BLOCK, dtype=jnp.int32) * Q_BLOCK)
    return jnp.moveaxis(out, 0, 2).reshape(B, H, S, dv)


def mla_causal_attention(q, k, v):
    B, H, S, dqk = q.shape
    scale = dqk ** -0.5
    kidx = jnp.arange(S)

    def body(start):
        qc = lax.dynamic_slice_in_dim(q, start, Q_BLOCK, axis=2)
        s = jnp.einsum('bhqd,bhkd->bhqk', qc, k, preferred_element_type=jnp.float32) * scale
        qidx = start + jnp.arange(Q_BLOCK)
        s = jnp.where(kidx[None, :] <= qidx[:, None], s, NEG)
        p = jax.nn.softmax(s, axis=-1)
        return jnp.einsum('bhqk,bhkd->bhqd', p.astype(v.dtype), v)

    return sweep_query_blocks(body, B, H, S, v.shape[-1])


def diff_causal_attention(q1, q2, k1, k2, v, pos, slopes, lam):
    B, H, S, d = q1.shape
    scale = d ** -0.5
    kidx = jnp.arange(S)

    def body(start):
        pq = lax.dynamic_slice_in_dim(pos, start, Q_BLOCK, axis=1)
        qidx = start + jnp.arange(Q_BLOCK)
        causal = kidx[None, :] <= qidx[:, None]
        dist = jnp.abs(pq[:, :, None] - pos[:, None, :]).astype(jnp.float32)
        bias = -slopes[None, :, None, None] * dist[:, None]

        def probs(qf, k):
            qc = lax.dynamic_slice_in_dim(qf, start, Q_BLOCK, axis=2)
            s = jnp.einsum('bhqd,bhkd->bhqk', qc, k, preferred_element_type=jnp.float32) * scale + bias
            return jax.nn.softmax(jnp.where(causal, s, NEG), axis=-1)

        p = probs(q1, k1) - lam * probs(q2, k2)
        return jnp.einsum('bhqk,bhkd->bhqd', p.astype(v.dtype), v)

    return sweep_query_blocks(body, B, H, S, v.shape[-1])


def moba_causal_attention(q, k, v, pos, slopes):
    B, H, S, dh = q.shape
    NB = -(-S // MOBA_BLOCK)
    pad = NB * MOBA_BLOCK - S
    kb = jnp.pad(k, ((0, 0), (0, 0), (0, pad), (0, 0))).reshape(B, H, NB, MOBA_BLOCK, dh)
    vb = jnp.pad(v, ((0, 0), (0, 0), (0, pad), (0, 0))).reshape(B, H, NB, MOBA_BLOCK, dh)
    pb = jnp.pad(pos, ((0, 0), (0, pad)), mode='edge').reshape(B, NB, MOBA_BLOCK)
    kmean = jnp.mean(kb.astype(jnp.float32), axis=3)
    K = min(MOBA_TOPK, NB)
    scale = dh ** -0.5
    bi = jnp.arange(B)[:, None, None, None]
    hi = jnp.arange(H)[None, :, None, None]
    m5 = slopes[None, :, None, None, None]

    def body(start):
        qc = lax.dynamic_slice_in_dim(q, start, MOBA_Q_CHUNK, axis=2)
        pq = lax.dynamic_slice_in_dim(pos, start, MOBA_Q_CHUNK, axis=1)
        qidx = start + jnp.arange(MOBA_Q_CHUNK)
        ob = start // MOBA_BLOCK
        gate = jnp.einsum('bhqd,bhnd->bhqn', qc.astype(jnp.float32), kmean)
        gate = jnp.where(jnp.arange(NB) < ob, gate, NEG)
        _, sel = lax.top_k(gate, K)
        valid = jnp.arange(K) < ob
        kg = kb[bi, hi, sel]
        vg = vb[bi, hi, sel]
        pg = pb[bi, sel]
        s_sel = jnp.einsum('bhqd,bhqkjd->bhqkj', qc, kg, preferred_element_type=jnp.float32) * scale
        s_sel = s_sel - m5 * jnp.abs(pq[:, None, :, None, None] - pg).astype(jnp.float32)
        s_sel = jnp.where(valid[None, None, None, :, None], s_sel, NEG)
        s_sel = s_sel.reshape(B, H, MOBA_Q_CHUNK, K * MOBA_BLOCK)
        ko = lax.dynamic_index_in_dim(kb, ob, axis=2, keepdims=False)
        vo = lax.dynamic_index_in_dim(vb, ob, axis=2, keepdims=False)
        po = lax.dynamic_index_in_dim(pb, ob, axis=1, keepdims=False)
        s_own = jnp.einsum('bhqd,bhjd->bhqj', qc, ko, preferred_element_type=jnp.float32) * scale
        s_own = s_own - slopes[None, :, None, None] * jnp.abs(pq[:, None, :, None] - po[:, None, None, :]).astype(jnp.float32)
        kidx = ob * MOBA_BLOCK + jnp.arange(MOBA_BLOCK)
        s_own = jnp.where(kidx[None, :] <= qidx[:, None], s_own, NEG)
        p = jax.nn.softmax(jnp.concatenate([s_sel, s_own], axis=-1), axis=-1).astype(v.dtype)
        p_sel = p[..., :K * MOBA_BLOCK].reshape(B, H, MOBA_Q_CHUNK, K, MOBA_BLOCK)
        return (jnp.einsum('bhqkj,bhqkjd->bhqd', p_sel, vg)
                + jnp.einsum('bhqj,bhjd->bhqd', p[..., K * MOBA_BLOCK:], vo))

    out = lax.map(body, jnp.arange(S // MOBA_Q_CHUNK, dtype=jnp.int32) * MOBA_Q_CHUNK)
    return jnp.moveaxis(out, 0, 2).reshape(B, H, S, dh)


def hybrid_mixer(n, pos, layer_idx, w_in, q_norm, w_uq, kv_norm, w_ukv,
                 lq1, lk1, lq2, lk2, sub_norm, w_out):
    B, S, _ = n.shape
    proj = n @ w_in
    cuts = [int(c) for c in np.cumsum([C_Q, C_KV, C_KR, C_DIFF, C_DIFF, C_DIFF, C_MOBA, C_MOBA])]
    c_q, c_kv, k_r, dq, dk, dv, mq, mk, mv = jnp.split(proj, cuts, axis=-1)
    slopes = alibi_slopes()

    qh = to_heads(rmsnorm(c_q, q_norm) @ w_uq, MLA_NOPE + MLA_ROPE)
    kvh = to_heads(rmsnorm(c_kv, kv_norm) @ w_ukv, MLA_NOPE + MLA_V)
    q_rope = apply_rope(qh[..., MLA_NOPE:], pos)
    k_rope = apply_rope(k_r[:, None], pos)
    q_a = jnp.concatenate([qh[..., :MLA_NOPE], q_rope], axis=-1)
    k_a = jnp.concatenate([kvh[..., :MLA_NOPE],
                           jnp.broadcast_to(k_rope, (B, MLA_HEADS, S, MLA_ROPE))], axis=-1)
    o_a = mla_causal_attention(q_a, k_a, kvh[..., MLA_NOPE:])

    dqh, dkh, dvh = to_heads(dq, 2 * DIFF_HD), to_heads(dk, 2 * DIFF_HD), to_heads(dv, 2 * DIFF_HD)
    lam_init = 0.8 - 0.6 * math.exp(-0.3 * layer_idx)
    lam = (jnp.exp(jnp.sum(lq1.astype(jnp.float32) * lk1.astype(jnp.float32)))
           - jnp.exp(jnp.sum(lq2.astype(jnp.float32) * lk2.astype(jnp.float32))) + lam_init)
    o_b = diff_causal_attention(dqh[..., :DIFF_HD], dqh[..., DIFF_HD:],
                                dkh[..., :DIFF_HD], dkh[..., DIFF_HD:],
                                dvh, pos, slopes[:DIFF_HEADS], lam)
    o_b = rmsnorm(o_b, sub_norm) * (1.0 - lam_init)

    o_c = moba_causal_attention(to_heads(mq, MOBA_HD), to_heads(mk, MOBA_HD),
                                to_heads(mv, MOBA_HD), pos, slopes[DIFF_HEADS:])

    o = jnp.concatenate([from_heads(o_a), from_heads(o_b), from_heads(o_c)], axis=-1)
    return o @ w_out


def memory_cross_attention(n, mem_n, wq, wkv, wo):
    q = to_heads(n @ wq, CROSS_HD)
    kv = mem_n @ wkv
    k = to_heads(kv[..., :D_MODEL], CROSS_HD)
    v = to_heads(kv[..., D_MODEL:], CROSS_HD)
    s = jnp.einsum('bhqd,bhmd->bhqm', q, k, preferred_element_type=jnp.float32) * (CROSS_HD ** -0.5)
    p = jax.nn.softmax(s, axis=-1).astype(v.dtype)
    return from_heads(jnp.einsum('bhqm,bhmd->bhqd', p, v)) @ wo


def squared_relu_mlp(n, w1, w2):
    return jnp.square(jax.nn.relu(n @ w1)) @ w2


def setup_inputs(seed: int = 0) -> dict:
    key = jax.random.key(seed)
    k = jax.random.split(key, 24)
    L = DEPTH
    f32 = jnp.float32
    nrm = lambda kk, shape, scale: jax.random.normal(kk, shape, f32) * scale
    gain = lambda kk, shape: 1.0 + 0.02 * jax.random.normal(kk, shape, f32)
    offset = jax.random.randint(k[2], (BATCH, 1), 0, 1024, dtype=jnp.int32)
    positions = offset + jnp.arange(SEQ, dtype=jnp.int32)[None, :]
    return {
        "x": nrm(k[0], (BATCH, SEQ, D_MODEL), 1.0),
        "mem": nrm(k[1], (BATCH, MEM_LEN, D_MODEL), 1.0),
        "positions": positions,
        "attn_norm": gain(k[3], (L, D_MODEL)),
        "w_in": nrm(k[4], (L, D_MODEL, D_IN), D_MODEL ** -0.5),
        "mla_q_norm": gain(k[5], (L, MLA_Q_RANK)),
        "mla_w_uq": nrm(k[6], (L, MLA_Q_RANK, MLA_HEADS * (MLA_NOPE + MLA_ROPE)), MLA_Q_RANK ** -0.5),
        "mla_kv_norm": gain(k[7], (L, MLA_KV_RANK)),
        "mla_w_ukv": nrm(k[8], (L, MLA_KV_RANK, MLA_HEADS * (MLA_NOPE + MLA_V)), MLA_KV_RANK ** -0.5),
        "diff_lambda_q1": nrm(k[9], (L, DIFF_HD), 0.1),
        "diff_lambda_k1": nrm(k[10], (L, DIFF_HD), 0.1),
        "diff_lambda_q2": nrm(k[11], (L, DIFF_HD), 0.1),
        "diff_lambda_k2": nrm(k[12], (L, DIFF_HD), 0.1),
        "diff_sub_norm": gain(k[13], (L, 2 * DIFF_HD)),
        "w_out": nrm(k[14], (L, D_MIX, D_MODEL), D_MIX ** -0.5),
        "cross_norm": gain(k[15], (L, D_MODEL)),
        "mem_norm": gain(k[16], (L, D_MODEL)),
        "cross_wq": nrm(k[17], (L, D_MODEL, D_MODEL), D_MODEL ** -0.5),
        "cross_wkv": nrm(k[18], (L, D_MODEL, 2 * D_MODEL), D_MODEL ** -0.5),
        "cross_wo": nrm(k[19], (L, D_MODEL, D_MODEL), D_MODEL ** -0.5),
        "mlp_norm": gain(k[20], (L, D_MODEL)),
        "mlp_w1": nrm(k[21], (L, D_MODEL, D_FF), D_MODEL ** -0.5),
        "mlp_w2": nrm(k[22], (L, D_FF, D_MODEL), D_FF ** -0.5),
        "final_norm": gain(k[23], (D_MODEL,)),
    }


def reference(x, mem, positions, attn_norm, w_in, mla_q_norm, mla_w_uq, mla_kv_norm, mla_w_ukv,
              diff_lambda_q1, diff_lambda_k1, diff_lambda_q2, diff_lambda_k2, diff_sub_norm, w_out,
              cross_norm, mem_norm, cross_wq, cross_wkv, cross_wo, mlp_norm, mlp_w1, mlp_w2, final_norm):
    h = x
    for l in range(DEPTH):
        h = h + hybrid_mixer(rmsnorm(h, attn_norm[l]), positions, l, w_in[l],
                             mla_q_norm[l], mla_w_uq[l], mla_kv_norm[l], mla_w_ukv[l],
                             diff_lambda_q1[l], diff_lambda_k1[l], diff_lambda_q2[l], diff_lambda_k2[l],
                             diff_sub_norm[l], w_out[l])
        h = h + memory_cross_attention(rmsnorm(h, cross_norm[l]), rmsnorm(mem, mem_norm[l]),
                                       cross_wq[l], cross_wkv[l], cross_wo[l])
        h = h + squared_relu_mlp(rmsnorm(h, mlp_norm[l]), mlp_w1[l], mlp_w2[l])
    return rmsnorm(h, final_norm)
```

```python
import math
from contextlib import ExitStack

import numpy as np
import ml_dtypes
import concourse.bass as bass
import concourse.mybir as mybir
from concourse.bass_utils import run_bass_kernel_spmd

F32 = mybir.dt.float32
BF16 = mybir.dt.bfloat16
I32 = mybir.dt.int32
AF = mybir.ActivationFunctionType
ALU = mybir.AluOpType
AX = mybir.AxisListType
NPBF = ml_dtypes.bfloat16

D = 1024
DIN = 2336
EPS = 1e-6
NEGM = -30000.0
N_ALIBI = 10
SLOPES = [2.0 ** (-8.0 * (h + 1) / N_ALIBI) for h in range(N_ALIBI)]
KA_A, KA_D, KA_M = 96, 40, 104

ENGS = ("pe", "act", "dve", "pool", "sp")
SAME_ENGINE_SYNC = True


class Res:
    __slots__ = ("name", "writers", "readers", "prev_readers", "psum")

    def __init__(self, name="", psum=False):
        self.name = name
        self.psum = psum
        self.writers = []
        self.readers = []
        self.prev_readers = []


def _push(lst, o):
    if not o.is_dma:
        for i, x in enumerate(lst):
            if (not x.is_dma) and x.eng == o.eng:
                lst[i] = o
                return
    lst.append(o)


class Op:
    __slots__ = ("eng", "fn", "deps", "is_dma", "sem", "val", "needed", "prev_val")

    def __init__(self, eng, fn, is_dma):
        self.eng = eng
        self.fn = fn
        self.deps = []
        self.is_dma = is_dma
        self.sem = None
        self.val = None
        self.needed = False
        self.prev_val = 0


class Prog:
    def __init__(self, nc, n_dma_sems=48):
        self.nc = nc
        self.ops = []
        self.n_dma_sems = n_dma_sems
        self.out_dmas = []
        self.rr = 0

    def _add(self, eng, fn, r, w, is_dma, pw=()):
        o = Op(eng, fn, is_dma)
        deps = []
        for x in r:
            deps.extend(x.writers)
            if x.psum:
                deps.extend(d for d in x.readers if d.eng != eng)
        for x in w:
            deps.extend(x.writers)
            deps.extend(x.readers)
            deps.extend(x.prev_readers)
        for x in pw:
            if x.readers:
                x.prev_readers = x.readers
                x.readers = []
                x.writers = []
            deps.extend(x.prev_readers)
        seen = set()
        for d in deps:
            if id(d) in seen:
                continue
            seen.add(id(d))
            if (not d.is_dma) and (not is_dma) and d.eng == eng:
                if eng == "pe" or not SAME_ENGINE_SYNC:
                    continue
            o.deps.append(d)
        for x in r:
            _push(x.readers, o)
        for x in w:
            x.writers = [o]
            x.readers = []
            x.prev_readers = []
        for x in pw:
            _push(x.writers, o)
        self.ops.append(o)
        return o

    def op(self, eng, fn, r=(), w=(), pw=()):
        return self._add(eng, fn, r, w, False, pw)

    def dma(self, queue, out, in_, r=(), w=(), is_output=False, pw=()):
        o = self._add(queue, lambda e: e.dma_start(out=out, in_=in_), r, w, True, pw)
        if is_output:
            self.out_dmas.append(o)
        return o

    def mm(self, out, lhsT, rhs, start, stop, r=(), w=(), pw=()):
        return self.op("pe", lambda e: e.matmul(out, lhsT=lhsT, rhs=rhs, start=start, stop=stop,
                                                skip_group_check=True), r, w, pw)

    def tr(self, out, in_, ident, r=(), w=(), pw=()):
        return self.op("pe", lambda e: e.transpose(out, in_, ident), r, w, pw)

    def act(self, out, in_, func, scale=1.0, bias=0.0, accum_out=None, r=(), w=(), pw=()):
        return self.op("act", lambda e: e.activation(out=out, in_=in_, func=func, bias=bias, scale=scale,
                                                     accum_out=accum_out), r, w, pw)

    def cp(self, eng, out, in_, r=(), w=(), pw=()):
        if eng == "act":
            return self.op("act", lambda e: e.copy(out=out, in_=in_), r, w, pw)
        return self.op(eng, lambda e: e.tensor_copy(out=out, in_=in_), r, w, pw)

    def tt(self, eng, out, in0, in1, op, r=(), w=(), pw=()):
        return self.op(eng, lambda e: e.tensor_tensor(out=out, in0=in0, in1=in1, op=op), r, w, pw)

    def ts(self, eng, out, in0, s1, op0, s2=None, op1=None, r=(), w=(), pw=()):
        if op1 is None:
            return self.op(eng, lambda e: e.tensor_scalar(out=out, in0=in0, scalar1=s1, scalar2=None, op0=op0), r, w, pw)
        return self.op(eng, lambda e: e.tensor_scalar(out=out, in0=in0, scalar1=s1, scalar2=s2, op0=op0, op1=op1), r, w, pw)

    def stt(self, out, in0, scalar, in1, op0, op1, r=(), w=(), pw=()):
        return self.op("dve", lambda e: e.scalar_tensor_tensor(out=out, in0=in0, scalar=scalar, in1=in1,
                                                               op0=op0, op1=op1), r, w, pw)

    def memset(self, eng, ap, val, w=(), pw=()):
        return self.op(eng, lambda e: e.memset(ap, val), (), w, pw)

    def emit(self):
        nc = self.nc
        ops = self.ops
        for o in ops:
            for d in o.deps:
                d.needed = True
        with ExitStack() as es:
            eng_sem = {e: es.enter_context(nc.semaphore("s_" + e)) for e in ENGS}
            n_hw, n_sw = self.n_dma_sems, 16
            dma_sems = [es.enter_context(nc.semaphore("d%d" % i)) for i in range(n_hw + n_sw)]
            dma_tot = [0] * (n_hw + n_sw)
            cnt = {e: 0 for e in ENGS}
            k_hw = k_sw = 0
            for o in ops:
                if o.is_dma:
                    if o.eng == "pool":
                        si = n_hw + (k_sw % n_sw)
                        k_sw += 1
                    else:
                        si = k_hw % n_hw
                        k_hw += 1
                    o.sem = dma_sems[si]
                    o.prev_val = dma_tot[si]
                    dma_tot[si] += 16
                    o.val = dma_tot[si]
                elif o.needed:
                    cnt[o.eng] += 1
                    o.sem = eng_sem[o.eng]
                    o.val = cnt[o.eng]
            per = {e: [o for o in ops if o.eng == e] for e in ENGS}
            final_waits = [(s, v) for s, v in zip(dma_sems, dma_tot) if v > 0]

            def replay(ename, eobj, extra_final=False):
                known = {}
                for o in per[ename]:
                    waits = [(d.sem, d.val) for d in o.deps]
                    if o.is_dma and o.prev_val > 0:
                        waits.append((o.sem, o.prev_val))
                    for (s, v) in waits:
                        key = id(s)
                        if known.get(key, 0) >= v:
                            continue
                        eobj.wait_ge(s, v)
                        known[key] = v
                    ins = o.fn(eobj)
                    if o.is_dma:
                        ins.then_inc(o.sem, 16)
                    elif o.needed:
                        ins.then_inc(o.sem, 1)
                if extra_final:
                    for (s, v) in final_waits:
                        if known.get(id(s), 0) >= v:
                            continue
                        eobj.wait_ge(s, v)
                        known[id(s)] = v

            with nc.Block() as block:
                @block.sync
                def _(e):
                    replay("sp", e, extra_final=True)

                @block.scalar
                def _(e):
                    replay("act", e)

                @block.vector
                def _(e):
                    replay("dve", e)

                @block.gpsimd
                def _(e):
                    replay("pool", e)

                @block.tensor
                def _(e):
                    replay("pe", e)


class Ctx:
    def __init__(self, nc):
        self.nc = nc
        self.P = Prog(nc)
        self.n = 0

    def dram(self, name, shape, dt, out=False):
        return self.nc.dram_tensor(name, list(shape), dt, kind="ExternalOutput" if out else "ExternalInput").ap()

    def sb(self, shape, dt, name=None):
        self.n += 1
        return self.nc.alloc_sbuf_tensor(name or ("t%d" % self.n), list(shape), dt)

    def ps(self, shape, dt, name=None):
        self.n += 1
        return self.nc.alloc_psum_tensor(name or ("p%d" % self.n), list(shape), dt)


def split_bf16_2(x):
    a = np.float32(np.asarray(x, np.float32).astype(NPBF).astype(np.float32))
    b = np.float32(np.asarray(np.float32(x) - a, np.float32).astype(NPBF).astype(np.float32))
    return float(a), float(b)


def load_w_bf16(C, dst, src, rows, cols, r_w, queue="pool"):
    kc = rows // 128
    step = 1024
    for k in range(kc):
        for c0 in range(0, cols, step):
            c1 = min(cols, c0 + step)
            if kc == 1 and len(dst.shape) == 2:
                o = dst[:, c0:c1]
            else:
                o = dst[:, k, c0:c1]
            C.P.dma(queue, o, src[k * 128:(k + 1) * 128, c0:c1], pw=[r_w])


def rmsnorm_tile(C, x_ap, g_ap, out_ap, width, scr, rx, rg, ro, tag):
    P = C.P
    junk, ss, rstd = scr["junk"], scr["ss"], scr["rstd"]
    rs = scr["res"]
    P.act(junk[:, 0:width], x_ap, AF.Square, accum_out=ss[:, 0:1], r=[rx], w=[rs])
    P.ts("dve", rstd[:, 0:1], ss[:, 0:1], 1.0 / width, ALU.mult, EPS, ALU.add, r=[rs], w=[rs])
    P.op("act", lambda e: e.sqrt(out=rstd[:, 0:1], in_=rstd[:, 0:1]), r=[rs], w=[rs])
    P.op("dve", lambda e: e.reciprocal(out=rstd[:, 0:1], in_=rstd[:, 0:1]), r=[rs], w=[rs])
    P.stt(out_ap, x_ap, rstd[:, 0:1], g_ap, ALU.mult, ALU.mult, r=[rx, rg, rs], w=[ro])


DBG_STOP = 99


def build_A(S):
    TO = S // 2
    TT = TO // 128
    NSLOT = TT // 4
    nc = bass.Bass("TRN2", target_bir_lowering=False)
    C = Ctx(nc)
    P = C.P
    h_in = C.dram("h", [TO, D], F32)
    pos_d = C.dram("pos", [128, TT], I32)
    pos0_d = C.dram("pos0", [1, 1], I32)
    onehot_d = C.dram("onehot", [128, TT, 32], BF16)
    invf_d = C.dram("invf", [1, 16], F32)
    ident_d = C.dram("ident", [128, 128], F32)
    g_attn_d = C.dram("attn_norm", [1, D], F32)
    w_in_d = C.dram("w_in", [D, DIN], F32)
    g_q_d = C.dram("q_norm", [1, 256], F32)
    w_uq_d = C.dram("w_uq", [256, 576], F32)
    g_kv_d = C.dram("kv_norm", [1, 128], F32)
    w_ukv_d = C.dram("w_ukv", [128, 768], F32)
    kA_d = C.dram("kA", [6, 128, TT, KA_A], BF16, out=True)
    qA_d = C.dram("qA", [6, 128, TT, KA_A], BF16, out=True)
    vA_d = C.dram("vA", [6, 128, TT, 65], BF16, out=True)
    kD_d = C.dram("kD", [12, 128, TT, KA_D], BF16, out=True)
    qD_d = C.dram("qD", [12, 128, TT, KA_D], BF16, out=True)
    vD_d = C.dram("vD", [6, 128, TT, 65], BF16, out=True)
    kM_d = C.dram("kM", [4, 128, TT, KA_M], BF16, out=True)
    qM_d = C.dram("qM", [4, 128, TT, KA_M], BF16, out=True)
    vM_d = C.dram("vM", [4, 128, TT, 65], BF16, out=True)

    w_in = C.sb([128, 8, DIN], BF16)
    w_uq = C.sb([128, 2, 576], BF16)
    w_ukv = C.sb([128, 768], BF16)
    ident = C.sb([128, 128], BF16)
    g_attn = C.sb([128, D], F32)
    g_q = C.sb([128, 256], F32)
    g_kv = C.sb([128, 128], F32)
    r_w = Res("weights")
    P.dma("pool", ident[:], ident_d, pw=[r_w])
    P.dma("sp", g_attn[:], g_attn_d[0, :].partition_broadcast(128), pw=[r_w])
    P.dma("sp", g_q[:], g_q_d[0, :].partition_broadcast(128), pw=[r_w])
    P.dma("sp", g_kv[:], g_kv_d[0, :].partition_broadcast(128), pw=[r_w])
    load_w_bf16(C, w_in, w_in_d, D, DIN, r_w)
    load_w_bf16(C, w_uq, w_uq_d, 256, 576, r_w)
    load_w_bf16(C, w_ukv, w_ukv_d, 128, 768, r_w)

    r_pos = Res("pos")
    pos_i = C.sb([128, TT], I32)
    pos0_i = C.sb([128, 1], I32)
    pos_f = C.sb([128, TT], F32)
    pos0_f = C.sb([128, 1], F32)
    prel_f = C.sb([128, TT], F32)
    prel_i = C.sb([128, TT], I32)
    hi_i = C.sb([128, TT], I32)
    lo_i = C.sb([128, TT], I32)
    hl = C.sb([128, 2, TT], F32)
    invf = C.sb([128, 16], F32)
    ang = C.sb([128, TT, 16], F32)
    kq = C.sb([128, TT, 16], F32)
    kq_i = C.sb([128, TT, 16], I32)
    cos_t = C.sb([128, TT, 16], F32)
    sin_t = C.sb([128, TT, 16], F32)
    P.dma("sp", pos_i[:], pos_d, w=[r_pos])
    P.dma("sp", pos0_i[:], pos0_d[0, :].partition_broadcast(128), w=[r_pos])
    P.dma("sp", invf[:], invf_d[0, :].partition_broadcast(128), w=[r_pos])
    P.cp("dve", pos_f[:], pos_i[:], r=[r_pos], w=[r_pos])
    P.cp("dve", pos0_f[:], pos0_i[:], r=[r_pos], w=[r_pos])
    P.ts("dve", prel_f[:], pos_f[:], pos0_f[:, 0:1], ALU.subtract, r=[r_pos], w=[r_pos])
    P.cp("dve", prel_i[:], prel_f[:], r=[r_pos], w=[r_pos])
    P.op("dve", lambda e: e.tensor_single_scalar(out=hi_i[:], in_=prel_i[:], scalar=7, op=ALU.arith_shift_right),
         r=[r_pos], w=[r_pos])
    P.op("dve", lambda e: e.tensor_single_scalar(out=lo_i[:], in_=prel_i[:], scalar=127, op=ALU.bitwise_and),
         r=[r_pos], w=[r_pos])
    P.cp("dve", hl[:, 0, :], hi_i[:], r=[r_pos], w=[r_pos])
    P.cp("dve", hl[:, 1, :], lo_i[:], r=[r_pos], w=[r_pos])
    TWO_PI = 2.0 * math.pi
    c1 = float(np.float32(TWO_PI))
    c2 = float(TWO_PI - np.float64(np.float32(TWO_PI)))
    for t in range(TT):
        P.ts("dve", ang[:, t, :], invf[:], pos_f[:, t:t + 1], ALU.mult, r=[r_pos], w=[r_pos])
    P.ts("dve", kq[:], ang[:], 1.0 / TWO_PI, ALU.mult, r=[r_pos], w=[r_pos])
    P.cp("dve", kq_i[:], kq[:], r=[r_pos], w=[r_pos])
    P.cp("dve", kq[:], kq_i[:], r=[r_pos], w=[r_pos])
    P.stt(ang[:], kq[:], -c1, ang[:], ALU.mult, ALU.add, r=[r_pos], w=[r_pos])
    P.stt(ang[:], kq[:], -c2, ang[:], ALU.mult, ALU.add, r=[r_pos], w=[r_pos])
    P.ts("dve", ang[:], ang[:], math.pi, ALU.min, -math.pi, ALU.max, r=[r_pos], w=[r_pos])
    P.act(sin_t[:], ang[:], AF.Sin, r=[r_pos], w=[r_pos])
    P.stt(kq[:], ang[:], -1.0, ang[:], ALU.mult, ALU.max, r=[r_pos], w=[r_pos])
    P.ts("dve", kq[:], kq[:], -1.0, ALU.mult, math.pi / 2, ALU.add, r=[r_pos], w=[r_pos])
    P.act(cos_t[:], kq[:], AF.Sin, r=[r_pos], w=[r_pos])
    augK = C.sb([128, N_ALIBI, TT, 8], BF16)
    augQ = C.sb([128, N_ALIBI, TT, 8], BF16)
    r_aug = Res("aug")
    for hh in range(N_ALIBI):
        d_h = 32 if hh < 6 else 64
        cc = SLOPES[hh] / (d_h ** -0.5)
        a1, a2 = split_bf16_2(cc)
        for j, v in enumerate([-128 * a1, -128 * a2, -a1, -a2]):
            P.memset("pool", augK[:, hh, :, 4 + j], v, pw=[r_aug])
        for j, v in enumerate([128 * a1, 128 * a2, a1, a2]):
            P.memset("pool", augQ[:, hh, :, j], v, pw=[r_aug])
        for j in range(4):
            P.cp("dve", augK[:, hh, :, j], hl[:, j // 2, :], r=[r_pos], pw=[r_aug])
            P.cp("dve", augQ[:, hh, :, 4 + j], hl[:, j // 2, :], r=[r_pos], pw=[r_aug])
    onehot = C.sb([128, TT, 32], BF16)
    P.dma("sp", onehot[:], onehot_d, pw=[r_aug])

    if DBG_STOP == 0:
        P.emit()
        return nc
    NB = 2
    hbuf = [C.sb([128, D], F32) for _ in range(3)]
    r_h = [Res("h%d" % i) for i in range(3)]
    nbf = [C.sb([128, D], BF16) for _ in range(2)]
    r_n = [Res("n%d" % i) for i in range(2)]
    nT = [C.sb([128, 8, 512], BF16) for _ in range(NB)]
    r_nT = [Res("nT%d" % i) for i in range(NB)]
    scr = {"junk": C.sb([128, D], BF16), "ss": C.sb([128, 1], F32), "rstd": C.sb([128, 1], F32), "res": Res("scr")}
    scr2 = {"junk": C.sb([128, 256], BF16), "ss": C.sb([128, 1], F32), "rstd": C.sb([128, 1], F32), "res": Res("scr2")}
    cqn = [C.sb([128, 384], BF16) for _ in range(2)]
    r_cqn = [Res("cqn%d" % i) for i in range(2)]
    cT = [C.sb([128, 3, 512], BF16) for _ in range(NB)]
    r_cT = [Res("cT%d" % i) for i in range(NB)]
    ropet = [C.sb([128, 6, 16], F32) for _ in range(4)]
    r_rope = [Res("ropetmp%d" % i) for i in range(6)]
    krt = C.sb([128, 32], BF16)
    r_kr = Res("krt")
    stKA = [C.sb([128, 6, 4, KA_A], BF16) for _ in range(NB)]
    stQA = [C.sb([128, 6, 4, KA_A], BF16) for _ in range(NB)]
    stVA = [C.sb([128, 6, 4, 65], BF16) for _ in range(NB)]
    stKD = [C.sb([128, 12, 4, KA_D], BF16) for _ in range(NB)]
    stQD = [C.sb([128, 12, 4, KA_D], BF16) for _ in range(NB)]
    stVD = [C.sb([128, 6, 4, 65], BF16) for _ in range(NB)]
    stKM = [C.sb([128, 4, 4, KA_M], BF16) for _ in range(NB)]
    stQM = [C.sb([128, 4, 4, KA_M], BF16) for _ in range(NB)]
    stVM = [C.sb([128, 4, 4, 65], BF16) for _ in range(NB)]
    r_st = [Res("st%d" % i) for i in range(NB)]
    for b in range(NB):
        for v in (stVA[b], stVD[b], stVM[b]):
            P.memset("pool", v[:, :, :, 64], 1.0, pw=[r_st[b]])
        P.memset("pool", stQM[b][:, :, :, 64:96], 0.0, pw=[r_st[b]])
    ptr = [C.ps([128, D], BF16) for _ in range(2)]
    r_ptr = [Res("ptr%d" % i, psum=True) for i in range(2)]
    pp = [C.ps([128, 512], F32) for _ in range(4)]
    r_pp = [Res("pp%d" % i, psum=True) for i in range(4)]
    pq = [C.ps([128, 512], F32) for _ in range(2)]
    r_pq = [Res("pq%d" % i, psum=True) for i in range(2)]
    cnt = {"h": 0, "n": 0, "ptr": 0, "pp": 0, "pq": 0, "ev": 0}

    def evac(out, in_, r, w=(), pw=()):
        cnt["ev"] += 1
        P.cp("act" if cnt["ev"] % 2 else "dve", out, in_, r=r, w=w, pw=pw)

    chunks = [(0, 416), (416, 800), (800, 1184), (1184, 1568), (1568, 2080), (2080, 2336)]
    for s in range(NSLOT):
        b = s % NB
        rst = r_st[b]
        tsl = slice(4 * s, 4 * s + 4)
        for i in range(2):
            P.cp("pool", stKD[b][:, i::2, :, 32:40], augK[:, 0:6, tsl, :], r=[r_aug], pw=[rst])
            P.cp("pool", stQD[b][:, i::2, :, 32:40], augQ[:, 0:6, tsl, :], r=[r_aug], pw=[rst])
        P.cp("pool", stKM[b][:, :, :, 96:104], augK[:, 6:10, tsl, :], r=[r_aug], pw=[rst])
        P.cp("pool", stQM[b][:, :, :, 96:104], augQ[:, 6:10, tsl, :], r=[r_aug], pw=[rst])
        for hh in range(4):
            P.cp("pool", stKM[b][:, hh, :, 64:96], onehot[:, tsl, :], r=[r_aug], pw=[rst])
        for tt in range(4):
            t = 4 * s + tt
            hi_ = cnt["h"] % 3
            cnt["h"] += 1
            P.dma("sp", hbuf[hi_][:], h_in[t * 128:(t + 1) * 128, :], w=[r_h[hi_]])
            ni = cnt["n"] % 2
            cnt["n"] += 1
            rmsnorm_tile(C, hbuf[hi_][:], g_attn[:], nbf[ni][:], D, scr, r_h[hi_], r_w, r_n[ni], "a")
            pi = cnt["ptr"] % 2
            cnt["ptr"] += 1
            for kc in range(8):
                P.tr(ptr[pi][:, kc * 128:(kc + 1) * 128], nbf[ni][:, kc * 128:(kc + 1) * 128], ident[:],
                     r=[r_n[ni], r_w], pw=[r_ptr[pi]])
            evac(nT[b][:, :, tt * 128:(tt + 1) * 128], ptr[pi][:].rearrange("p (k c) -> p k c", k=8),
                 r=[r_ptr[pi]], pw=[r_nT[b]])
        if DBG_STOP == 1:
            P.emit()
            return nc
        for tt in range(4):
            t = 4 * s + tt
            ci = cnt["n"] % 2
            cnt["n"] += 1
            for ch, (c0, c1) in enumerate(chunks):
                pi = cnt["pp"] % 4
                cnt["pp"] += 1
                w_ = c1 - c0
                for kc in range(8):
                    P.mm(pp[pi][:, 0:w_], nT[b][:, kc, tt * 128:(tt + 1) * 128], w_in[:, kc, c0:c1],
                         kc == 0, kc == 7, r=[r_nT[b], r_w], w=[r_pp[pi]])
                src = pp[pi]
                rp = r_pp[pi]
                if ch == 0:
                    rmsnorm_tile(C, src[:, 0:256], g_q[:], cqn[ci][:, 0:256], 256, scr2, rp, r_w, r_cqn[ci], "q")
                    rmsnorm_tile(C, src[:, 256:384], g_kv[:], cqn[ci][:, 256:384], 128, scr2, rp, r_w, r_cqn[ci], "kv")
                    x1 = src[:, 384:400]
                    x2 = src[:, 400:416]
                    co = cos_t[:, t, :]
                    si = sin_t[:, t, :]
                    ta, tb = ropet[0][:, 0, :], ropet[1][:, 0, :]
                    P.tt("dve", ta, x1, co, ALU.mult, r=[rp, r_pos], w=[r_rope[0]])
                    P.tt("dve", tb, x2, si, ALU.mult, r=[rp, r_pos], w=[r_rope[1]])
                    P.tt("dve", krt[:, 0:16], ta, tb, ALU.subtract, r=[r_rope[0], r_rope[1]], w=[r_kr])
                    P.tt("dve", ta, x1, si, ALU.mult, r=[rp, r_pos], w=[r_rope[0]])
                    P.tt("dve", tb, x2, co, ALU.mult, r=[rp, r_pos], w=[r_rope[1]])
                    P.tt("dve", krt[:, 16:32], ta, tb, ALU.add, r=[r_rope[0], r_rope[1]], pw=[r_kr])
                    for hh in range(6):
                        P.cp("pool", stKA[b][:, hh, tt, 64:96], krt[:], r=[r_kr], pw=[rst])
                    ti = cnt["ptr"] % 2
                    cnt["ptr"] += 1
                    for kc in range(3):
                        P.tr(ptr[ti][:, kc * 128:(kc + 1) * 128], cqn[ci][:, kc * 128:(kc + 1) * 128], ident[:],
                             r=[r_cqn[ci], r_w], w=[r_ptr[ti]])
                    evac(cT[b][:, :, tt * 128:(tt + 1) * 128],
                         ptr[ti][:, 0:384].rearrange("p (k c) -> p k c", k=3), r=[r_ptr[ti]], pw=[r_cT[b]])
                elif ch == 1:
                    evac(stQD[b][:, :, tt, 0:32], src[:, 0:384].rearrange("p (m c) -> p m c", m=12), r=[rp], pw=[rst])
                elif ch == 2:
                    evac(stKD[b][:, :, tt, 0:32], src[:, 0:384].rearrange("p (m c) -> p m c", m=12), r=[rp], pw=[rst])
                elif ch == 3:
                    evac(stVD[b][:, :, tt, 0:64], src[:, 0:384].rearrange("p (m c) -> p m c", m=6), r=[rp], pw=[rst])
                elif ch == 4:
                    evac(stQM[b][:, :, tt, 0:64], src[:, 0:256].rearrange("p (m c) -> p m c", m=4), r=[rp], pw=[rst])
                    evac(stKM[b][:, :, tt, 0:64], src[:, 256:512].rearrange("p (m c) -> p m c", m=4), r=[rp], pw=[rst])
                else:
                    evac(stVM[b][:, :, tt, 0:64], src[:, 0:256].rearrange("p (m c) -> p m c", m=4), r=[rp], pw=[rst])
        if DBG_STOP == 2:
            P.emit()
            return nc
        for tt in range(4):
            t = 4 * s + tt
            co = cos_t[:, t, :]
            si = sin_t[:, t, :]
            for half in range(2):
                qi = cnt["pq"] % 2
                cnt["pq"] += 1
                for kc in range(2):
                    P.mm(pq[qi][:, 0:288], cT[b][:, kc, tt * 128:(tt + 1) * 128], w_uq[:, kc, half * 288:(half + 1) * 288],
                         kc == 0, kc == 1, r=[r_cT[b], r_w], w=[r_pq[qi]])
                v3 = pq[qi][:, 0:288].rearrange("p (m c) -> p m c", m=3)
                hs = slice(3 * half, 3 * half + 3)
                evac(stQA[b][:, hs, tt, 0:64], v3[:, :, 0:64], r=[r_pq[qi]], pw=[rst])
                for m in range(3):
                    hh = 3 * half + m
                    x1 = pq[qi][:, m * 96 + 64:m * 96 + 80]
                    x2 = pq[qi][:, m * 96 + 80:m * 96 + 96]
                    ta, tb, tc, td = (ropet[i][:, hh, :] for i in range(4))
                    rr_ = r_rope[hh]
                    P.tt("dve", ta, x1, co, ALU.mult, r=[r_pq[qi], r_pos], w=[rr_])
                    P.tt("dve", tb, x2, si, ALU.mult, r=[r_pq[qi], r_pos], pw=[rr_])
                    P.tt("dve", tc, x1, si, ALU.mult, r=[r_pq[qi], r_pos], pw=[rr_])
                    P.tt("dve", td, x2, co, ALU.mult, r=[r_pq[qi], r_pos], pw=[rr_])
                    P.tt("pool", stQA[b][:, hh, tt, 64:80], ta, tb, ALU.subtract, r=[rr_], pw=[rst])
                    P.tt("pool", stQA[b][:, hh, tt, 80:96], tc, td, ALU.add, r=[rr_], pw=[rst])
                qi = cnt["pq"] % 2
                cnt["pq"] += 1
                P.mm(pq[qi][:, 0:384], cT[b][:, 2, tt * 128:(tt + 1) * 128], w_ukv[:, half * 384:(half + 1) * 384],
                     True, True, r=[r_cT[b], r_w], w=[r_pq[qi]])
                v3 = pq[qi][:, 0:384].rearrange("p (m c) -> p m c", m=3)
                evac(stKA[b][:, hs, tt, 0:64], v3[:, :, 0:64], r=[r_pq[qi]], pw=[rst])
                evac(stVA[b][:, hs, tt, 0:64], v3[:, :, 64:128], r=[r_pq[qi]], pw=[rst])
        if DBG_STOP == 3:
            P.emit()
            return nc
        for (st, dd, nm) in ((stKA, kA_d, 6), (stQA, qA_d, 6), (stVA, vA_d, 6), (stKD, kD_d, 12), (stQD, qD_d, 12),
                             (stVD, vD_d, 6), (stKM, kM_d, 4), (stQM, qM_d, 4), (stVM, vM_d, 4)):
            for m in range(nm):
                P.dma("sp", dd[m, :, tsl, :], st[b][:, m, :, :], r=[rst], is_output=True)
    P.emit()
    return nc


def own_qtiles(S, parity):
    nqt = S // 512
    out = []
    for p in range(nqt // 2):
        out.append(2 * p + parity if p % 2 == 0 else 2 * p + 1 - parity)
    return out


def own_tiles(S, parity):
    return [4 * q + i for q in own_qtiles(S, parity) for i in range(4)]


def const_inputs_A(S, parity):
    tiles = own_tiles(S, parity)
    TT = len(tiles)
    onehot = np.zeros((128, TT, 32), np.float32)
    for i, g in enumerate(tiles):
        onehot[:, i, g // 2] = 1.0
    invf = (np.float32(10000.0) ** (-np.arange(16, dtype=np.float32) / np.float32(16))).astype(np.float32)
    return {"onehot": onehot.astype(NPBF), "invf": invf.reshape(1, 16),
            "ident": np.eye(128, dtype=np.float32)}


def tok_rows(S, parity):
    return np.concatenate([np.arange(g * 128, (g + 1) * 128) for g in own_tiles(S, parity)])


def const_inputs_B(S, parity):
    tiles = own_tiles(S, parity)
    TT = len(tiles)
    kk = np.arange(128)[:, None]
    qq = np.arange(512)[None, :]
    diag = [np.where(d * 128 + kk <= qq, 0.0, NEGM).astype(np.float32) for d in range(4)]
    full = np.full((128, 512), NEGM, np.float32)
    zero = np.zeros((128, 512), np.float32)
    role_min = np.stack(diag + [full] * 4)
    role_max = np.stack([zero] * 4 + diag)
    m_even = role_min if parity == 0 else role_max
    m_odd = role_max if parity == 0 else role_min
    pastmask = np.zeros((128, TT, 32), np.float32)
    isown = np.zeros((128, TT, 32), np.float32)
    for i, g in enumerate(tiles):
        ob = g // 2
        pastmask[:, i, ob:] = -1e30
        if ob < 32:
            isown[:, i, ob] = 1.0
    return {"m_even": m_even.astype(NPBF), "m_odd": m_odd.astype(NPBF), "pastmask": pastmask, "isown": isown,
            "ident": np.eye(128, dtype=np.float32)}


def build_B(S, lam_init):
    TO = S // 2
    TT = TO // 128
    NSLOT = TT // 4
    NKT = S // 128
    NBLK = S // 256
    nc = bass.Bass("TRN2", target_bir_lowering=False)
    C = Ctx(nc)
    P = C.P
    kA_d = C.dram("kA", [6, 128, NKT, KA_A], BF16)
    qA_d = C.dram("qA", [6, 128, TT, KA_A], BF16)
    vA_d = C.dram("vA", [6, 128, NKT, 65], BF16)
    kD_d = C.dram("kD", [12, 128, NKT, KA_D], BF16)
    qD_d = C.dram("qD", [12, 128, TT, KA_D], BF16)
    vD_d = C.dram("vD", [6, 128, NKT, 65], BF16)
    kM_d = C.dram("kM", [4, 128, NKT, KA_M], BF16)
    qM_d = C.dram("qM", [4, 128, TT, KA_M], BF16)
    vM_d = C.dram("vM", [4, 128, NKT, 65], BF16)
    m_even_d = C.dram("m_even", [8, 128, 512], BF16)
    m_odd_d = C.dram("m_odd", [8, 128, 512], BF16)
    pastmask_d = C.dram("pastmask", [128, TT, 32], F32)
    isown_d = C.dram("isown", [128, TT, 32], F32)
    ident_d = C.dram("ident", [128, 128], F32)
    lam_d = [C.dram(n, [1, 32], F32) for n in ("lq1", "lk1", "lq2", "lk2")]
    gsub_d = C.dram("sub_norm", [1, 64], F32)
    o_d = C.dram("o", [128, TT, D], BF16, out=True)

    r_c = Res("consts")
    ident = C.sb([128, 128], BF16)
    masks = [C.sb([128, 8, 512], BF16) for _ in range(2)]
    pastmask = C.sb([128, TT, 32], F32)
    isown = C.sb([128, TT, 32], F32)
    lamt = [C.sb([128, 32], F32) for _ in range(4)]
    gsub = C.sb([128, 64], F32)
    P.dma("pool", ident[:], ident_d, pw=[r_c])
    for i, md in enumerate((m_even_d, m_odd_d)):
        for j in range(8):
            P.dma("sp", masks[i][:, j, :], md[j], pw=[r_c])
    P.dma("sp", pastmask[:], pastmask_d, pw=[r_c])
    P.dma("sp", isown[:], isown_d, pw=[r_c])
    for i in range(4):
        P.dma("sp", lamt[i][:], lam_d[i][0, :].partition_broadcast(128), pw=[r_c])
    P.dma("sp", gsub[:], gsub_d[0, :].partition_broadcast(128), pw=[r_c])
    r_lam = Res("lam")
    lprod = C.sb([128, 32], F32)
    lsum = C.sb([128, 2], F32)
    neg_lam = C.sb([128, 1], F32)
    for i in range(2):
        P.tt("dve", lprod[:], lamt[2 * i][:], lamt[2 * i + 1][:], ALU.mult, r=[r_c], w=[r_lam])
        P.op("dve", lambda e, i=i: e.reduce_sum(out=lsum[:, i:i + 1], in_=lprod[:], axis=AX.X), r=[r_lam], w=[r_lam])
    P.act(lsum[:], lsum[:], AF.Exp, r=[r_lam], w=[r_lam])
    P.stt(neg_lam[:], lsum[:, 1:2], -float(lam_init), lsum[:, 0:1], ALU.add, ALU.subtract, r=[r_lam], w=[r_lam])
    P.ts("dve", gsub[:], gsub[:], 1.0 - float(lam_init), ALU.mult, r=[r_c], w=[r_c])

    KT = [C.sb([128, 2, S], BF16) for _ in range(2)]
    QT = [C.sb([128, 2, TO], BF16) for _ in range(2)]
    V = [C.sb([128, NKT, 65], BF16) for _ in range(2)]
    r_KT = [Res("KT%d" % i) for i in range(2)]
    r_QT = [Res("QT%d" % i) for i in range(2)]
    r_V = [Res("V%d" % i) for i in range(2)]
    CH = 16 if NKT >= 16 else NKT
    stg = [C.sb([128, CH, KA_M], BF16) for _ in range(3)]
    r_stg = [Res("stg%d" % i) for i in range(3)]
    PT = [C.sb([128, 512], BF16) for _ in range(4)]
    r_PT = [Res("PT%d" % i) for i in range(4)]
    osl = [C.sb([128, 4, 64], BF16) for _ in range(2)]
    r_osl = [Res("osl%d" % i) for i in range(2)]
    pS = [C.ps([128, 512], F32) for _ in range(2)]
    r_pS = [Res("pS%d" % i, psum=True) for i in range(2)]
    pO = [C.ps([128, 512], F32) for _ in range(2)]
    r_pO = [Res("pO%d" % i, psum=True) for i in range(2)]
    pOT = [C.ps([128, 512], F32) for _ in range(2)]
    r_pOT = [Res("pOT%d" % i, psum=True) for i in range(2)]
    oTs = [C.sb([65, 512], F32) for _ in range(2)]
    r_oTs = [Res("oTs%d" % i) for i in range(2)]
    pT = [C.ps([128, 1024], BF16) for _ in range(1)]
    r_pT = [Res("pT%d" % i, psum=True) for i in range(1)]
    pG = C.ps([128, 512], F32)
    r_pG = Res("pG", psum=True)
    kmT = C.sb([64, 32], F32)
    kmT_bf = C.sb([64, 32], BF16)
    gm = C.sb([128, 32], F32)
    m8 = C.sb([128, 8], F32)
    msel = C.sb([128, 4, 32], BF16)
    r_g = Res("gate")
    r_ms = Res("msel")
    rz = C.sb([128, 2, 4], F32)
    t1 = C.sb([128, 4, 64], F32)
    t2 = C.sb([128, 4, 64], F32)
    sq = C.sb([128, 4, 64], F32)
    ssd = C.sb([128, 4], F32)
    r_nrm = Res("nrm")
    cnt = {"stg": 0, "pT": 0, "pS": 0, "pO": 0, "PT": 0, "osl": 0, "pOT": 0}
    identf = C.sb([128, 128], F32)
    P.dma("sp", identf[:], ident_d, pw=[r_c])

    units = []
    for h in range(6):
        units.append(dict(kind="A", maps=[(kA_d[h], qA_d[h], KA_A)], v=vA_d[h], scale=96 ** -0.5, col=h * 64))
    for h in range(6):
        units.append(dict(kind="D", maps=[(kD_d[2 * h + i], qD_d[2 * h + i], KA_D) for i in range(2)], v=vD_d[h],
                          scale=32 ** -0.5, col=384 + h * 64))
    for h in range(4):
        units.append(dict(kind="M", maps=[(kM_d[h], qM_d[h], KA_M)], v=vM_d[h], scale=64 ** -0.5, col=768 + h * 64))

    def transpose_in(src_d, ntiles, dst, mi, KA, r_dst):
        for c0 in range(0, ntiles, CH):
            n = min(CH, ntiles - c0)
            si = cnt["stg"] % 3
            cnt["stg"] += 1
            P.dma("sp", stg[si][:, 0:n, 0:KA], src_d[:, c0:c0 + n, :], w=[r_stg[si]])
            for g0 in range(0, n, 4):
                gn = min(4, n - g0)
                pi = cnt["pT"] % len(pT)
                cnt["pT"] += 1
                for t in range(gn):
                    P.tr(pT[pi][0:KA, t * 128:(t + 1) * 128], stg[si][:, g0 + t, 0:KA], ident[:],
                         r=[r_stg[si], r_c], pw=[r_pT[pi]])
                c = (c0 + g0) * 128
                P.cp("dve", dst[0:KA, mi, c:c + gn * 128], pT[pi][0:KA, 0:gn * 128], r=[r_pT[pi]], pw=[r_dst])

    def prep(u):
        U = units[u]
        ub = u % 2
        for c0 in range(0, NKT, 32):
            n = min(32, NKT - c0)
            P.dma("sp", V[ub][:, c0:c0 + n, :], U["v"][:, c0:c0 + n, :], pw=[r_V[ub]])
        for mi, (kd, qd, KA) in enumerate(U["maps"]):
            transpose_in(kd, NKT, KT[ub], mi, KA, r_KT[ub])
            transpose_in(qd, TT, QT[ub], mi, KA, r_QT[ub])
        if U["kind"] == "M":
            P.memset("dve", kmT[:], 0.0, w=[r_g])
            P.op("dve", lambda e: e.tensor_reduce(out=kmT[:, 0:NBLK],
                                                  in_=KT[ub][0:64, 0, :].rearrange("p (n k) -> p n k", k=256),
                                                  axis=AX.X, op=ALU.add), r=[r_KT[ub]], w=[r_g])
            P.ts("dve", kmT_bf[:], kmT[:], 1.0 / 256.0, ALU.mult, r=[r_g], w=[r_g])
            for g0 in range(0, TT, 4):
                for t4 in range(4):
                    t = g0 + t4
                    P.mm(pG[:, 0:32], QT[ub][0:64, 0, t * 128:(t + 1) * 128], kmT_bf[:, :], True, True,
                         r=[r_QT[ub], r_g], w=[r_pG])
                    P.tt("dve", gm[:], pG[:, 0:32], pastmask[:, t, :], ALU.add, r=[r_pG, r_c], w=[r_g])
                    P.op("dve", lambda e: e.max(out=m8[:], in_=gm[:]), r=[r_g], w=[r_g])
                    P.ts("dve", gm[:], gm[:], m8[:, 2:3], ALU.is_ge, r=[r_g], w=[r_g])
                    P.tt("dve", gm[:], gm[:], isown[:, t, :], ALU.max, r=[r_g, r_c], w=[r_g])
                    P.ts("dve", msel[:, t4, :], gm[:], -1.0, ALU.add, -NEGM, ALU.mult, r=[r_g],
                         **({"w": [r_ms]} if t4 == 0 else {"pw": [r_ms]}))
                pi = cnt["pT"] % len(pT)
                cnt["pT"] += 1
                for t4 in range(4):
                    P.tr(pT[pi][0:32, t4 * 128:(t4 + 1) * 128], msel[:, t4, :], ident[:], r=[r_ms, r_c], pw=[r_pT[pi]])
                P.cp("act", QT[ub][64:96, 0, g0 * 128:(g0 + 4) * 128], pT[pi][0:32, 0:512], r=[r_pT[pi]], pw=[r_QT[ub]])

    def attention(u):
        U = units[u]
        ub = u % 2
        scale = float(U["scale"])
        nmap = len(U["maps"])
        for p in range(NSLOT):
            nkt = 8 * (p + 1)
            mk = masks[p % 2]
            obanks = []
            for mi in range(nmap):
                KA = U["maps"][mi][2]
                oi = cnt["pO"] % 2
                cnt["pO"] += 1
                obanks.append(oi)
                ti = cnt["pOT"] % 2
                cnt["pOT"] += 1
                q_ap = QT[ub][0:KA, mi, p * 512:(p + 1) * 512]
                sbank = {}

                def qk(j):
                    si = cnt["pS"] % 2
                    cnt["pS"] += 1
                    sbank[j] = si
                    band = j >= 8 * p
                    P.mm(pS[si][:, :], KT[ub][0:KA, mi, j * 128:(j + 1) * 128], q_ap, True, not band,
                         r=[r_KT[ub], r_QT[ub]], w=[r_pS[si]])
                    if band:
                        P.mm(pS[si][:, :], ident[:, :], mk[:, j - 8 * p, :], False, True, r=[r_c], pw=[r_pS[si]])

                qk(0)
                if nkt > 1:
                    qk(1)
                for j in range(nkt):
                    si = sbank[j]
                    pi = cnt["PT"] % 4
                    cnt["PT"] += 1
                    P.act(PT[pi][:], pS[si][:, :], AF.Exp, scale=scale, r=[r_pS[si]], w=[r_PT[pi]])
                    P.mm(pOT[ti][0:65, :], V[ub][:, j, :], PT[pi][:, :], j == 0, j == nkt - 1,
                         r=[r_PT[pi], r_V[ub]], **({"w": [r_pOT[ti]]} if j == 0 else {"pw": [r_pOT[ti]]}))
                    if j + 2 < nkt:
                        qk(j + 2)
                P.cp("dve", oTs[ti][:, :], pOT[ti][0:65, :], r=[r_pOT[ti]], w=[r_oTs[ti]])
                for u4 in range(4):
                    P.tr(pO[oi][:, u4 * 65:(u4 + 1) * 65], oTs[ti][0:65, u4 * 128:(u4 + 1) * 128], identf[0:65, 0:65],
                         r=[r_oTs[ti], r_c], **({"w": [r_pO[oi]]} if u4 == 0 else {"pw": [r_pO[oi]]}))
            oi0 = obanks[0]
            O0 = pO[oi0][:, 0:260].rearrange("p (u c) -> p u c", c=65)
            so = cnt["osl"] % 2
            cnt["osl"] += 1
            P.op("dve", lambda e, O0=O0: e.reciprocal(out=rz[:, 0, :], in_=O0[:, :, 64]), r=[r_pO[oi0]], w=[r_nrm])
            if U["kind"] != "D":
                for u4 in range(4):
                    P.ts("dve", osl[so][:, u4, :], O0[:, u4, 0:64], rz[:, 0, u4:u4 + 1], ALU.mult,
                         r=[r_pO[oi0], r_nrm], **({"w": [r_osl[so]]} if u4 == 0 else {"pw": [r_osl[so]]}))
            else:
                oi1 = obanks[1]
                O1 = pO[oi1][:, 0:260].rearrange("p (u c) -> p u c", c=65)
                P.op("dve", lambda e, O1=O1: e.reciprocal(out=rz[:, 1, :], in_=O1[:, :, 64]), r=[r_pO[oi1]], pw=[r_nrm])
                for u4 in range(4):
                    P.ts("dve", t1[:, u4, :], O0[:, u4, 0:64], rz[:, 0, u4:u4 + 1], ALU.mult,
                         r=[r_pO[oi0], r_nrm], pw=[r_nrm])
                    P.ts("dve", t2[:, u4, :], O1[:, u4, 0:64], rz[:, 1, u4:u4 + 1], ALU.mult, neg_lam[:, 0:1], ALU.mult,
                         r=[r_pO[oi1], r_nrm, r_lam], pw=[r_nrm])
                P.tt("dve", t1[:], t1[:], t2[:], ALU.add, r=[r_nrm], w=[r_nrm])
                P.tt("dve", sq[:], t1[:], t1[:], ALU.mult, r=[r_nrm], w=[r_nrm])
                P.op("dve", lambda e: e.reduce_sum(out=ssd[:], in_=sq[:], axis=AX.X), r=[r_nrm], w=[r_nrm])
                P.ts("dve", ssd[:], ssd[:], 1.0 / 64.0, ALU.mult, EPS, ALU.add, r=[r_nrm], w=[r_nrm])
                P.op("act", lambda e: e.sqrt(out=ssd[:], in_=ssd[:]), r=[r_nrm], w=[r_nrm])
                P.op("dve", lambda e: e.reciprocal(out=ssd[:], in_=ssd[:]), r=[r_nrm], w=[r_nrm])
                for u4 in range(4):
                    P.stt(osl[so][:, u4, :], t1[:, u4, :], ssd[:, u4:u4 + 1], gsub[:], ALU.mult, ALU.mult,
                          r=[r_nrm, r_c], **({"w": [r_osl[so]]} if u4 == 0 else {"pw": [r_osl[so]]}))
            P.dma("sp", o_d[:, 4 * p:4 * p + 4, U["col"]:U["col"] + 64], osl[so][:], r=[r_osl[so]], is_output=True)

    prep(0)
    for u in range(len(units)):
        if u + 1 < len(units):
            prep(u + 1)
        attention(u)
    P.emit()
    return nc


def assemble_kv(outA_pair, S):
    res = {}
    pos_of = {}
    for par in range(2):
        for i, g in enumerate(own_tiles(S, par)):
            pos_of[g] = (par, i)
    NKT = S // 128
    for name in ("kA", "vA", "kD", "vD", "kM", "vM"):
        a0, a1 = outA_pair[0][name], outA_pair[1][name]
        full = np.empty((a0.shape[0], 128, NKT, a0.shape[3]), dtype=a0.dtype)
        for g in range(NKT):
            par, i = pos_of[g]
            full[:, :, g, :] = (a0 if par == 0 else a1)[:, :, i, :]
        res[name] = full
    return res


_NC_CACHE = {}


def _get_nc(key, fn):
    if key not in _NC_CACHE:
        _NC_CACHE[key] = fn()
    return _NC_CACHE[key]


def _run(nc, in_maps):
    res = run_bass_kernel_spmd(nc, in_maps, core_ids=list(range(8)))
    return res.results


def launch_A(S, h_own, positions, W, l):
    nc = _get_nc(("A", S), lambda: build_A(S))
    in_maps = []
    for c in range(8):
        b, par = c // 2, c % 2
        rows = tok_rows(S, par)
        TT = len(rows) // 128
        m = dict(const_inputs_A(S, par))
        m["h"] = np.ascontiguousarray(h_own[c])
        m["pos"] = np.ascontiguousarray(positions[b][rows].reshape(TT, 128).T)
        m["pos0"] = np.ascontiguousarray(positions[b, 0:1].reshape(1, 1))
        m["attn_norm"] = W["attn_norm"][l].reshape(1, -1)
        m["w_in"] = W["w_in"][l]
        m["q_norm"] = W["mla_q_norm"][l].reshape(1, -1)
        m["w_uq"] = W["mla_w_uq"][l]
        m["kv_norm"] = W["mla_kv_norm"][l].reshape(1, -1)
        m["w_ukv"] = W["mla_w_ukv"][l]
        in_maps.append(m)
    return _run(nc, in_maps)


def launch_B(S, outA, W, l):
    lam_init = 0.8 - 0.6 * math.exp(-0.3 * l)
    nc = _get_nc(("B", S, l), lambda: build_B(S, lam_init))
    in_maps = []
    for b in range(4):
        kv = assemble_kv([outA[2 * b], outA[2 * b + 1]], S)
        for par in range(2):
            m = dict(const_inputs_B(S, par))
            m.update(kv)
            for nm in ("qA", "qD", "qM"):
                m[nm] = outA[2 * b + par][nm]
            m["lq1"] = W["diff_lambda_q1"][l].reshape(1, -1)
            m["lk1"] = W["diff_lambda_k1"][l].reshape(1, -1)
            m["lq2"] = W["diff_lambda_q2"][l].reshape(1, -1)
            m["lk2"] = W["diff_lambda_k2"][l].reshape(1, -1)
            m["sub_norm"] = W["diff_sub_norm"][l].reshape(1, -1)
            in_maps.append(m)
    return _run(nc, in_maps)


def transpose_tile(C, src_bf, dstT, col0, ident, pT, r_pT, r_src, r_dst, r_id, cnt, nchunk=8):
    P = C.P
    pi = cnt["pT"] % len(pT)
    cnt["pT"] += 1
    for kc in range(nchunk):
        P.tr(pT[pi][:, kc * 128:(kc + 1) * 128], src_bf[:, kc * 128:(kc + 1) * 128], ident[:],
             r=[r_src, r_id], pw=[r_pT[pi]])
    cnt["ev"] += 1
    P.cp("act" if cnt["ev"] % 2 else "dve", dstT[:, 0:nchunk, col0:col0 + 128],
         pT[pi][:, 0:nchunk * 128].rearrange("p (k c) -> p k c", k=nchunk), r=[r_pT[pi]], pw=[r_dst])


def build_C1(S):
    TO = S // 2
    TT = TO // 128
    NSLOT = TT // 4
    nc = bass.Bass("TRN2", target_bir_lowering=False)
    C = Ctx(nc)
    P = C.P
    h_d = C.dram("h", [TO, D], F32)
    o_d = C.dram("o", [128, TT, D], BF16)
    mem_d = C.dram("mem", [256, D], F32)
    ident_d = C.dram("ident", [128, 128], F32)
    w_out_d = C.dram("w_out", [D, D], F32)
    g_cross_d = C.dram("cross_norm", [1, D], F32)
    g_mem_d = C.dram("mem_norm", [1, D], F32)
    wq_d = C.dram("wq", [D, D], F32)
    wkv_d = C.dram("wkv", [D, 2 * D], F32)
    wo_d = C.dram("wo", [D, D], F32)
    hout_d = C.dram("h_out", [TO, D], F32, out=True)

    r_w = Res("w")
    ident = C.sb([128, 128], BF16)
    w_out = C.sb([128, 8, D], BF16)
    wq = C.sb([128, 8, D], BF16)
    wo = C.sb([128, 8, D], BF16)
    wkv = C.sb([128, 8, 2 * D], BF16)
    g_cross = C.sb([128, D], F32)
    g_mem = C.sb([128, D], F32)
    P.dma("pool", ident[:], ident_d, pw=[r_w])
    P.dma("sp", g_cross[:], g_cross_d[0, :].partition_broadcast(128), pw=[r_w])
    P.dma("sp", g_mem[:], g_mem_d[0, :].partition_broadcast(128), pw=[r_w])
    load_w_bf16(C, wkv, wkv_d, D, 2 * D, r_w)
    load_w_bf16(C, w_out, w_out_d, D, D, r_w)
    load_w_bf16(C, wq, wq_d, D, D, r_w)
    load_w_bf16(C, wo, wo_d, D, D, r_w)

    hb = [C.sb([128, D], F32) for _ in range(4)]
    r_hb = [Res("hb%d" % i) for i in range(4)]
    nbf = [C.sb([128, D], BF16) for _ in range(2)]
    r_nbf = [Res("nbf%d" % i) for i in range(2)]
    xT = [C.sb([128, 8, 512], BF16) for _ in range(2)]
    r_xT = [Res("xT%d" % i) for i in range(2)]
    osb = C.sb([128, 4, D], BF16)
    r_osb = Res("osb")
    cqT = C.sb([128, 8, 512], BF16)
    r_cqT = Res("cqT")
    oc = C.sb([128, 4, D], BF16)
    r_oc = Res("oc")
    PTc = [C.sb([128, 512], BF16) for _ in range(2)]
    r_PTc = [Res("PTc%d" % i) for i in range(2)]
    kmemT = C.sb([128, 8, 256], BF16)
    vmem = C.sb([128, 2, 4, 257], BF16)
    r_kv = Res("memkv")
    rzc = C.sb([128, 1], F32)
    r_rz = Res("rzc")
    scr = {"junk": C.sb([128, D], BF16), "ss": C.sb([128, 1], F32), "rstd": C.sb([128, 1], F32), "res": Res("scr")}
    pT = [C.ps([128, D], BF16) for _ in range(2)]
    r_pT = [Res("pT%d" % i, psum=True) for i in range(2)]
    pP = [C.ps([128, 512], F32) for _ in range(2)]
    r_pP = [Res("pP%d" % i, psum=True) for i in range(2)]
    pS = [C.ps([128, 512], F32) for _ in range(2)]
    r_pS = [Res("pS%d" % i, psum=True) for i in range(2)]
    pO = [C.ps([128, 512], F32) for _ in range(2)]
    r_pO = [Res("pO%d" % i, psum=True) for i in range(2)]
    cnt = {"pT": 0, "ev": 0, "pP": 0, "pS": 0, "pO": 0, "n": 0, "x": 0}

    def evac(out, in_, r, w=(), pw=()):
        cnt["ev"] += 1
        P.cp("act" if cnt["ev"] % 2 else "dve", out, in_, r=r, w=w, pw=pw)

    memT = xT[1]
    for mt in range(2):
        hi = mt
        P.dma("sp", hb[hi][:], mem_d[mt * 128:(mt + 1) * 128, :], w=[r_hb[hi]])
        ni = cnt["n"] % 2
        cnt["n"] += 1
        rmsnorm_tile(C, hb[hi][:], g_mem[:], nbf[ni][:], D, scr, r_hb[hi], r_w, r_nbf[ni], "m")
        transpose_tile(C, nbf[ni], memT, mt * 128, ident, pT, r_pT, r_nbf[ni], r_xT[1], r_w, cnt)
    for fc in range(8):
        pi = cnt["pP"] % 2
        cnt["pP"] += 1
        for kc in range(8):
            P.mm(pP[pi][:, 0:256], wkv[:, kc, fc * 128:(fc + 1) * 128], memT[:, kc, 0:256], kc == 0, kc == 7,
                 r=[r_w, r_xT[1]], w=[r_pP[pi]])
        evac(kmemT[:, fc, :], pP[pi][:, 0:256], r=[r_pP[pi]], pw=[r_kv])
    P.memset("pool", vmem[:, :, :, 256], 1.0, pw=[r_kv])
    for mt in range(2):
        for half in range(2):
            pi = cnt["pP"] % 2
            cnt["pP"] += 1
            for kc in range(8):
                P.mm(pP[pi][:, :], memT[:, kc, mt * 128:(mt + 1) * 128], wkv[:, kc, D + half * 512:D + (half + 1) * 512],
                     kc == 0, kc == 7, r=[r_w, r_xT[1]], w=[r_pP[pi]])
            evac(vmem[:, mt, 2 * half:2 * half + 2, 0:256], pP[pi][:, :].rearrange("p (h c) -> p h c", h=2),
                 r=[r_pP[pi]], pw=[r_kv])

    def proj_add(srcT, r_srcT, w_sb, tt):
        for half in range(2):
            pi = cnt["pP"] % 2
            cnt["pP"] += 1
            for kc in range(8):
                P.mm(pP[pi][:, :], srcT[:, kc, tt * 128:(tt + 1) * 128], w_sb[:, kc, half * 512:(half + 1) * 512],
                     kc == 0, kc == 7, r=[r_srcT, r_w], w=[r_pP[pi]])
            P.tt("dve", hb[tt][:, half * 512:(half + 1) * 512], hb[tt][:, half * 512:(half + 1) * 512], pP[pi][:, :],
                 ALU.add, r=[r_pP[pi]], w=[r_hb[tt]])

    for p in range(NSLOT):
        P.dma("sp", osb[:], o_d[:, 4 * p:4 * p + 4, :], w=[r_osb])
        for tt in range(4):
            t = 4 * p + tt
            P.dma("sp", hb[tt][:], h_d[t * 128:(t + 1) * 128, :], w=[r_hb[tt]])
        xa = cnt["x"] % 2
        cnt["x"] += 1
        for tt in range(4):
            transpose_tile(C, osb[:, tt, :], xT[xa], tt * 128, ident, pT, r_pT, r_osb, r_xT[xa], r_w, cnt)
        for tt in range(4):
            proj_add(xT[xa], r_xT[xa], w_out, tt)
        xb = cnt["x"] % 2
        cnt["x"] += 1
        for tt in range(4):
            ni = cnt["n"] % 2
            cnt["n"] += 1
            rmsnorm_tile(C, hb[tt][:], g_cross[:], nbf[ni][:], D, scr, r_hb[tt], r_w, r_nbf[ni], "c")
            transpose_tile(C, nbf[ni], xT[xb], tt * 128, ident, pT, r_pT, r_nbf[ni], r_xT[xb], r_w, cnt)
        for fc in range(8):
            pi = cnt["pP"] % 2
            cnt["pP"] += 1
            for kc in range(8):
                P.mm(pP[pi][:, :], wq[:, kc, fc * 128:(fc + 1) * 128], xT[xb][:, kc, :], kc == 0, kc == 7,
                     r=[r_w, r_xT[xb]], w=[r_pP[pi]])
            evac(cqT[:, fc, :], pP[pi][:, :], r=[r_pP[pi]], pw=[r_cqT])
        for hh in range(4):
            for mt in range(2):
                si = cnt["pS"] % 2
                cnt["pS"] += 1
                for dc in range(2):
                    P.mm(pS[si][:, :], kmemT[:, hh * 2 + dc, mt * 128:(mt + 1) * 128], cqT[:, hh * 2 + dc, :],
                         dc == 0, dc == 1, r=[r_kv, r_cqT], w=[r_pS[si]])
                P.act(PTc[mt][:], pS[si][:, :], AF.Exp, scale=1.0 / 16.0, r=[r_pS[si]], w=[r_PTc[mt]])
            for tt in range(4):
                oi = cnt["pO"] % 2
                cnt["pO"] += 1
                for mt in range(2):
                    P.mm(pO[oi][:, 0:257], PTc[mt][:, tt * 128:(tt + 1) * 128], vmem[:, mt, hh, :], mt == 0, mt == 1,
                         r=[r_PTc[mt], r_kv], w=[r_pO[oi]])
                P.op("dve", lambda e, oi=oi: e.reciprocal(out=rzc[:], in_=pO[oi][:, 256:257]), r=[r_pO[oi]], w=[r_rz])
                P.ts("dve", oc[:, tt, hh * 256:(hh + 1) * 256], pO[oi][:, 0:256], rzc[:, 0:1], ALU.mult,
                     r=[r_pO[oi], r_rz], pw=[r_oc])
        xc = cnt["x"] % 2
        cnt["x"] += 1
        for tt in range(4):
            transpose_tile(C, oc[:, tt, :], xT[xc], tt * 128, ident, pT, r_pT, r_oc, r_xT[xc], r_w, cnt)
        for tt in range(4):
            t = 4 * p + tt
            proj_add(xT[xc], r_xT[xc], wo, tt)
            P.dma("sp", hout_d[t * 128:(t + 1) * 128, :], hb[tt][:], r=[r_hb[tt]], is_output=True)
    P.emit()
    return nc


def build_C2(S, final):
    TO = S // 2
    TT = TO // 128
    nc = bass.Bass("TRN2", target_bir_lowering=False)
    C = Ctx(nc)
    P = C.P
    h_d = C.dram("h", [TO, D], F32)
    ident_d = C.dram("ident", [128, 128], F32)
    g_mlp_d = C.dram("mlp_norm", [1, D], F32)
    w1_d = C.dram("w1", [D, 4 * D], F32)
    w2_d = C.dram("w2", [4 * D, D], F32)
    g_fin_d = C.dram("final_norm", [1, D], F32)
    hout_d = C.dram("h_out", [TO, D], F32, out=True)
    r_w = Res("w")
    ident = C.sb([128, 128], BF16)
    w1 = C.sb([128, 8, 4 * D], BF16)
    w2 = C.sb([128, 32, D], BF16)
    g_mlp = C.sb([128, D], F32)
    g_fin = C.sb([128, D], F32)
    P.dma("pool", ident[:], ident_d, pw=[r_w])
    P.dma("sp", g_mlp[:], g_mlp_d[0, :].partition_broadcast(128), pw=[r_w])
    P.dma("sp", g_fin[:], g_fin_d[0, :].partition_broadcast(128), pw=[r_w])
    load_w_bf16(C, w1, w1_d, D, 4 * D, r_w)
    load_w_bf16(C, w2, w2_d, 4 * D, D, r_w)
    NT = 2
    W = NT * 128
    hb = [C.sb([128, D], F32) for _ in range(2 * NT)]
    r_hb = [Res("hb%d" % i) for i in range(2 * NT)]
    nbf = [C.sb([128, D], BF16) for _ in range(2)]
    r_nbf = [Res("nbf%d" % i) for i in range(2)]
    xT = [C.sb([128, 8, W], BF16) for _ in range(2)]
    r_xT = [Res("xT%d" % i) for i in range(2)]
    hidT = C.sb([128, 32, W], BF16)
    r_hid = Res("hidT")
    rl = [C.sb([128, W], F32) for _ in range(2)]
    r_rl = [Res("rl%d" % i) for i in range(2)]
    fo = [C.sb([128, D], F32) for _ in range(2)]
    r_fo = [Res("fo%d" % i) for i in range(2)]
    scr = {"junk": C.sb([128, D], BF16), "ss": C.sb([128, 1], F32), "rstd": C.sb([128, 1], F32), "res": Res("scr")}
    pT = [C.ps([128, D], BF16) for _ in range(2)]
    r_pT = [Res("pT%d" % i, psum=True) for i in range(2)]
    pH = [C.ps([128, 512], F32) for _ in range(3)]
    r_pH = [Res("pH%d" % i, psum=True) for i in range(3)]
    pP = [C.ps([128, 512], F32) for _ in range(3)]
    r_pP = [Res("pP%d" % i, psum=True) for i in range(3)]
    cnt = {"pT": 0, "ev": 0, "pP": 0, "pH": 0, "n": 0, "x": 0, "rl": 0, "hb": 0, "fo": 0}
    for g in range(TT // NT):
        hs = []
        xa = cnt["x"] % 2
        cnt["x"] += 1
        for tt in range(NT):
            t = g * NT + tt
            hi = cnt["hb"] % (2 * NT)
            cnt["hb"] += 1
            hs.append(hi)
            P.dma("sp", hb[hi][:], h_d[t * 128:(t + 1) * 128, :], w=[r_hb[hi]])
            ni = cnt["n"] % 2
            cnt["n"] += 1
            rmsnorm_tile(C, hb[hi][:], g_mlp[:], nbf[ni][:], D, scr, r_hb[hi], r_w, r_nbf[ni], "m")
            transpose_tile(C, nbf[ni], xT[xa], tt * 128, ident, pT, r_pT, r_nbf[ni], r_xT[xa], r_w, cnt)
        for fc in range(32):
            pi = cnt["pH"] % 3
            cnt["pH"] += 1
            for kc in range(8):
                P.mm(pH[pi][:, 0:W], w1[:, kc, fc * 128:(fc + 1) * 128], xT[xa][:, kc, :], kc == 0, kc == 7,
                     r=[r_w, r_xT[xa]], w=[r_pH[pi]])
            ri = cnt["rl"] % 2
            cnt["rl"] += 1
            P.act(rl[ri][:], pH[pi][:, 0:W], AF.Relu, r=[r_pH[pi]], w=[r_rl[ri]])
            P.tt("pool", hidT[:, fc, :], rl[ri][:], rl[ri][:], ALU.mult, r=[r_rl[ri]], pw=[r_hid])
        for tt in range(NT):
            t = g * NT + tt
            hi = hs[tt]
            for half in range(2):
                pi = cnt["pP"] % 3
                cnt["pP"] += 1
                for fc in range(32):
                    P.mm(pP[pi][:, :], hidT[:, fc, tt * 128:(tt + 1) * 128], w2[:, fc, half * 512:(half + 1) * 512],
                         fc == 0, fc == 31, r=[r_hid, r_w], w=[r_pP[pi]])
                P.tt("dve", hb[hi][:, half * 512:(half + 1) * 512], hb[hi][:, half * 512:(half + 1) * 512], pP[pi][:, :],
                     ALU.add, r=[r_pP[pi]], w=[r_hb[hi]])
            if final:
                fi = cnt["fo"] % 2
                cnt["fo"] += 1
                rmsnorm_tile(C, hb[hi][:], g_fin[:], fo[fi][:], D, scr, r_hb[hi], r_w, r_fo[fi], "f")
                P.dma("sp", hout_d[t * 128:(t + 1) * 128, :], fo[fi][:], r=[r_fo[fi]], is_output=True)
            else:
                P.dma("sp", hout_d[t * 128:(t + 1) * 128, :], hb[hi][:], r=[r_hb[hi]], is_output=True)
    P.emit()
    return nc


def launch_C1(S, h_own, outB, mem, W, l):
    nc = _get_nc(("C1", S), lambda: build_C1(S))
    in_maps = []
    for c in range(8):
        b = c // 2
        m = {"h": np.ascontiguousarray(h_own[c]), "o": outB[c]["o"], "mem": np.ascontiguousarray(mem[b]),
             "ident": np.eye(128, dtype=np.float32), "w_out": W["w_out"][l],
             "cross_norm": W["cross_norm"][l].reshape(1, -1), "mem_norm": W["mem_norm"][l].reshape(1, -1),
             "wq": W["cross_wq"][l], "wkv": W["cross_wkv"][l], "wo": W["cross_wo"][l]}
        in_maps.append(m)
    return [r["h_out"] for r in _run(nc, in_maps)]


def launch_C2(S, h_own, W, l, final):
    nc = _get_nc(("C2", S, final), lambda: build_C2(S, final))
    in_maps = []
    for c in range(8):
        m = {"h": np.ascontiguousarray(h_own[c]), "ident": np.eye(128, dtype=np.float32),
             "mlp_norm": W["mlp_norm"][l].reshape(1, -1), "w1": W["mlp_w1"][l], "w2": W["mlp_w2"][l],
             "final_norm": W["final_norm"].reshape(1, -1)}
        in_maps.append(m)
    return [r["h_out"] for r in _run(nc, in_maps)]


def forward(S, inputs, depth=2):
    x = np.asarray(inputs["x"])
    positions = np.asarray(inputs["positions"])
    mem = np.asarray(inputs["mem"])
    W = {k: np.asarray(v) for k, v in inputs.items()}
    h_own = [np.ascontiguousarray(x[c // 2][tok_rows(S, c % 2)]) for c in range(8)]
    for l in range(depth):
        outA = launch_A(S, h_own, positions, W, l)
        outB = launch_B(S, outA, W, l)
        h_own = launch_C1(S, h_own, outB, mem, W, l)
        h_own = launch_C2(S, h_own, W, l, final=(l == depth - 1))
    out = np.empty((4, S, D), np.float32)
    for c in range(8):
        out[c // 2][tok_rows(S, c % 2)] = h_own[c]
    return out


def kernel(**inputs):
    return forward(8192, inputs)
```

```python
import math
from contextlib import ExitStack

import numpy as np
import ml_dtypes
import concourse.bass as bass
import concourse.mybir as mybir
from concourse.bass_utils import run_bass_kernel_spmd

F32 = mybir.dt.float32
BF16 = mybir.dt.bfloat16
I32 = mybir.dt.int32
AF = mybir.ActivationFunctionType
ALU = mybir.AluOpType
AX = mybir.AxisListType
NPBF = ml_dtypes.bfloat16

D = 1024
DIN = 2336
EPS = 1e-6
NEGM = -30000.0
N_ALIBI = 10
SLOPES = [2.0 ** (-8.0 * (h + 1) / N_ALIBI) for h in range(N_ALIBI)]
KA_A, KA_D, KA_M = 96, 40, 104

ENGS = ("pe", "act", "dve", "pool", "sp")
SAME_ENGINE_SYNC = True


class Res:
    __slots__ = ("name", "writers", "readers", "prev_readers", "psum")

    def __init__(self, name="", psum=False):
        self.name = name
        self.psum = psum
        self.writers = []
        self.readers = []
        self.prev_readers = []


def _push(lst, o):
    if not o.is_dma:
        for i, x in enumerate(lst):
            if (not x.is_dma) and x.eng == o.eng:
                lst[i] = o
                return
    lst.append(o)


class Op:
    __slots__ = ("eng", "fn", "deps", "is_dma", "sem", "val", "needed", "prev_val")

    def __init__(self, eng, fn, is_dma):
        self.eng = eng
        self.fn = fn
        self.deps = []
        self.is_dma = is_dma
        self.sem = None
        self.val = None
        self.needed = False
        self.prev_val = 0


class Prog:
    def __init__(self, nc, n_dma_sems=48):
        self.nc = nc
        self.ops = []
        self.n_dma_sems = n_dma_sems
        self.out_dmas = []
        self.rr = 0

    def _add(self, eng, fn, r, w, is_dma, pw=()):
        o = Op(eng, fn, is_dma)
        deps = []
        for x in r:
            deps.extend(x.writers)
            if x.psum:
                deps.extend(d for d in x.readers if d.eng != eng)
        for x in w:
            deps.extend(x.writers)
            deps.extend(x.readers)
            deps.extend(x.prev_readers)
        for x in pw:
            if x.readers:
                x.prev_readers = x.readers
                x.readers = []
                x.writers = []
            deps.extend(x.prev_readers)
        seen = set()
        for d in deps:
            if id(d) in seen:
                continue
            seen.add(id(d))
            if (not d.is_dma) and (not is_dma) and d.eng == eng:
                if eng == "pe" or not SAME_ENGINE_SYNC:
                    continue
            o.deps.append(d)
        for x in r:
            _push(x.readers, o)
        for x in w:
            x.writers = [o]
            x.readers = []
            x.prev_readers = []
        for x in pw:
            _push(x.writers, o)
        self.ops.append(o)
        return o

    def op(self, eng, fn, r=(), w=(), pw=()):
        return self._add(eng, fn, r, w, False, pw)

    def dma(self, queue, out, in_, r=(), w=(), is_output=False, pw=()):
        o = self._add(queue, lambda e: e.dma_start(out=out, in_=in_), r, w, True, pw)
        if is_output:
            self.out_dmas.append(o)
        return o

    def mm(self, out, lhsT, rhs, start, stop, r=(), w=(), pw=()):
        return self.op("pe", lambda e: e.matmul(out, lhsT=lhsT, rhs=rhs, start=start, stop=stop,
                                                skip_group_check=True), r, w, pw)

    def tr(self, out, in_, ident, r=(), w=(), pw=()):
        return self.op("pe", lambda e: e.transpose(out, in_, ident), r, w, pw)

    def act(self, out, in_, func, scale=1.0, bias=0.0, accum_out=None, r=(), w=(), pw=()):
        return self.op("act", lambda e: e.activation(out=out, in_=in_, func=func, bias=bias, scale=scale,
                                                     accum_out=accum_out), r, w, pw)

    def cp(self, eng, out, in_, r=(), w=(), pw=()):
        if eng == "act":
            return self.op("act", lambda e: e.copy(out=out, in_=in_), r, w, pw)
        return self.op(eng, lambda e: e.tensor_copy(out=out, in_=in_), r, w, pw)

    def tt(self, eng, out, in0, in1, op, r=(), w=(), pw=()):
        return self.op(eng, lambda e: e.tensor_tensor(out=out, in0=in0, in1=in1, op=op), r, w, pw)

    def ts(self, eng, out, in0, s1, op0, s2=None, op1=None, r=(), w=(), pw=()):
        if op1 is None:
            return self.op(eng, lambda e: e.tensor_scalar(out=out, in0=in0, scalar1=s1, scalar2=None, op0=op0), r, w, pw)
        return self.op(eng, lambda e: e.tensor_scalar(out=out, in0=in0, scalar1=s1, scalar2=s2, op0=op0, op1=op1), r, w, pw)

    def stt(self, out, in0, scalar, in1, op0, op1, r=(), w=(), pw=()):
        return self.op("dve", lambda e: e.scalar_tensor_tensor(out=out, in0=in0, scalar=scalar, in1=in1,
                                                               op0=op0, op1=op1), r, w, pw)

    def memset(self, eng, ap, val, w=(), pw=()):
        return self.op(eng, lambda e: e.memset(ap, val), (), w, pw)

    def emit(self):
        nc = self.nc
        ops = self.ops
        for o in ops:
            for d in o.deps:
                d.needed = True
        with ExitStack() as es:
            eng_sem = {e: es.enter_context(nc.semaphore("s_" + e)) for e in ENGS}
            n_hw, n_sw = self.n_dma_sems, 16
            dma_sems = [es.enter_context(nc.semaphore("d%d" % i)) for i in range(n_hw + n_sw)]
            dma_tot = [0] * (n_hw + n_sw)
            cnt = {e: 0 for e in ENGS}
            k_hw = k_sw = 0
            for o in ops:
                if o.is_dma:
                    if o.eng == "pool":
                        si = n_hw + (k_sw % n_sw)
                        k_sw += 1
                    else:
                        si = k_hw % n_hw
                        k_hw += 1
                    o.sem = dma_sems[si]
                    o.prev_val = dma_tot[si]
                    dma_tot[si] += 16
                    o.val = dma_tot[si]
                elif o.needed:
                    cnt[o.eng] += 1
                    o.sem = eng_sem[o.eng]
                    o.val = cnt[o.eng]
            per = {e: [o for o in ops if o.eng == e] for e in ENGS}
            final_waits = [(s, v) for s, v in zip(dma_sems, dma_tot) if v > 0]

            def replay(ename, eobj, extra_final=False):
                known = {}
                for o in per[ename]:
                    waits = [(d.sem, d.val) for d in o.deps]
                    if o.is_dma and o.prev_val > 0:
                        waits.append((o.sem, o.prev_val))
                    for (s, v) in waits:
                        key = id(s)
                        if known.get(key, 0) >= v:
                            continue
                        eobj.wait_ge(s, v)
                        known[key] = v
                    ins = o.fn(eobj)
                    if o.is_dma:
                        ins.then_inc(o.sem, 16)
                    elif o.needed:
                        ins.then_inc(o.sem, 1)
                if extra_final:
                    for (s, v) in final_waits:
                        if known.get(id(s), 0) >= v:
                            continue
                        eobj.wait_ge(s, v)
                        known[id(s)] = v

            with nc.Block() as block:
                @block.sync
                def _(e):
                    replay("sp", e, extra_final=True)

                @block.scalar
                def _(e):
                    replay("act", e)

                @block.vector
                def _(e):
                    replay("dve", e)

                @block.gpsimd
                def _(e):
                    replay("pool", e)

                @block.tensor
                def _(e):
                    replay("pe", e)


class Ctx:
    def __init__(self, nc):
        self.nc = nc
        self.P = Prog(nc)
        self.n = 0

    def dram(self, name, shape, dt, out=False):
        return self.nc.dram_tensor(name, list(shape), dt, kind="ExternalOutput" if out else "ExternalInput").ap()

    def sb(self, shape, dt, name=None):
        self.n += 1
        return self.nc.alloc_sbuf_tensor(name or ("t%d" % self.n), list(shape), dt)

    def ps(self, shape, dt, name=None):
        self.n += 1
        return self.nc.alloc_psum_tensor(name or ("p%d" % self.n), list(shape), dt)


def split_bf16_2(x):
    a = np.float32(np.asarray(x, np.float32).astype(NPBF).astype(np.float32))
    b = np.float32(np.asarray(np.float32(x) - a, np.float32).astype(NPBF).astype(np.float32))
    return float(a), float(b)


def load_w_bf16(C, dst, src, rows, cols, r_w, queue="pool"):
    kc = rows // 128
    step = 1024
    for k in range(kc):
        for c0 in range(0, cols, step):
            c1 = min(cols, c0 + step)
            if kc == 1 and len(dst.shape) == 2:
                o = dst[:, c0:c1]
            else:
                o = dst[:, k, c0:c1]
            C.P.dma(queue, o, src[k * 128:(k + 1) * 128, c0:c1], pw=[r_w])


def rmsnorm_tile(C, x_ap, g_ap, out_ap, width, scr, rx, rg, ro, tag):
    P = C.P
    junk, ss, rstd = scr["junk"], scr["ss"], scr["rstd"]
    rs = scr["res"]
    P.act(junk[:, 0:width], x_ap, AF.Square, accum_out=ss[:, 0:1], r=[rx], w=[rs])
    P.ts("dve", rstd[:, 0:1], ss[:, 0:1], 1.0 / width, ALU.mult, EPS, ALU.add, r=[rs], w=[rs])
    P.op("act", lambda e: e.sqrt(out=rstd[:, 0:1], in_=rstd[:, 0:1]), r=[rs], w=[rs])
    P.op("dve", lambda e: e.reciprocal(out=rstd[:, 0:1], in_=rstd[:, 0:1]), r=[rs], w=[rs])
    P.stt(out_ap, x_ap, rstd[:, 0:1], g_ap, ALU.mult, ALU.mult, r=[rx, rg, rs], w=[ro])


DBG_STOP = 99


def build_A(S):
    TO = S // 2
    TT = TO // 128
    NSLOT = TT // 4
    nc = bass.Bass("TRN2", target_bir_lowering=False)
    C = Ctx(nc)
    P = C.P
    h_in = C.dram("h", [TO, D], F32)
    pos_d = C.dram("pos", [128, TT], I32)
    pos0_d = C.dram("pos0", [1, 1], I32)
    onehot_d = C.dram("onehot", [128, TT, 32], BF16)
    invf_d = C.dram("invf", [1, 16], F32)
    ident_d = C.dram("ident", [128, 128], F32)
    g_attn_d = C.dram("attn_norm", [1, D], F32)
    w_in_d = C.dram("w_in", [D, DIN], F32)
    g_q_d = C.dram("q_norm", [1, 256], F32)
    w_uq_d = C.dram("w_uq", [256, 576], F32)
    g_kv_d = C.dram("kv_norm", [1, 128], F32)
    w_ukv_d = C.dram("w_ukv", [128, 768], F32)
    kA_d = C.dram("kA", [6, 128, TT, KA_A], BF16, out=True)
    qA_d = C.dram("qA", [6, 128, TT, KA_A], BF16, out=True)
    vA_d = C.dram("vA", [6, 128, TT, 65], BF16, out=True)
    kD_d = C.dram("kD", [12, 128, TT, KA_D], BF16, out=True)
    qD_d = C.dram("qD", [12, 128, TT, KA_D], BF16, out=True)
    vD_d = C.dram("vD", [6, 128, TT, 65], BF16, out=True)
    kM_d = C.dram("kM", [4, 128, TT, KA_M], BF16, out=True)
    qM_d = C.dram("qM", [4, 128, TT, KA_M], BF16, out=True)
    vM_d = C.dram("vM", [4, 128, TT, 65], BF16, out=True)

    w_in = C.sb([128, 8, DIN], BF16)
    w_uq = C.sb([128, 2, 576], BF16)
    w_ukv = C.sb([128, 768], BF16)
    ident = C.sb([128, 128], BF16)
    g_attn = C.sb([128, D], F32)
    g_q = C.sb([128, 256], F32)
    g_kv = C.sb([128, 128], F32)
    r_w = Res("weights")
    P.dma("pool", ident[:], ident_d, pw=[r_w])
    P.dma("sp", g_attn[:], g_attn_d[0, :].partition_broadcast(128), pw=[r_w])
    P.dma("sp", g_q[:], g_q_d[0, :].partition_broadcast(128), pw=[r_w])
    P.dma("sp", g_kv[:], g_kv_d[0, :].partition_broadcast(128), pw=[r_w])
    load_w_bf16(C, w_in, w_in_d, D, DIN, r_w)
    load_w_bf16(C, w_uq, w_uq_d, 256, 576, r_w)
    load_w_bf16(C, w_ukv, w_ukv_d, 128, 768, r_w)

    r_pos = Res("pos")
    pos_i = C.sb([128, TT], I32)
    pos0_i = C.sb([128, 1], I32)
    pos_f = C.sb([128, TT], F32)
    pos0_f = C.sb([128, 1], F32)
    prel_f = C.sb([128, TT], F32)
    prel_i = C.sb([128, TT], I32)
    hi_i = C.sb([128, TT], I32)
    lo_i = C.sb([128, TT], I32)
    hl = C.sb([128, 2, TT], F32)
    invf = C.sb([128, 16], F32)
    ang = C.sb([128, TT, 16], F32)
    kq = C.sb([128, TT, 16], F32)
    kq_i = C.sb([128, TT, 16], I32)
    cos_t = C.sb([128, TT, 16], F32)
    sin_t = C.sb([128, TT, 16], F32)
    P.dma("sp", pos_i[:], pos_d, w=[r_pos])
    P.dma("sp", pos0_i[:], pos0_d[0, :].partition_broadcast(128), w=[r_pos])
    P.dma("sp", invf[:], invf_d[0, :].partition_broadcast(128), w=[r_pos])
    P.cp("dve", pos_f[:], pos_i[:], r=[r_pos], w=[r_pos])
    P.cp("dve", pos0_f[:], pos0_i[:], r=[r_pos], w=[r_pos])
    P.ts("dve", prel_f[:], pos_f[:], pos0_f[:, 0:1], ALU.subtract, r=[r_pos], w=[r_pos])
    P.cp("dve", prel_i[:], prel_f[:], r=[r_pos], w=[r_pos])
    P.op("dve", lambda e: e.tensor_single_scalar(out=hi_i[:], in_=prel_i[:], scalar=7, op=ALU.arith_shift_right),
         r=[r_pos], w=[r_pos])
    P.op("dve", lambda e: e.tensor_single_scalar(out=lo_i[:], in_=prel_i[:], scalar=127, op=ALU.bitwise_and),
         r=[r_pos], w=[r_pos])
    P.cp("dve", hl[:, 0, :], hi_i[:], r=[r_pos], w=[r_pos])
    P.cp("dve", hl[:, 1, :], lo_i[:], r=[r_pos], w=[r_pos])
    TWO_PI = 2.0 * math.pi
    c1 = float(np.float32(TWO_PI))
    c2 = float(TWO_PI - np.float64(np.float32(TWO_PI)))
    for t in range(TT):
        P.ts("dve", ang[:, t, :], invf[:], pos_f[:, t:t + 1], ALU.mult, r=[r_pos], w=[r_pos])
    P.ts("dve", kq[:], ang[:], 1.0 / TWO_PI, ALU.mult, r=[r_pos], w=[r_pos])
    P.cp("dve", kq_i[:], kq[:], r=[r_pos], w=[r_pos])
    P.cp("dve", kq[:], kq_i[:], r=[r_pos], w=[r_pos])
    P.stt(ang[:], kq[:], -c1, ang[:], ALU.mult, ALU.add, r=[r_pos], w=[r_pos])
    P.stt(ang[:], kq[:], -c2, ang[:], ALU.mult, ALU.add, r=[r_pos], w=[r_pos])
    P.ts("dve", ang[:], ang[:], math.pi, ALU.min, -math.pi, ALU.max, r=[r_pos], w=[r_pos])
    P.act(sin_t[:], ang[:], AF.Sin, r=[r_pos], w=[r_pos])
    P.stt(kq[:], ang[:], -1.0, ang[:], ALU.mult, ALU.max, r=[r_pos], w=[r_pos])
    P.ts("dve", kq[:], kq[:], -1.0, ALU.mult, math.pi / 2, ALU.add, r=[r_pos], w=[r_pos])
    P.act(cos_t[:], kq[:], AF.Sin, r=[r_pos], w=[r_pos])
    augK = C.sb([128, N_ALIBI, TT, 8], BF16)
    augQ = C.sb([128, N_ALIBI, TT, 8], BF16)
    r_aug = Res("aug")
    for hh in range(N_ALIBI):
        d_h = 32 if hh < 6 else 64
        cc = SLOPES[hh] / (d_h ** -0.5)
        a1, a2 = split_bf16_2(cc)
        for j, v in enumerate([-128 * a1, -128 * a2, -a1, -a2]):
            P.memset("pool", augK[:, hh, :, 4 + j], v, pw=[r_aug])
        for j, v in enumerate([128 * a1, 128 * a2, a1, a2]):
            P.memset("pool", augQ[:, hh, :, j], v, pw=[r_aug])
        for j in range(4):
            P.cp("dve", augK[:, hh, :, j], hl[:, j // 2, :], r=[r_pos], pw=[r_aug])
            P.cp("dve", augQ[:, hh, :, 4 + j], hl[:, j // 2, :], r=[r_pos], pw=[r_aug])
    onehot = C.sb([128, TT, 32], BF16)
    P.dma("sp", onehot[:], onehot_d, pw=[r_aug])

    if DBG_STOP == 0:
        P.emit()
        return nc
    NB = 2
    hbuf = [C.sb([128, D], F32) for _ in range(3)]
    r_h = [Res("h%d" % i) for i in range(3)]
    nbf = [C.sb([128, D], BF16) for _ in range(2)]
    r_n = [Res("n%d" % i) for i in range(2)]
    nT = [C.sb([128, 8, 512], BF16) for _ in range(NB)]
    r_nT = [Res("nT%d" % i) for i in range(NB)]
    scr = {"junk": C.sb([128, D], BF16), "ss": C.sb([128, 1], F32), "rstd": C.sb([128, 1], F32), "res": Res("scr")}
    scr2 = {"junk": C.sb([128, 256], BF16), "ss": C.sb([128, 1], F32), "rstd": C.sb([128, 1], F32), "res": Res("scr2")}
    cqn = [C.sb([128, 384], BF16) for _ in range(2)]
    r_cqn = [Res("cqn%d" % i) for i in range(2)]
    cT = [C.sb([128, 3, 512], BF16) for _ in range(NB)]
    r_cT = [Res("cT%d" % i) for i in range(NB)]
    ropet = [C.sb([128, 6, 16], F32) for _ in range(4)]
    r_rope = [Res("ropetmp%d" % i) for i in range(6)]
    krt = C.sb([128, 32], BF16)
    r_kr = Res("krt")
    stKA = [C.sb([128, 6, 4, KA_A], BF16) for _ in range(NB)]
    stQA = [C.sb([128, 6, 4, KA_A], BF16) for _ in range(NB)]
    stVA = [C.sb([128, 6, 4, 65], BF16) for _ in range(NB)]
    stKD = [C.sb([128, 12, 4, KA_D], BF16) for _ in range(NB)]
    stQD = [C.sb([128, 12, 4, KA_D], BF16) for _ in range(NB)]
    stVD = [C.sb([128, 6, 4, 65], BF16) for _ in range(NB)]
    stKM = [C.sb([128, 4, 4, KA_M], BF16) for _ in range(NB)]
    stQM = [C.sb([128, 4, 4, KA_M], BF16) for _ in range(NB)]
    stVM = [C.sb([128, 4, 4, 65], BF16) for _ in range(NB)]
    r_st = [Res("st%d" % i) for i in range(NB)]
    for b in range(NB):
        for v in (stVA[b], stVD[b], stVM[b]):
            P.memset("pool", v[:, :, :, 64], 1.0, pw=[r_st[b]])
        P.memset("pool", stQM[b][:, :, :, 64:96], 0.0, pw=[r_st[b]])
    ptr = [C.ps([128, D], BF16) for _ in range(2)]
    r_ptr = [Res("ptr%d" % i, psum=True) for i in range(2)]
    pp = [C.ps([128, 512], F32) for _ in range(4)]
    r_pp = [Res("pp%d" % i, psum=True) for i in range(4)]
    pq = [C.ps([128, 512], F32) for _ in range(2)]
    r_pq = [Res("pq%d" % i, psum=True) for i in range(2)]
    cnt = {"h": 0, "n": 0, "ptr": 0, "pp": 0, "pq": 0, "ev": 0}

    def evac(out, in_, r, w=(), pw=()):
        cnt["ev"] += 1
        P.cp("act" if cnt["ev"] % 2 else "dve", out, in_, r=r, w=w, pw=pw)

    chunks = [(0, 416), (416, 800), (800, 1184), (1184, 1568), (1568, 2080), (2080, 2336)]
    for s in range(NSLOT):
        b = s % NB
        rst = r_st[b]
        tsl = slice(4 * s, 4 * s + 4)
        for i in range(2):
            P.cp("pool", stKD[b][:, i::2, :, 32:40], augK[:, 0:6, tsl, :], r=[r_aug], pw=[rst])
            P.cp("pool", stQD[b][:, i::2, :, 32:40], augQ[:, 0:6, tsl, :], r=[r_aug], pw=[rst])
        P.cp("pool", stKM[b][:, :, :, 96:104], augK[:, 6:10, tsl, :], r=[r_aug], pw=[rst])
        P.cp("pool", stQM[b][:, :, :, 96:104], augQ[:, 6:10, tsl, :], r=[r_aug], pw=[rst])
        for hh in range(4):
            P.cp("pool", stKM[b][:, hh, :, 64:96], onehot[:, tsl, :], r=[r_aug], pw=[rst])
        for tt in range(4):
            t = 4 * s + tt
            hi_ = cnt["h"] % 3
            cnt["h"] += 1
            P.dma("sp", hbuf[hi_][:], h_in[t * 128:(t + 1) * 128, :], w=[r_h[hi_]])
            ni = cnt["n"] % 2
            cnt["n"] += 1
            rmsnorm_tile(C, hbuf[hi_][:], g_attn[:], nbf[ni][:], D, scr, r_h[hi_], r_w, r_n[ni], "a")
            pi = cnt["ptr"] % 2
            cnt["ptr"] += 1
            for kc in range(8):
                P.tr(ptr[pi][:, kc * 128:(kc + 1) * 128], nbf[ni][:, kc * 128:(kc + 1) * 128], ident[:],
                     r=[r_n[ni], r_w], pw=[r_ptr[pi]])
            evac(nT[b][:, :, tt * 128:(tt + 1) * 128], ptr[pi][:].rearrange("p (k c) -> p k c", k=8),
                 r=[r_ptr[pi]], pw=[r_nT[b]])
        if DBG_STOP == 1:
            P.emit()
            return nc
        for tt in range(4):
            t = 4 * s + tt
            ci = cnt["n"] % 2
            cnt["n"] += 1
            for ch, (c0, c1) in enumerate(chunks):
                pi = cnt["pp"] % 4
                cnt["pp"] += 1
                w_ = c1 - c0
                for kc in range(8):
                    P.mm(pp[pi][:, 0:w_], nT[b][:, kc, tt * 128:(tt + 1) * 128], w_in[:, kc, c0:c1],
                         kc == 0, kc == 7, r=[r_nT[b], r_w], w=[r_pp[pi]])
                src = pp[pi]
                rp = r_pp[pi]
                if ch == 0:
                    rmsnorm_tile(C, src[:, 0:256], g_q[:], cqn[ci][:, 0:256], 256, scr2, rp, r_w, r_cqn[ci], "q")
                    rmsnorm_tile(C, src[:, 256:384], g_kv[:], cqn[ci][:, 256:384], 128, scr2, rp, r_w, r_cqn[ci], "kv")
                    x1 = src[:, 384:400]
                    x2 = src[:, 400:416]
                    co = cos_t[:, t, :]
                    si = sin_t[:, t, :]
                    ta, tb = ropet[0][:, 0, :], ropet[1][:, 0, :]
                    P.tt("dve", ta, x1, co, ALU.mult, r=[rp, r_pos], w=[r_rope[0]])
                    P.tt("dve", tb, x2, si, ALU.mult, r=[rp, r_pos], w=[r_rope[1]])
                    P.tt("dve", krt[:, 0:16], ta, tb, ALU.subtract, r=[r_rope[0], r_rope[1]], w=[r_kr])
                    P.tt("dve", ta, x1, si, ALU.mult, r=[rp, r_pos], w=[r_rope[0]])
                    P.tt("dve", tb, x2, co, ALU.mult, r=[rp, r_pos], w=[r_rope[1]])
                    P.tt("dve", krt[:, 16:32], ta, tb, ALU.add, r=[r_rope[0], r_rope[1]], pw=[r_kr])
                    for hh in range(6):
                        P.cp("pool", stKA[b][:, hh, tt, 64:96], krt[:], r=[r_kr], pw=[rst])
                    ti = cnt["ptr"] % 2
                    cnt["ptr"] += 1
                    for kc in range(3):
                        P.tr(ptr[ti][:, kc * 128:(kc + 1) * 128], cqn[ci][:, kc * 128:(kc + 1) * 128], ident[:],
                             r=[r_cqn[ci], r_w], w=[r_ptr[ti]])
                    evac(cT[b][:, :, tt * 128:(tt + 1) * 128],
                         ptr[ti][:, 0:384].rearrange("p (k c) -> p k c", k=3), r=[r_ptr[ti]], pw=[r_cT[b]])
                elif ch == 1:
                    evac(stQD[b][:, :, tt, 0:32], src[:, 0:384].rearrange("p (m c) -> p m c", m=12), r=[rp], pw=[rst])
                elif ch == 2:
                    evac(stKD[b][:, :, tt, 0:32], src[:, 0:384].rearrange("p (m c) -> p m c", m=12), r=[rp], pw=[rst])
                elif ch == 3:
                    evac(stVD[b][:, :, tt, 0:64], src[:, 0:384].rearrange("p (m c) -> p m c", m=6), r=[rp], pw=[rst])
                elif ch == 4:
                    evac(stQM[b][:, :, tt, 0:64], src[:, 0:256].rearrange("p (m c) -> p m c", m=4), r=[rp], pw=[rst])
                    evac(stKM[b][:, :, tt, 0:64], src[:, 256:512].rearrange("p (m c) -> p m c", m=4), r=[rp], pw=[rst])
                else:
                    evac(stVM[b][:, :, tt, 0:64], src[:, 0:256].rearrange("p (m c) -> p m c", m=4), r=[rp], pw=[rst])
        if DBG_STOP == 2:
            P.emit()
            return nc
        for tt in range(4):
            t = 4 * s + tt
            co = cos_t[:, t, :]
            si = sin_t[:, t, :]
            for half in range(2):
                qi = cnt["pq"] % 2
                cnt["pq"] += 1
                for kc in range(2):
                    P.mm(pq[qi][:, 0:288], cT[b][:, kc, tt * 128:(tt + 1) * 128], w_uq[:, kc, half * 288:(half + 1) * 288],
                         kc == 0, kc == 1, r=[r_cT[b], r_w], w=[r_pq[qi]])
                v3 = pq[qi][:, 0:288].rearrange("p (m c) -> p m c", m=3)
                hs = slice(3 * half, 3 * half + 3)
                evac(stQA[b][:, hs, tt, 0:64], v3[:, :, 0:64], r=[r_pq[qi]], pw=[rst])
                for m in range(3):
                    hh = 3 * half + m
                    x1 = pq[qi][:, m * 96 + 64:m * 96 + 80]
                    x2 = pq[qi][:, m * 96 + 80:m * 96 + 96]
                    ta, tb, tc, td = (ropet[i][:, hh, :] for i in range(4))
                    rr_ = r_rope[hh]
                    P.tt("dve", ta, x1, co, ALU.mult, r=[r_pq[qi], r_pos], w=[rr_])
                    P.tt("dve", tb, x2, si, ALU.mult, r=[r_pq[qi], r_pos], pw=[rr_])
                    P.tt("dve", tc, x1, si, ALU.mult, r=[r_pq[qi], r_pos], pw=[rr_])
                    P.tt("dve", td, x2, co, ALU.mult, r=[r_pq[qi], r_pos], pw=[rr_])
                    P.tt("pool", stQA[b][:, hh, tt, 64:80], ta, tb, ALU.subtract, r=[rr_], pw=[rst])
                    P.tt("pool", stQA[b][:, hh, tt, 80:96], tc, td, ALU.add, r=[rr_], pw=[rst])
                qi = cnt["pq"] % 2
                cnt["pq"] += 1
                P.mm(pq[qi][:, 0:384], cT[b][:, 2, tt * 128:(tt + 1) * 128], w_ukv[:, half * 384:(half + 1) * 384],
                     True, True, r=[r_cT[b], r_w], w=[r_pq[qi]])
                v3 = pq[qi][:, 0:384].rearrange("p (m c) -> p m c", m=3)
                evac(stKA[b][:, hs, tt, 0:64], v3[:, :, 0:64], r=[r_pq[qi]], pw=[rst])
                evac(stVA[b][:, hs, tt, 0:64], v3[:, :, 64:128], r=[r_pq[qi]], pw=[rst])
        if DBG_STOP == 3:
            P.emit()
            return nc
        for (st, dd, nm) in ((stKA, kA_d, 6), (stQA, qA_d, 6), (stVA, vA_d, 6), (stKD, kD_d, 12), (stQD, qD_d, 12),
                             (stVD, vD_d, 6), (stKM, kM_d, 4), (stQM, qM_d, 4), (stVM, vM_d, 4)):
            for m in range(nm):
                P.dma("sp", dd[m, :, tsl, :], st[b][:, m, :, :], r=[rst], is_output=True)
    P.emit()
    return nc


def own_qtiles(S, parity):
    nqt = S // 512
    out = []
    for p in range(nqt // 2):
        out.append(2 * p + parity if p % 2 == 0 else 2 * p + 1 - parity)
    return out


def own_tiles(S, parity):
    return [4 * q + i for q in own_qtiles(S, parity) for i in range(4)]


def const_inputs_A(S, parity):
    tiles = own_tiles(S, parity)
    TT = len(tiles)
    onehot = np.zeros((128, TT, 32), np.float32)
    for i, g in enumerate(tiles):
        onehot[:, i, g // 2] = 1.0
    invf = (np.float32(10000.0) ** (-np.arange(16, dtype=np.float32) / np.float32(16))).astype(np.float32)
    return {"onehot": onehot.astype(NPBF), "invf": invf.reshape(1, 16),
            "ident": np.eye(128, dtype=np.float32)}


def tok_rows(S, parity):
    return np.concatenate([np.arange(g * 128, (g + 1) * 128) for g in own_tiles(S, parity)])


def const_inputs_B(S, parity):
    tiles = own_tiles(S, parity)
    TT = len(tiles)
    kk = np.arange(128)[:, None]
    qq = np.arange(512)[None, :]
    diag = [np.where(d * 128 + kk <= qq, 0.0, NEGM).astype(np.float32) for d in range(4)]
    full = np.full((128, 512), NEGM, np.float32)
    zero = np.zeros((128, 512), np.float32)
    role_min = np.stack(diag + [full] * 4)
    role_max = np.stack([zero] * 4 + diag)
    m_even = role_min if parity == 0 else role_max
    m_odd = role_max if parity == 0 else role_min
    pastmask = np.zeros((128, TT, 32), np.float32)
    isown = np.zeros((128, TT, 32), np.float32)
    for i, g in enumerate(tiles):
        ob = g // 2
        pastmask[:, i, ob:] = -1e30
        if ob < 32:
            isown[:, i, ob] = 1.0
    return {"m_even": m_even.astype(NPBF), "m_odd": m_odd.astype(NPBF), "pastmask": pastmask, "isown": isown,
            "ident": np.eye(128, dtype=np.float32)}


def build_B(S, lam_init):
    TO = S // 2
    TT = TO // 128
    NSLOT = TT // 4
    NKT = S // 128
    NBLK = S // 256
    nc = bass.Bass("TRN2", target_bir_lowering=False)
    C = Ctx(nc)
    P = C.P
    kA_d = C.dram("kA", [6, 128, NKT, KA_A], BF16)
    qA_d = C.dram("qA", [6, 128, TT, KA_A], BF16)
    vA_d = C.dram("vA", [6, 128, NKT, 65], BF16)
    kD_d = C.dram("kD", [12, 128, NKT, KA_D], BF16)
    qD_d = C.dram("qD", [12, 128, TT, KA_D], BF16)
    vD_d = C.dram("vD", [6, 128, NKT, 65], BF16)
    kM_d = C.dram("kM", [4, 128, NKT, KA_M], BF16)
    qM_d = C.dram("qM", [4, 128, TT, KA_M], BF16)
    vM_d = C.dram("vM", [4, 128, NKT, 65], BF16)
    m_even_d = C.dram("m_even", [8, 128, 512], BF16)
    m_odd_d = C.dram("m_odd", [8, 128, 512], BF16)
    pastmask_d = C.dram("pastmask", [128, TT, 32], F32)
    isown_d = C.dram("isown", [128, TT, 32], F32)
    ident_d = C.dram("ident", [128, 128], F32)
    lam_d = [C.dram(n, [1, 32], F32) for n in ("lq1", "lk1", "lq2", "lk2")]
    gsub_d = C.dram("sub_norm", [1, 64], F32)
    o_d = C.dram("o", [128, TT, D], BF16, out=True)

    r_c = Res("consts")
    ident = C.sb([128, 128], BF16)
    masks = [C.sb([128, 8, 512], BF16) for _ in range(2)]
    pastmask = C.sb([128, TT, 32], F32)
    isown = C.sb([128, TT, 32], F32)
    lamt = [C.sb([128, 32], F32) for _ in range(4)]
    gsub = C.sb([128, 64], F32)
    P.dma("pool", ident[:], ident_d, pw=[r_c])
    for i, md in enumerate((m_even_d, m_odd_d)):
        for j in range(8):
            P.dma("sp", masks[i][:, j, :], md[j], pw=[r_c])
    P.dma("sp", pastmask[:], pastmask_d, pw=[r_c])
    P.dma("sp", isown[:], isown_d, pw=[r_c])
    for i in range(4):
        P.dma("sp", lamt[i][:], lam_d[i][0, :].partition_broadcast(128), pw=[r_c])
    P.dma("sp", gsub[:], gsub_d[0, :].partition_broadcast(128), pw=[r_c])
    r_lam = Res("lam")
    lprod = C.sb([128, 32], F32)
    lsum = C.sb([128, 2], F32)
    neg_lam = C.sb([128, 1], F32)
    for i in range(2):
        P.tt("dve", lprod[:], lamt[2 * i][:], lamt[2 * i + 1][:], ALU.mult, r=[r_c], w=[r_lam])
        P.op("dve", lambda e, i=i: e.reduce_sum(out=lsum[:, i:i + 1], in_=lprod[:], axis=AX.X), r=[r_lam], w=[r_lam])
    P.act(lsum[:], lsum[:], AF.Exp, r=[r_lam], w=[r_lam])
    P.stt(neg_lam[:], lsum[:, 1:2], -float(lam_init), lsum[:, 0:1], ALU.add, ALU.subtract, r=[r_lam], w=[r_lam])
    P.ts("dve", gsub[:], gsub[:], 1.0 - float(lam_init), ALU.mult, r=[r_c], w=[r_c])

    KT = [C.sb([128, 2, S], BF16) for _ in range(2)]
    QT = [C.sb([128, 2, TO], BF16) for _ in range(2)]
    V = [C.sb([128, NKT, 65], BF16) for _ in range(2)]
    r_KT = [Res("KT%d" % i) for i in range(2)]
    r_QT = [Res("QT%d" % i) for i in range(2)]
    r_V = [Res("V%d" % i) for i in range(2)]
    CH = 16 if NKT >= 16 else NKT
    stg = [C.sb([128, CH, KA_M], BF16) for _ in range(3)]
    r_stg = [Res("stg%d" % i) for i in range(3)]
    PT = [C.sb([128, 512], BF16) for _ in range(4)]
    r_PT = [Res("PT%d" % i) for i in range(4)]
    osl = [C.sb([128, 4, 64], BF16) for _ in range(2)]
    r_osl = [Res("osl%d" % i) for i in range(2)]
    pS4 = C.ps([128, 4, 512], F32)
    pS = [pS4[:, i, :] for i in range(4)]
    r_pS = [Res("pS%d" % i, psum=True) for i in range(4)]
    pO = [C.ps([128, 512], F32) for _ in range(3)]
    r_pO = [Res("pO%d" % i, psum=True) for i in range(3)]
    pT = [C.ps([128, 1024], BF16) for _ in range(1)]
    r_pT = [Res("pT%d" % i, psum=True) for i in range(1)]
    pG = pS4[:, 0, :]
    r_pG = r_pS[0]
    PT2 = [C.sb([128, 2, 512], BF16) for _ in range(3)]
    r_PT2 = [Res("PT2_%d" % i) for i in range(3)]
    kmT = C.sb([64, 32], F32)
    kmT_bf = C.sb([64, 32], BF16)
    gm = C.sb([128, 32], F32)
    m8 = C.sb([128, 8], F32)
    msel = C.sb([128, 4, 32], BF16)
    r_g = Res("gate")
    r_ms = Res("msel")
    rz = C.sb([128, 2, 4], F32)
    t1 = C.sb([128, 4, 64], F32)
    t2 = C.sb([128, 4, 64], F32)
    sq = C.sb([128, 4, 64], F32)
    ssd = C.sb([128, 4], F32)
    r_nrm = Res("nrm")
    cnt = {"stg": 0, "pT": 0, "pS": 0, "pO": 0, "PT": 0, "osl": 0}

    units = []
    for h in range(6):
        units.append(dict(kind="A", maps=[(kA_d[h], qA_d[h], KA_A)], v=vA_d[h], scale=96 ** -0.5, col=h * 64))
    for h in range(6):
        units.append(dict(kind="D", maps=[(kD_d[2 * h + i], qD_d[2 * h + i], KA_D) for i in range(2)], v=vD_d[h],
                          scale=32 ** -0.5, col=384 + h * 64))
    for h in range(4):
        units.append(dict(kind="M", maps=[(kM_d[h], qM_d[h], KA_M)], v=vM_d[h], scale=64 ** -0.5, col=768 + h * 64))

    for si in range(3):
        P.memset("pool", stg[si][:], 0.0, w=[r_stg[si]])

    def transpose_in(src_d, ntiles, dst, mi, KA, r_dst, src2=None):
        for c0 in range(0, ntiles, CH):
            n = min(CH, ntiles - c0)
            si = cnt["stg"] % 3
            cnt["stg"] += 1
            if src2 is None:
                P.dma("sp", stg[si][:, 0:n, 0:KA], src_d[:, c0:c0 + n, :], w=[r_stg[si]])
            else:
                P.dma("sp", stg[si][:, 0:n, 0:KA_D], src_d[:, c0:c0 + n, :], w=[r_stg[si]])
                P.dma("sp", stg[si][:, 0:n, 64:64 + KA_D], src2[:, c0:c0 + n, :], pw=[r_stg[si]])
            for g0 in range(0, n, 4):
                gn = min(4, n - g0)
                pi = cnt["pT"] % len(pT)
                cnt["pT"] += 1
                for t in range(gn):
                    P.tr(pT[pi][0:KA, t * 128:(t + 1) * 128], stg[si][:, g0 + t, 0:KA], ident[:],
                         r=[r_stg[si], r_c], pw=[r_pT[pi]])
                c = (c0 + g0) * 128
                P.cp("dve", dst[0:KA, mi, c:c + gn * 128], pT[pi][0:KA, 0:gn * 128], r=[r_pT[pi]], pw=[r_dst])

    def prep(u):
        U = units[u]
        ub = u % 2
        for c0 in range(0, NKT, 32):
            n = min(32, NKT - c0)
            P.dma("sp", V[ub][:, c0:c0 + n, :], U["v"][:, c0:c0 + n, :], pw=[r_V[ub]])
        if U["kind"] == "D":
            (k0, q0, _), (k1, q1, _) = U["maps"]
            transpose_in(k0, NKT, KT[ub], 0, KA_M, r_KT[ub], src2=k1)
            transpose_in(q0, TT, QT[ub], 0, KA_M, r_QT[ub], src2=q1)
        else:
            for mi, (kd, qd, KA) in enumerate(U["maps"]):
                transpose_in(kd, NKT, KT[ub], mi, KA, r_KT[ub])
                transpose_in(qd, TT, QT[ub], mi, KA, r_QT[ub])
        if U["kind"] == "M":
            P.memset("dve", kmT[:], 0.0, w=[r_g])
            P.op("dve", lambda e: e.tensor_reduce(out=kmT[:, 0:NBLK],
                                                  in_=KT[ub][0:64, 0, :].rearrange("p (n k) -> p n k", k=256),
                                                  axis=AX.X, op=ALU.add), r=[r_KT[ub]], w=[r_g])
            P.ts("dve", kmT_bf[:], kmT[:], 1.0 / 256.0, ALU.mult, r=[r_g], w=[r_g])
            for g0 in range(0, TT, 4):
                for t4 in range(4):
                    t = g0 + t4
                    P.mm(pG[:, 0:32], QT[ub][0:64, 0, t * 128:(t + 1) * 128], kmT_bf[:, :], True, True,
                         r=[r_QT[ub], r_g], w=[r_pG])
                    P.tt("dve", gm[:], pG[:, 0:32], pastmask[:, t, :], ALU.add, r=[r_pG, r_c], w=[r_g])
                    P.op("dve", lambda e: e.max(out=m8[:], in_=gm[:]), r=[r_g], w=[r_g])
                    P.ts("dve", gm[:], gm[:], m8[:, 2:3], ALU.is_ge, r=[r_g], w=[r_g])
                    P.tt("dve", gm[:], gm[:], isown[:, t, :], ALU.max, r=[r_g, r_c], w=[r_g])
                    P.ts("dve", msel[:, t4, :], gm[:], -1.0, ALU.add, -NEGM, ALU.mult, r=[r_g],
                         **({"w": [r_ms]} if t4 == 0 else {"pw": [r_ms]}))
                pi = cnt["pT"] % len(pT)
                cnt["pT"] += 1
                for t4 in range(4):
                    P.tr(pT[pi][0:32, t4 * 128:(t4 + 1) * 128], msel[:, t4, :], ident[:], r=[r_ms, r_c], pw=[r_pT[pi]])
                P.cp("act", QT[ub][64:96, 0, g0 * 128:(g0 + 4) * 128], pT[pi][0:32, 0:512], r=[r_pT[pi]], pw=[r_QT[ub]])

    def attention(u):
        U = units[u]
        ub = u % 2
        scale = float(U["scale"])
        dual = U["kind"] == "D"
        for p in range(NSLOT):
            nkt = 8 * (p + 1)
            mk = masks[p % 2]
            obanks = []
            if not dual:
                KA = U["maps"][0][2]
                oi = cnt["pO"] % 3
                cnt["pO"] += 1
                obanks.append(oi)
                q_ap = QT[ub][0:KA, 0, p * 512:(p + 1) * 512]
                sbank = {}

                def qk(j):
                    si = cnt["pS"] % 4
                    cnt["pS"] += 1
                    sbank[j] = si
                    band = j >= 8 * p
                    P.mm(pS[si], KT[ub][0:KA, 0, j * 128:(j + 1) * 128], q_ap, True, not band,
                         r=[r_KT[ub], r_QT[ub]], w=[r_pS[si]])
                    if band:
                        P.mm(pS[si], ident[:, :], mk[:, j - 8 * p, :], False, True, r=[r_c], pw=[r_pS[si]])

                LA = 3
                for j in range(min(LA, nkt)):
                    qk(j)
                first = True
                for j in range(nkt):
                    si = sbank[j]
                    pi = cnt["PT"] % 4
                    cnt["PT"] += 1
                    P.act(PT[pi][:], pS[si], AF.Exp, scale=scale, r=[r_pS[si]], w=[r_PT[pi]])
                    d2 = j - (8 * p + 4)
                    for u4 in range(4):
                        if d2 >= 0 and u4 < d2:
                            continue
                        P.mm(pO[oi][:, u4 * 65:(u4 + 1) * 65], PT[pi][:, u4 * 128:(u4 + 1) * 128], V[ub][:, j, :],
                             first, j == nkt - 1, r=[r_PT[pi], r_V[ub]],
                             **({"w": [r_pO[oi]]} if first else {"pw": [r_pO[oi]]}))
                        first = False
                    if j + LA < nkt:
                        qk(j + LA)
            else:
                ois = []
                for _ in range(2):
                    ois.append(cnt["pO"] % 3)
                    cnt["pO"] += 1
                obanks.extend(ois)
                sbank = {}

                def qk2(j):
                    pr = cnt["pS"] % 2
                    cnt["pS"] += 1
                    sbank[j] = pr
                    band = j >= 8 * p
                    for m, base in enumerate((0, 64)):
                        si = 2 * pr + m
                        P.mm(pS[si], KT[ub][base:base + KA_D, 0, j * 128:(j + 1) * 128],
                             QT[ub][base:base + KA_D, 0, p * 512:(p + 1) * 512], True, not band,
                             r=[r_KT[ub], r_QT[ub]], w=[r_pS[si]])
                    if band:
                        for m in range(2):
                            si = 2 * pr + m
                            P.mm(pS[si], ident[:, :], mk[:, j - 8 * p, :], False, True, r=[r_c], pw=[r_pS[si]])

                qk2(0)
                firsts = [True, True]
                for j in range(nkt):
                    pr = sbank[j]
                    pi = cnt["PT"] % 3
                    cnt["PT"] += 1
                    P.act(PT2[pi][:], pS4[:, 2 * pr:2 * pr + 2, :], AF.Exp, scale=scale,
                          r=[r_pS[2 * pr], r_pS[2 * pr + 1]], w=[r_PT2[pi]])
                    if j + 1 < nkt:
                        qk2(j + 1)
                    d2 = j - (8 * p + 4)
                    for m in range(2):
                        oi = ois[m]
                        for u4 in range(4):
                            if d2 >= 0 and u4 < d2:
                                continue
                            P.mm(pO[oi][:, u4 * 65:(u4 + 1) * 65], PT2[pi][:, m, u4 * 128:(u4 + 1) * 128], V[ub][:, j, :],
                                 firsts[m], j == nkt - 1, r=[r_PT2[pi], r_V[ub]],
                                 **({"w": [r_pO[oi]]} if firsts[m] else {"pw": [r_pO[oi]]}))
                            firsts[m] = False
            oi0 = obanks[0]
            O0 = pO[oi0][:, 0:260].rearrange("p (u c) -> p u c", c=65)
            so = cnt["osl"] % 2
            cnt["osl"] += 1
            P.op("dve", lambda e, O0=O0: e.reciprocal(out=rz[:, 0, :], in_=O0[:, :, 64]), r=[r_pO[oi0]], w=[r_nrm])
            if U["kind"] != "D":
                for u4 in range(4):
                    P.ts("dve", osl[so][:, u4, :], O0[:, u4, 0:64], rz[:, 0, u4:u4 + 1], ALU.mult,
                         r=[r_pO[oi0], r_nrm], **({"w": [r_osl[so]]} if u4 == 0 else {"pw": [r_osl[so]]}))
            else:
                oi1 = obanks[1]
                O1 = pO[oi1][:, 0:260].rearrange("p (u c) -> p u c", c=65)
                P.op("dve", lambda e, O1=O1: e.reciprocal(out=rz[:, 1, :], in_=O1[:, :, 64]), r=[r_pO[oi1]], pw=[r_nrm])
                for u4 in range(4):
                    P.ts("dve", t1[:, u4, :], O0[:, u4, 0:64], rz[:, 0, u4:u4 + 1], ALU.mult,
                         r=[r_pO[oi0], r_nrm], pw=[r_nrm])
                    P.ts("dve", t2[:, u4, :], O1[:, u4, 0:64], rz[:, 1, u4:u4 + 1], ALU.mult, neg_lam[:, 0:1], ALU.mult,
                         r=[r_pO[oi1], r_nrm, r_lam], pw=[r_nrm])
                P.tt("dve", t1[:], t1[:], t2[:], ALU.add, r=[r_nrm], w=[r_nrm])
                P.tt("dve", sq[:], t1[:], t1[:], ALU.mult, r=[r_nrm], w=[r_nrm])
                P.op("dve", lambda e: e.reduce_sum(out=ssd[:], in_=sq[:], axis=AX.X), r=[r_nrm], w=[r_nrm])
                P.ts("dve", ssd[:], ssd[:], 1.0 / 64.0, ALU.mult, EPS, ALU.add, r=[r_nrm], w=[r_nrm])
                P.op("act", lambda e: e.sqrt(out=ssd[:], in_=ssd[:]), r=[r_nrm], w=[r_nrm])
                P.op("dve", lambda e: e.reciprocal(out=ssd[:], in_=ssd[:]), r=[r_nrm], w=[r_nrm])
                for u4 in range(4):
                    P.stt(osl[so][:, u4, :], t1[:, u4, :], ssd[:, u4:u4 + 1], gsub[:], ALU.mult, ALU.mult,
                          r=[r_nrm, r_c], **({"w": [r_osl[so]]} if u4 == 0 else {"pw": [r_osl[so]]}))
            P.dma("sp", o_d[:, 4 * p:4 * p + 4, U["col"]:U["col"] + 64], osl[so][:], r=[r_osl[so]], is_output=True)

    prep(0)
    for u in range(len(units)):
        if u + 1 < len(units):
            prep(u + 1)
        attention(u)
    P.emit()
    return nc


def assemble_kv(outA_pair, S):
    res = {}
    pos_of = {}
    for par in range(2):
        for i, g in enumerate(own_tiles(S, par)):
            pos_of[g] = (par, i)
    NKT = S // 128
    for name in ("kA", "vA", "kD", "vD", "kM", "vM"):
        a0, a1 = outA_pair[0][name], outA_pair[1][name]
        full = np.empty((a0.shape[0], 128, NKT, a0.shape[3]), dtype=a0.dtype)
        for g in range(NKT):
            par, i = pos_of[g]
            full[:, :, g, :] = (a0 if par == 0 else a1)[:, :, i, :]
        res[name] = full
    return res


_NC_CACHE = {}


def _get_nc(key, fn):
    if key not in _NC_CACHE:
        _NC_CACHE[key] = fn()
    return _NC_CACHE[key]


def _run(nc, in_maps):
    res = run_bass_kernel_spmd(nc, in_maps, core_ids=list(range(8)))
    return res.results


def launch_A(S, h_own, positions, W, l):
    nc = _get_nc(("A", S), lambda: build_A(S))
    in_maps = []
    for c in range(8):
        b, par = c // 2, c % 2
        rows = tok_rows(S, par)
        TT = len(rows) // 128
        m = dict(const_inputs_A(S, par))
        m["h"] = np.ascontiguousarray(h_own[c])
        m["pos"] = np.ascontiguousarray(positions[b][rows].reshape(TT, 128).T)
        m["pos0"] = np.ascontiguousarray(positions[b, 0:1].reshape(1, 1))
        m["attn_norm"] = W["attn_norm"][l].reshape(1, -1)
        m["w_in"] = W["w_in"][l]
        m["q_norm"] = W["mla_q_norm"][l].reshape(1, -1)
        m["w_uq"] = W["mla_w_uq"][l]
        m["kv_norm"] = W["mla_kv_norm"][l].reshape(1, -1)
        m["w_ukv"] = W["mla_w_ukv"][l]
        in_maps.append(m)
    return _run(nc, in_maps)


def launch_B(S, outA, W, l):
    lam_init = 0.8 - 0.6 * math.exp(-0.3 * l)
    nc = _get_nc(("B", S, l), lambda: build_B(S, lam_init))
    in_maps = []
    for b in range(4):
        kv = assemble_kv([outA[2 * b], outA[2 * b + 1]], S)
        for par in range(2):
            m = dict(const_inputs_B(S, par))
            m.update(kv)
            for nm in ("qA", "qD", "qM"):
                m[nm] = outA[2 * b + par][nm]
            m["lq1"] = W["diff_lambda_q1"][l].reshape(1, -1)
            m["lk1"] = W["diff_lambda_k1"][l].reshape(1, -1)
            m["lq2"] = W["diff_lambda_q2"][l].reshape(1, -1)
            m["lk2"] = W["diff_lambda_k2"][l].reshape(1, -1)
            m["sub_norm"] = W["diff_sub_norm"][l].reshape(1, -1)
            in_maps.append(m)
    return _run(nc, in_maps)


def transpose_tile(C, src_bf, dstT, col0, ident, pT, r_pT, r_src, r_dst, r_id, cnt, nchunk=8):
    P = C.P
    pi = cnt["pT"] % len(pT)
    cnt["pT"] += 1
    for kc in range(nchunk):
        P.tr(pT[pi][:, kc * 128:(kc + 1) * 128], src_bf[:, kc * 128:(kc + 1) * 128], ident[:],
             r=[r_src, r_id], pw=[r_pT[pi]])
    cnt["ev"] += 1
    P.cp("act" if cnt["ev"] % 2 else "dve", dstT[:, 0:nchunk, col0:col0 + 128],
         pT[pi][:, 0:nchunk * 128].rearrange("p (k c) -> p k c", k=nchunk), r=[r_pT[pi]], pw=[r_dst])


def build_C1(S):
    TO = S // 2
    TT = TO // 128
    NSLOT = TT // 4
    nc = bass.Bass("TRN2", target_bir_lowering=False)
    C = Ctx(nc)
    P = C.P
    h_d = C.dram("h", [TO, D], F32)
    o_d = C.dram("o", [128, TT, D], BF16)
    mem_d = C.dram("mem", [256, D], F32)
    ident_d = C.dram("ident", [128, 128], F32)
    w_out_d = C.dram("w_out", [D, D], F32)
    g_cross_d = C.dram("cross_norm", [1, D], F32)
    g_mem_d = C.dram("mem_norm", [1, D], F32)
    wq_d = C.dram("wq", [D, D], F32)
    wkv_d = C.dram("wkv", [D, 2 * D], F32)
    wo_d = C.dram("wo", [D, D], F32)
    hout_d = C.dram("h_out", [TO, D], F32, out=True)

    r_w = Res("w")
    ident = C.sb([128, 128], BF16)
    w_out = C.sb([128, 8, D], BF16)
    wq = C.sb([128, 8, D], BF16)
    wo = C.sb([128, 8, D], BF16)
    wkv = C.sb([128, 8, 2 * D], BF16)
    g_cross = C.sb([128, D], F32)
    g_mem = C.sb([128, D], F32)
    P.dma("pool", ident[:], ident_d, pw=[r_w])
    P.dma("sp", g_cross[:], g_cross_d[0, :].partition_broadcast(128), pw=[r_w])
    P.dma("sp", g_mem[:], g_mem_d[0, :].partition_broadcast(128), pw=[r_w])
    load_w_bf16(C, wkv, wkv_d, D, 2 * D, r_w)
    load_w_bf16(C, w_out, w_out_d, D, D, r_w)
    load_w_bf16(C, wq, wq_d, D, D, r_w)
    load_w_bf16(C, wo, wo_d, D, D, r_w)

    hb = [C.sb([128, D], F32) for _ in range(4)]
    r_hb = [Res("hb%d" % i) for i in range(4)]
    nbf = [C.sb([128, D], BF16) for _ in range(2)]
    r_nbf = [Res("nbf%d" % i) for i in range(2)]
    xT = [C.sb([128, 8, 512], BF16) for _ in range(2)]
    r_xT = [Res("xT%d" % i) for i in range(2)]
    osb = C.sb([128, 4, D], BF16)
    r_osb = Res("osb")
    cqT = C.sb([128, 8, 512], BF16)
    r_cqT = Res("cqT")
    oc = C.sb([128, 4, D], BF16)
    r_oc = Res("oc")
    PTc = [C.sb([128, 512], BF16) for _ in range(2)]
    r_PTc = [Res("PTc%d" % i) for i in range(2)]
    kmemT = C.sb([128, 8, 256], BF16)
    vmem = C.sb([128, 2, 4, 257], BF16)
    r_kv = Res("memkv")
    rzc = C.sb([128, 1], F32)
    r_rz = Res("rzc")
    scr = {"junk": C.sb([128, D], BF16), "ss": C.sb([128, 1], F32), "rstd": C.sb([128, 1], F32), "res": Res("scr")}
    pT = [C.ps([128, D], BF16) for _ in range(2)]
    r_pT = [Res("pT%d" % i, psum=True) for i in range(2)]
    pP = [C.ps([128, 512], F32) for _ in range(2)]
    r_pP = [Res("pP%d" % i, psum=True) for i in range(2)]
    pS = [C.ps([128, 512], F32) for _ in range(2)]
    r_pS = [Res("pS%d" % i, psum=True) for i in range(2)]
    pO = [C.ps([128, 512], F32) for _ in range(2)]
    r_pO = [Res("pO%d" % i, psum=True) for i in range(2)]
    cnt = {"pT": 0, "ev": 0, "pP": 0, "pS": 0, "pO": 0, "n": 0, "x": 0}

    def evac(out, in_, r, w=(), pw=()):
        cnt["ev"] += 1
        P.cp("act" if cnt["ev"] % 2 else "dve", out, in_, r=r, w=w, pw=pw)

    memT = xT[1]
    for mt in range(2):
        hi = mt
        P.dma("sp", hb[hi][:], mem_d[mt * 128:(mt + 1) * 128, :], w=[r_hb[hi]])
        ni = cnt["n"] % 2
        cnt["n"] += 1
        rmsnorm_tile(C, hb[hi][:], g_mem[:], nbf[ni][:], D, scr, r_hb[hi], r_w, r_nbf[ni], "m")
        transpose_tile(C, nbf[ni], memT, mt * 128, ident, pT, r_pT, r_nbf[ni], r_xT[1], r_w, cnt)
    for fc in range(8):
        pi = cnt["pP"] % 2
        cnt["pP"] += 1
        for kc in range(8):
            P.mm(pP[pi][:, 0:256], wkv[:, kc, fc * 128:(fc + 1) * 128], memT[:, kc, 0:256], kc == 0, kc == 7,
                 r=[r_w, r_xT[1]], w=[r_pP[pi]])
        evac(kmemT[:, fc, :], pP[pi][:, 0:256], r=[r_pP[pi]], pw=[r_kv])
    P.memset("pool", vmem[:, :, :, 256], 1.0, pw=[r_kv])
    for mt in range(2):
        for half in range(2):
            pi = cnt["pP"] % 2
            cnt["pP"] += 1
            for kc in range(8):
                P.mm(pP[pi][:, :], memT[:, kc, mt * 128:(mt + 1) * 128], wkv[:, kc, D + half * 512:D + (half + 1) * 512],
                     kc == 0, kc == 7, r=[r_w, r_xT[1]], w=[r_pP[pi]])
            evac(vmem[:, mt, 2 * half:2 * half + 2, 0:256], pP[pi][:, :].rearrange("p (h c) -> p h c", h=2),
                 r=[r_pP[pi]], pw=[r_kv])

    def proj_add(srcT, r_srcT, w_sb, tt):
        for half in range(2):
            pi = cnt["pP"] % 2
            cnt["pP"] += 1
            for kc in range(8):
                P.mm(pP[pi][:, :], srcT[:, kc, tt * 128:(tt + 1) * 128], w_sb[:, kc, half * 512:(half + 1) * 512],
                     kc == 0, kc == 7, r=[r_srcT, r_w], w=[r_pP[pi]])
            P.tt("dve", hb[tt][:, half * 512:(half + 1) * 512], hb[tt][:, half * 512:(half + 1) * 512], pP[pi][:, :],
                 ALU.add, r=[r_pP[pi]], w=[r_hb[tt]])

    for p in range(NSLOT):
        P.dma("sp", osb[:], o_d[:, 4 * p:4 * p + 4, :], w=[r_osb])
        for tt in range(4):
            t = 4 * p + tt
            P.dma("sp", hb[tt][:], h_d[t * 128:(t + 1) * 128, :], w=[r_hb[tt]])
        xa = cnt["x"] % 2
        cnt["x"] += 1
        for tt in range(4):
            transpose_tile(C, osb[:, tt, :], xT[xa], tt * 128, ident, pT, r_pT, r_osb, r_xT[xa], r_w, cnt)
        for tt in range(4):
            proj_add(xT[xa], r_xT[xa], w_out, tt)
        xb = cnt["x"] % 2
        cnt["x"] += 1
        for tt in range(4):
            ni = cnt["n"] % 2
            cnt["n"] += 1
            rmsnorm_tile(C, hb[tt][:], g_cross[:], nbf[ni][:], D, scr, r_hb[tt], r_w, r_nbf[ni], "c")
            transpose_tile(C, nbf[ni], xT[xb], tt * 128, ident, pT, r_pT, r_nbf[ni], r_xT[xb], r_w, cnt)
        for fc in range(8):
            pi = cnt["pP"] % 2
            cnt["pP"] += 1
            for kc in range(8):
                P.mm(pP[pi][:, :], wq[:, kc, fc * 128:(fc + 1) * 128], xT[xb][:, kc, :], kc == 0, kc == 7,
                     r=[r_w, r_xT[xb]], w=[r_pP[pi]])
            evac(cqT[:, fc, :], pP[pi][:, :], r=[r_pP[pi]], pw=[r_cqT])
        for hh in range(4):
            for mt in range(2):
                si = cnt["pS"] % 2
                cnt["pS"] += 1
                for dc in range(2):
                    P.mm(pS[si][:, :], kmemT[:, hh * 2 + dc, mt * 128:(mt + 1) * 128], cqT[:, hh * 2 + dc, :],
                         dc == 0, dc == 1, r=[r_kv, r_cqT], w=[r_pS[si]])
                P.act(PTc[mt][:], pS[si][:, :], AF.Exp, scale=1.0 / 16.0, r=[r_pS[si]], w=[r_PTc[mt]])
            for tt in range(4):
                oi = cnt["pO"] % 2
                cnt["pO"] += 1
                for mt in range(2):
                    P.mm(pO[oi][:, 0:257], PTc[mt][:, tt * 128:(tt + 1) * 128], vmem[:, mt, hh, :], mt == 0, mt == 1,
                         r=[r_PTc[mt], r_kv], w=[r_pO[oi]])
                P.op("dve", lambda e, oi=oi: e.reciprocal(out=rzc[:], in_=pO[oi][:, 256:257]), r=[r_pO[oi]], w=[r_rz])
                P.ts("dve", oc[:, tt, hh * 256:(hh + 1) * 256], pO[oi][:, 0:256], rzc[:, 0:1], ALU.mult,
                     r=[r_pO[oi], r_rz], pw=[r_oc])
        xc = cnt["x"] % 2
        cnt["x"] += 1
        for tt in range(4):
            transpose_tile(C, oc[:, tt, :], xT[xc], tt * 128, ident, pT, r_pT, r_oc, r_xT[xc], r_w, cnt)
        for tt in range(4):
            t = 4 * p + tt
            proj_add(xT[xc], r_xT[xc], wo, tt)
            P.dma("sp", hout_d[t * 128:(t + 1) * 128, :], hb[tt][:], r=[r_hb[tt]], is_output=True)
    P.emit()
    return nc


def build_C2(S, final):
    TO = S // 2
    TT = TO // 128
    nc = bass.Bass("TRN2", target_bir_lowering=False)
    C = Ctx(nc)
    P = C.P
    h_d = C.dram("h", [TO, D], F32)
    ident_d = C.dram("ident", [128, 128], F32)
    g_mlp_d = C.dram("mlp_norm", [1, D], F32)
    w1_d = C.dram("w1", [D, 4 * D], F32)
    w2_d = C.dram("w2", [4 * D, D], F32)
    g_fin_d = C.dram("final_norm", [1, D], F32)
    hout_d = C.dram("h_out", [TO, D], F32, out=True)
    r_w = Res("w")
    ident = C.sb([128, 128], BF16)
    w1 = C.sb([128, 8, 4 * D], BF16)
    w2 = C.sb([128, 32, D], BF16)
    g_mlp = C.sb([128, D], F32)
    g_fin = C.sb([128, D], F32)
    P.dma("pool", ident[:], ident_d, pw=[r_w])
    P.dma("sp", g_mlp[:], g_mlp_d[0, :].partition_broadcast(128), pw=[r_w])
    P.dma("sp", g_fin[:], g_fin_d[0, :].partition_broadcast(128), pw=[r_w])
    load_w_bf16(C, w1, w1_d, D, 4 * D, r_w)
    load_w_bf16(C, w2, w2_d, 4 * D, D, r_w)
    NT = 2
    W = NT * 128
    hb = [C.sb([128, D], F32) for _ in range(2 * NT)]
    r_hb = [Res("hb%d" % i) for i in range(2 * NT)]
    nbf = [C.sb([128, D], BF16) for _ in range(2)]
    r_nbf = [Res("nbf%d" % i) for i in range(2)]
    xT = [C.sb([128, 8, W], BF16) for _ in range(2)]
    r_xT = [Res("xT%d" % i) for i in range(2)]
    hidT = C.sb([128, 32, W], BF16)
    r_hid = Res("hidT")
    rl = [C.sb([128, W], F32) for _ in range(2)]
    r_rl = [Res("rl%d" % i) for i in range(2)]
    fo = [C.sb([128, D], F32) for _ in range(2)]
    r_fo = [Res("fo%d" % i) for i in range(2)]
    scr = {"junk": C.sb([128, D], BF16), "ss": C.sb([128, 1], F32), "rstd": C.sb([128, 1], F32), "res": Res("scr")}
    pT = [C.ps([128, D], BF16) for _ in range(2)]
    r_pT = [Res("pT%d" % i, psum=True) for i in range(2)]
    pH = [C.ps([128, 512], F32) for _ in range(3)]
    r_pH = [Res("pH%d" % i, psum=True) for i in range(3)]
    pP = [C.ps([128, 512], F32) for _ in range(3)]
    r_pP = [Res("pP%d" % i, psum=True) for i in range(3)]
    cnt = {"pT": 0, "ev": 0, "pP": 0, "pH": 0, "n": 0, "x": 0, "rl": 0, "hb": 0, "fo": 0}
    for g in range(TT // NT):
        hs = []
        xa = cnt["x"] % 2
        cnt["x"] += 1
        for tt in range(NT):
            t = g * NT + tt
            hi = cnt["hb"] % (2 * NT)
            cnt["hb"] += 1
            hs.append(hi)
            P.dma("sp", hb[hi][:], h_d[t * 128:(t + 1) * 128, :], w=[r_hb[hi]])
            ni = cnt["n"] % 2
            cnt["n"] += 1
            rmsnorm_tile(C, hb[hi][:], g_mlp[:], nbf[ni][:], D, scr, r_hb[hi], r_w, r_nbf[ni], "m")
            transpose_tile(C, nbf[ni], xT[xa], tt * 128, ident, pT, r_pT, r_nbf[ni], r_xT[xa], r_w, cnt)
        for fc in range(32):
            pi = cnt["pH"] % 3
            cnt["pH"] += 1
            for kc in range(8):
                P.mm(pH[pi][:, 0:W], w1[:, kc, fc * 128:(fc + 1) * 128], xT[xa][:, kc, :], kc == 0, kc == 7,
                     r=[r_w, r_xT[xa]], w=[r_pH[pi]])
            ri = cnt["rl"] % 2
            cnt["rl"] += 1
            P.act(rl[ri][:], pH[pi][:, 0:W], AF.Relu, r=[r_pH[pi]], w=[r_rl[ri]])
            P.tt("pool", hidT[:, fc, :], rl[ri][:], rl[ri][:], ALU.mult, r=[r_rl[ri]], pw=[r_hid])
        for tt in range(NT):
            t = g * NT + tt
            hi = hs[tt]
            for half in range(2):
                pi = cnt["pP"] % 3
                cnt["pP"] += 1
                for fc in range(32):
                    P.mm(pP[pi][:, :], hidT[:, fc, tt * 128:(tt + 1) * 128], w2[:, fc, half * 512:(half + 1) * 512],
                         fc == 0, fc == 31, r=[r_hid, r_w], w=[r_pP[pi]])
                P.tt("dve", hb[hi][:, half * 512:(half + 1) * 512], hb[hi][:, half * 512:(half + 1) * 512], pP[pi][:, :],
                     ALU.add, r=[r_pP[pi]], w=[r_hb[hi]])
            if final:
                fi = cnt["fo"] % 2
                cnt["fo"] += 1
                rmsnorm_tile(C, hb[hi][:], g_fin[:], fo[fi][:], D, scr, r_hb[hi], r_w, r_fo[fi], "f")
                P.dma("sp", hout_d[t * 128:(t + 1) * 128, :], fo[fi][:], r=[r_fo[fi]], is_output=True)
            else:
                P.dma("sp", hout_d[t * 128:(t + 1) * 128, :], hb[hi][:], r=[r_hb[hi]], is_output=True)
    P.emit()
    return nc


def launch_C1(S, h_own, outB, mem, W, l):
    nc = _get_nc(("C1", S), lambda: build_C1(S))
    in_maps = []
    for c in range(8):
        b = c // 2
        m = {"h": np.ascontiguousarray(h_own[c]), "o": outB[c]["o"], "mem": np.ascontiguousarray(mem[b]),
             "ident": np.eye(128, dtype=np.float32), "w_out": W["w_out"][l],
             "cross_norm": W["cross_norm"][l].reshape(1, -1), "mem_norm": W["mem_norm"][l].reshape(1, -1),
             "wq": W["cross_wq"][l], "wkv": W["cross_wkv"][l], "wo": W["cross_wo"][l]}
        in_maps.append(m)
    return [r["h_out"] for r in _run(nc, in_maps)]


def launch_C2(S, h_own, W, l, final):
    nc = _get_nc(("C2", S, final), lambda: build_C2(S, final))
    in_maps = []
    for c in range(8):
        m = {"h": np.ascontiguousarray(h_own[c]), "ident": np.eye(128, dtype=np.float32),
             "mlp_norm": W["mlp_norm"][l].reshape(1, -1), "w1": W["mlp_w1"][l], "w2": W["mlp_w2"][l],
             "final_norm": W["final_norm"].reshape(1, -1)}
        in_maps.append(m)
    return [r["h_out"] for r in _run(nc, in_maps)]


def forward(S, inputs, depth=2):
    x = np.asarray(inputs["x"])
    positions = np.asarray(inputs["positions"])
    mem = np.asarray(inputs["mem"])
    W = {k: np.asarray(v) for k, v in inputs.items()}
    h_own = [np.ascontiguousarray(x[c // 2][tok_rows(S, c % 2)]) for c in range(8)]
    for l in range(depth):
        outA = launch_A(S, h_own, positions, W, l)
        outB = launch_B(S, outA, W, l)
        h_own = launch_C1(S, h_own, outB, mem, W, l)
        h_own = launch_C2(S, h_own, W, l, final=(l == depth - 1))
    out = np.empty((4, S, D), np.float32)
    for c in range(8):
        out[c // 2][tok_rows(S, c % 2)] = h_own[c]
    return out


def kernel(**inputs):
    return forward(8192, inputs)
```

```python
import math
from contextlib import ExitStack

import numpy as np
import ml_dtypes
import concourse.bass as bass
import concourse.mybir as mybir
from concourse.bass_utils import run_bass_kernel_spmd

F32 = mybir.dt.float32
BF16 = mybir.dt.bfloat16
I32 = mybir.dt.int32
AF = mybir.ActivationFunctionType
ALU = mybir.AluOpType
AX = mybir.AxisListType
NPBF = ml_dtypes.bfloat16

D = 1024
DIN = 2336
EPS = 1e-6
NEGM = -30000.0
N_ALIBI = 10
SLOPES = [2.0 ** (-8.0 * (h + 1) / N_ALIBI) for h in range(N_ALIBI)]
KA_A, KA_D, KA_M = 96, 40, 104

ENGS = ("pe", "act", "dve", "pool", "sp")
SAME_ENGINE_SYNC = True


class Res:
    __slots__ = ("name", "writers", "readers", "prev_readers", "psum")

    def __init__(self, name="", psum=False):
        self.name = name
        self.psum = psum
        self.writers = []
        self.readers = []
        self.prev_readers = []


def _push(lst, o):
    if not o.is_dma:
        for i, x in enumerate(lst):
            if (not x.is_dma) and x.eng == o.eng:
                lst[i] = o
                return
    lst.append(o)


class Op:
    __slots__ = ("eng", "fn", "deps", "is_dma", "sem", "val", "needed", "prev_val")

    def __init__(self, eng, fn, is_dma):
        self.eng = eng
        self.fn = fn
        self.deps = []
        self.is_dma = is_dma
        self.sem = None
        self.val = None
        self.needed = False
        self.prev_val = 0


class Prog:
    def __init__(self, nc, n_dma_sems=48):
        self.nc = nc
        self.ops = []
        self.n_dma_sems = n_dma_sems
        self.out_dmas = []
        self.rr = 0

    def _add(self, eng, fn, r, w, is_dma, pw=()):
        o = Op(eng, fn, is_dma)
        deps = []
        for x in r:
            deps.extend(x.writers)
            if x.psum:
                deps.extend(d for d in x.readers if d.eng != eng)
        for x in w:
            deps.extend(x.writers)
            deps.extend(x.readers)
            deps.extend(x.prev_readers)
        for x in pw:
            if x.readers:
                x.prev_readers = x.readers
                x.readers = []
                x.writers = []
            deps.extend(x.prev_readers)
        seen = set()
        for d in deps:
            if id(d) in seen:
                continue
            seen.add(id(d))
            if (not d.is_dma) and (not is_dma) and d.eng == eng:
                if eng == "pe" or not SAME_ENGINE_SYNC:
                    continue
            o.deps.append(d)
        for x in r:
            _push(x.readers, o)
        for x in w:
            x.writers = [o]
            x.readers = []
            x.prev_readers = []
        for x in pw:
            _push(x.writers, o)
        self.ops.append(o)
        return o

    def op(self, eng, fn, r=(), w=(), pw=()):
        return self._add(eng, fn, r, w, False, pw)

    def dma(self, queue, out, in_, r=(), w=(), is_output=False, pw=()):
        o = self._add(queue, lambda e: e.dma_start(out=out, in_=in_), r, w, True, pw)
        if is_output:
            self.out_dmas.append(o)
        return o

    def mm(self, out, lhsT, rhs, start, stop, r=(), w=(), pw=()):
        return self.op("pe", lambda e: e.matmul(out, lhsT=lhsT, rhs=rhs, start=start, stop=stop,
                                                skip_group_check=True), r, w, pw)

    def tr(self, out, in_, ident, r=(), w=(), pw=()):
        return self.op("pe", lambda e: e.transpose(out, in_, ident), r, w, pw)

    def act(self, out, in_, func, scale=1.0, bias=0.0, accum_out=None, r=(), w=(), pw=()):
        return self.op("act", lambda e: e.activation(out=out, in_=in_, func=func, bias=bias, scale=scale,
                                                     accum_out=accum_out), r, w, pw)

    def cp(self, eng, out, in_, r=(), w=(), pw=()):
        if eng == "act":
            return self.op("act", lambda e: e.copy(out=out, in_=in_), r, w, pw)
        return self.op(eng, lambda e: e.tensor_copy(out=out, in_=in_), r, w, pw)

    def tt(self, eng, out, in0, in1, op, r=(), w=(), pw=()):
        return self.op(eng, lambda e: e.tensor_tensor(out=out, in0=in0, in1=in1, op=op), r, w, pw)

    def ts(self, eng, out, in0, s1, op0, s2=None, op1=None, r=(), w=(), pw=()):
        if op1 is None:
            return self.op(eng, lambda e: e.tensor_scalar(out=out, in0=in0, scalar1=s1, scalar2=None, op0=op0), r, w, pw)
        return self.op(eng, lambda e: e.tensor_scalar(out=out, in0=in0, scalar1=s1, scalar2=s2, op0=op0, op1=op1), r, w, pw)

    def stt(self, out, in0, scalar, in1, op0, op1, r=(), w=(), pw=()):
        return self.op("dve", lambda e: e.scalar_tensor_tensor(out=out, in0=in0, scalar=scalar, in1=in1,
                                                               op0=op0, op1=op1), r, w, pw)

    def memset(self, eng, ap, val, w=(), pw=()):
        return self.op(eng, lambda e: e.memset(ap, val), (), w, pw)

    def emit(self):
        nc = self.nc
        ops = self.ops
        for o in ops:
            for d in o.deps:
                d.needed = True
        with ExitStack() as es:
            eng_sem = {e: es.enter_context(nc.semaphore("s_" + e)) for e in ENGS}
            n_hw, n_sw = self.n_dma_sems, 16
            dma_sems = [es.enter_context(nc.semaphore("d%d" % i)) for i in range(n_hw + n_sw)]
            dma_tot = [0] * (n_hw + n_sw)
            cnt = {e: 0 for e in ENGS}
            k_hw = k_sw = 0
            for o in ops:
                if o.is_dma:
                    if o.eng == "pool":
                        si = n_hw + (k_sw % n_sw)
                        k_sw += 1
                    else:
                        si = k_hw % n_hw
                        k_hw += 1
                    o.sem = dma_sems[si]
                    o.prev_val = dma_tot[si]
                    dma_tot[si] += 16
                    o.val = dma_tot[si]
                elif o.needed:
                    cnt[o.eng] += 1
                    o.sem = eng_sem[o.eng]
                    o.val = cnt[o.eng]
            per = {e: [o for o in ops if o.eng == e] for e in ENGS}
            final_waits = [(s, v) for s, v in zip(dma_sems, dma_tot) if v > 0]

            def replay(ename, eobj, extra_final=False):
                known = {}
                for o in per[ename]:
                    waits = [(d.sem, d.val) for d in o.deps]
                    if o.is_dma and o.prev_val > 0:
                        waits.append((o.sem, o.prev_val))
                    for (s, v) in waits:
                        key = id(s)
                        if known.get(key, 0) >= v:
                            continue
                        eobj.wait_ge(s, v)
                        known[key] = v
                    ins = o.fn(eobj)
                    if o.is_dma:
                        ins.then_inc(o.sem, 16)
                    elif o.needed:
                        ins.then_inc(o.sem, 1)
                if extra_final:
                    for (s, v) in final_waits:
                        if known.get(id(s), 0) >= v:
                            continue
                        eobj.wait_ge(s, v)
                        known[id(s)] = v

            with nc.Block() as block:
                @block.sync
                def _(e):
                    replay("sp", e, extra_final=True)

                @block.scalar
                def _(e):
                    replay("act", e)

                @block.vector
                def _(e):
                    replay("dve", e)

                @block.gpsimd
                def _(e):
                    replay("pool", e)

                @block.tensor
                def _(e):
                    replay("pe", e)


class Ctx:
    def __init__(self, nc):
        self.nc = nc
        self.P = Prog(nc)
        self.n = 0

    def dram(self, name, shape, dt, out=False):
        return self.nc.dram_tensor(name, list(shape), dt, kind="ExternalOutput" if out else "ExternalInput").ap()

    def sb(self, shape, dt, name=None):
        self.n += 1
        return self.nc.alloc_sbuf_tensor(name or ("t%d" % self.n), list(shape), dt)

    def ps(self, shape, dt, name=None):
        self.n += 1
        return self.nc.alloc_psum_tensor(name or ("p%d" % self.n), list(shape), dt)


def split_bf16_2(x):
    a = np.float32(np.asarray(x, np.float32).astype(NPBF).astype(np.float32))
    b = np.float32(np.asarray(np.float32(x) - a, np.float32).astype(NPBF).astype(np.float32))
    return float(a), float(b)


def load_w_bf16(C, dst, src, rows, cols, r_w, queue="pool"):
    kc = rows // 128
    step = 1024
    for k in range(kc):
        for c0 in range(0, cols, step):
            c1 = min(cols, c0 + step)
            if kc == 1 and len(dst.shape) == 2:
                o = dst[:, c0:c1]
            else:
                o = dst[:, k, c0:c1]
            C.P.dma(queue, o, src[k * 128:(k + 1) * 128, c0:c1], pw=[r_w])


def rmsnorm_tile(C, x_ap, g_ap, out_ap, width, scr, rx, rg, ro, tag):
    P = C.P
    junk, ss, rstd = scr["junk"], scr["ss"], scr["rstd"]
    rs = scr["res"]
    P.act(junk[:, 0:width], x_ap, AF.Square, accum_out=ss[:, 0:1], r=[rx], w=[rs])
    P.ts("dve", rstd[:, 0:1], ss[:, 0:1], 1.0 / width, ALU.mult, EPS, ALU.add, r=[rs], w=[rs])
    P.op("act", lambda e: e.sqrt(out=rstd[:, 0:1], in_=rstd[:, 0:1]), r=[rs], w=[rs])
    P.op("dve", lambda e: e.reciprocal(out=rstd[:, 0:1], in_=rstd[:, 0:1]), r=[rs], w=[rs])
    P.stt(out_ap, x_ap, rstd[:, 0:1], g_ap, ALU.mult, ALU.mult, r=[rx, rg, rs], w=[ro])


DBG_STOP = 99


def build_A(S):
    TO = S // 2
    TT = TO // 128
    NSLOT = TT // 4
    nc = bass.Bass("TRN2", target_bir_lowering=False)
    C = Ctx(nc)
    P = C.P
    h_in = C.dram("h", [TO, D], F32)
    pos_d = C.dram("pos", [128, TT], I32)
    pos0_d = C.dram("pos0", [1, 1], I32)
    onehot_d = C.dram("onehot", [128, TT, 32], BF16)
    invf_d = C.dram("invf", [1, 16], F32)
    ident_d = C.dram("ident", [128, 128], F32)
    g_attn_d = C.dram("attn_norm", [1, D], F32)
    w_in_d = C.dram("w_in", [D, DIN], F32)
    g_q_d = C.dram("q_norm", [1, 256], F32)
    w_uq_d = C.dram("w_uq", [256, 576], F32)
    g_kv_d = C.dram("kv_norm", [1, 128], F32)
    w_ukv_d = C.dram("w_ukv", [128, 768], F32)
    kA_d = C.dram("kA", [6, 128, TT, KA_A], BF16, out=True)
    qA_d = C.dram("qA", [6, 128, TT, KA_A], BF16, out=True)
    vA_d = C.dram("vA", [6, 128, TT, 65], BF16, out=True)
    kD_d = C.dram("kD", [12, 128, TT, KA_D], BF16, out=True)
    qD_d = C.dram("qD", [12, 128, TT, KA_D], BF16, out=True)
    vD_d = C.dram("vD", [6, 128, TT, 65], BF16, out=True)
    kM_d = C.dram("kM", [4, 128, TT, KA_M], BF16, out=True)
    qM_d = C.dram("qM", [4, 128, TT, KA_M], BF16, out=True)
    vM_d = C.dram("vM", [4, 128, TT, 65], BF16, out=True)

    w_in = C.sb([128, 8, DIN], BF16)
    w_uq = C.sb([128, 2, 576], BF16)
    w_ukv = C.sb([128, 768], BF16)
    ident = C.sb([128, 128], BF16)
    g_attn = C.sb([128, D], F32)
    g_q = C.sb([128, 256], F32)
    g_kv = C.sb([128, 128], F32)
    r_w = Res("weights")
    P.dma("pool", ident[:], ident_d, pw=[r_w])
    P.dma("sp", g_attn[:], g_attn_d[0, :].partition_broadcast(128), pw=[r_w])
    P.dma("sp", g_q[:], g_q_d[0, :].partition_broadcast(128), pw=[r_w])
    P.dma("sp", g_kv[:], g_kv_d[0, :].partition_broadcast(128), pw=[r_w])
    load_w_bf16(C, w_in, w_in_d, D, DIN, r_w)
    load_w_bf16(C, w_uq, w_uq_d, 256, 576, r_w)
    load_w_bf16(C, w_ukv, w_ukv_d, 128, 768, r_w)

    r_pos = Res("pos")
    pos_i = C.sb([128, TT], I32)
    pos0_i = C.sb([128, 1], I32)
    pos_f = C.sb([128, TT], F32)
    pos0_f = C.sb([128, 1], F32)
    prel_f = C.sb([128, TT], F32)
    prel_i = C.sb([128, TT], I32)
    hi_i = C.sb([128, TT], I32)
    lo_i = C.sb([128, TT], I32)
    hl = C.sb([128, 2, TT], F32)
    invf = C.sb([128, 16], F32)
    ang = C.sb([128, TT, 16], F32)
    kq = C.sb([128, TT, 16], F32)
    kq_i = C.sb([128, TT, 16], I32)
    cos_t = C.sb([128, TT, 16], F32)
    sin_t = C.sb([128, TT, 16], F32)
    P.dma("sp", pos_i[:], pos_d, w=[r_pos])
    P.dma("sp", pos0_i[:], pos0_d[0, :].partition_broadcast(128), w=[r_pos])
    P.dma("sp", invf[:], invf_d[0, :].partition_broadcast(128), w=[r_pos])
    P.cp("dve", pos_f[:], pos_i[:], r=[r_pos], w=[r_pos])
    P.cp("dve", pos0_f[:], pos0_i[:], r=[r_pos], w=[r_pos])
    P.ts("dve", prel_f[:], pos_f[:], pos0_f[:, 0:1], ALU.subtract, r=[r_pos], w=[r_pos])
    P.cp("dve", prel_i[:], prel_f[:], r=[r_pos], w=[r_pos])
    P.op("dve", lambda e: e.tensor_single_scalar(out=hi_i[:], in_=prel_i[:], scalar=7, op=ALU.arith_shift_right),
         r=[r_pos], w=[r_pos])
    P.op("dve", lambda e: e.tensor_single_scalar(out=lo_i[:], in_=prel_i[:], scalar=127, op=ALU.bitwise_and),
         r=[r_pos], w=[r_pos])
    P.cp("dve", hl[:, 0, :], hi_i[:], r=[r_pos], w=[r_pos])
    P.cp("dve", hl[:, 1, :], lo_i[:], r=[r_pos], w=[r_pos])
    TWO_PI = 2.0 * math.pi
    c1 = float(np.float32(TWO_PI))
    c2 = float(TWO_PI - np.float64(np.float32(TWO_PI)))
    for t in range(TT):
        P.ts("dve", ang[:, t, :], invf[:], pos_f[:, t:t + 1], ALU.mult, r=[r_pos], w=[r_pos])
    P.ts("dve", kq[:], ang[:], 1.0 / TWO_PI, ALU.mult, r=[r_pos], w=[r_pos])
    P.cp("dve", kq_i[:], kq[:], r=[r_pos], w=[r_pos])
    P.cp("dve", kq[:], kq_i[:], r=[r_pos], w=[r_pos])
    P.stt(ang[:], kq[:], -c1, ang[:], ALU.mult, ALU.add, r=[r_pos], w=[r_pos])
    P.stt(ang[:], kq[:], -c2, ang[:], ALU.mult, ALU.add, r=[r_pos], w=[r_pos])
    P.ts("dve", ang[:], ang[:], math.pi, ALU.min, -math.pi, ALU.max, r=[r_pos], w=[r_pos])
    P.act(sin_t[:], ang[:], AF.Sin, r=[r_pos], w=[r_pos])
    P.stt(kq[:], ang[:], -1.0, ang[:], ALU.mult, ALU.max, r=[r_pos], w=[r_pos])
    P.ts("dve", kq[:], kq[:], -1.0, ALU.mult, math.pi / 2, ALU.add, r=[r_pos], w=[r_pos])
    P.act(cos_t[:], kq[:], AF.Sin, r=[r_pos], w=[r_pos])
    augK = C.sb([128, N_ALIBI, TT, 8], BF16)
    augQ = C.sb([128, N_ALIBI, TT, 8], BF16)
    r_aug = Res("aug")
    for hh in range(N_ALIBI):
        d_h = 32 if hh < 6 else 64
        cc = SLOPES[hh] / (d_h ** -0.5)
        a1, a2 = split_bf16_2(cc)
        for j, v in enumerate([-128 * a1, -128 * a2, -a1, -a2]):
            P.memset("pool", augK[:, hh, :, 4 + j], v, pw=[r_aug])
        for j, v in enumerate([128 * a1, 128 * a2, a1, a2]):
            P.memset("pool", augQ[:, hh, :, j], v, pw=[r_aug])
        for j in range(4):
            P.cp("dve", augK[:, hh, :, j], hl[:, j // 2, :], r=[r_pos], pw=[r_aug])
            P.cp("dve", augQ[:, hh, :, 4 + j], hl[:, j // 2, :], r=[r_pos], pw=[r_aug])
    onehot = C.sb([128, TT, 32], BF16)
    P.dma("sp", onehot[:], onehot_d, pw=[r_aug])

    if DBG_STOP == 0:
        P.emit()
        return nc
    NB = 2
    hbuf = [C.sb([128, D], F32) for _ in range(3)]
    r_h = [Res("h%d" % i) for i in range(3)]
    nbf = [C.sb([128, D], BF16) for _ in range(2)]
    r_n = [Res("n%d" % i) for i in range(2)]
    nT = [C.sb([128, 8, 512], BF16) for _ in range(NB)]
    r_nT = [Res("nT%d" % i) for i in range(NB)]
    scr = {"junk": C.sb([128, D], BF16), "ss": C.sb([128, 1], F32), "rstd": C.sb([128, 1], F32), "res": Res("scr")}
    scr2 = {"junk": C.sb([128, 256], BF16), "ss": C.sb([128, 1], F32), "rstd": C.sb([128, 1], F32), "res": Res("scr2")}
    cqn = [C.sb([128, 384], BF16) for _ in range(2)]
    r_cqn = [Res("cqn%d" % i) for i in range(2)]
    cT = [C.sb([128, 3, 512], BF16) for _ in range(NB)]
    r_cT = [Res("cT%d" % i) for i in range(NB)]
    ropet = [C.sb([128, 6, 16], F32) for _ in range(4)]
    r_rope = [Res("ropetmp%d" % i) for i in range(6)]
    krt = C.sb([128, 32], BF16)
    r_kr = Res("krt")
    stKA = [C.sb([128, 6, 4, KA_A], BF16) for _ in range(NB)]
    stQA = [C.sb([128, 6, 4, KA_A], BF16) for _ in range(NB)]
    stVA = [C.sb([128, 6, 4, 65], BF16) for _ in range(NB)]
    stKD = [C.sb([128, 12, 4, KA_D], BF16) for _ in range(NB)]
    stQD = [C.sb([128, 12, 4, KA_D], BF16) for _ in range(NB)]
    stVD = [C.sb([128, 6, 4, 65], BF16) for _ in range(NB)]
    stKM = [C.sb([128, 4, 4, KA_M], BF16) for _ in range(NB)]
    stQM = [C.sb([128, 4, 4, KA_M], BF16) for _ in range(NB)]
    stVM = [C.sb([128, 4, 4, 65], BF16) for _ in range(NB)]
    r_st = [Res("st%d" % i) for i in range(NB)]
    for b in range(NB):
        for v in (stVA[b], stVD[b], stVM[b]):
            P.memset("pool", v[:, :, :, 64], 1.0, pw=[r_st[b]])
        P.memset("pool", stQM[b][:, :, :, 64:96], 0.0, pw=[r_st[b]])
    ptr = [C.ps([128, D], BF16) for _ in range(2)]
    r_ptr = [Res("ptr%d" % i, psum=True) for i in range(2)]
    pp = [C.ps([128, 512], F32) for _ in range(4)]
    r_pp = [Res("pp%d" % i, psum=True) for i in range(4)]
    pq = [C.ps([128, 512], F32) for _ in range(2)]
    r_pq = [Res("pq%d" % i, psum=True) for i in range(2)]
    cnt = {"h": 0, "n": 0, "ptr": 0, "pp": 0, "pq": 0, "ev": 0}

    def evac(out, in_, r, w=(), pw=()):
        cnt["ev"] += 1
        P.cp("act" if cnt["ev"] % 2 else "dve", out, in_, r=r, w=w, pw=pw)

    chunks = [(0, 416), (416, 800), (800, 1184), (1184, 1568), (1568, 2080), (2080, 2336)]
    for s in range(NSLOT):
        b = s % NB
        rst = r_st[b]
        tsl = slice(4 * s, 4 * s + 4)
        for i in range(2):
            P.cp("pool", stKD[b][:, i::2, :, 32:40], augK[:, 0:6, tsl, :], r=[r_aug], pw=[rst])
            P.cp("pool", stQD[b][:, i::2, :, 32:40], augQ[:, 0:6, tsl, :], r=[r_aug], pw=[rst])
        P.cp("pool", stKM[b][:, :, :, 96:104], augK[:, 6:10, tsl, :], r=[r_aug], pw=[rst])
        P.cp("pool", stQM[b][:, :, :, 96:104], augQ[:, 6:10, tsl, :], r=[r_aug], pw=[rst])
        for hh in range(4):
            P.cp("pool", stKM[b][:, hh, :, 64:96], onehot[:, tsl, :], r=[r_aug], pw=[rst])
        for tt in range(4):
            t = 4 * s + tt
            hi_ = cnt["h"] % 3
            cnt["h"] += 1
            P.dma("sp", hbuf[hi_][:], h_in[t * 128:(t + 1) * 128, :], w=[r_h[hi_]])
            ni = cnt["n"] % 2
            cnt["n"] += 1
            rmsnorm_tile(C, hbuf[hi_][:], g_attn[:], nbf[ni][:], D, scr, r_h[hi_], r_w, r_n[ni], "a")
            pi = cnt["ptr"] % 2
            cnt["ptr"] += 1
            for kc in range(8):
                P.tr(ptr[pi][:, kc * 128:(kc + 1) * 128], nbf[ni][:, kc * 128:(kc + 1) * 128], ident[:],
                     r=[r_n[ni], r_w], pw=[r_ptr[pi]])
            evac(nT[b][:, :, tt * 128:(tt + 1) * 128], ptr[pi][:].rearrange("p (k c) -> p k c", k=8),
                 r=[r_ptr[pi]], pw=[r_nT[b]])
        if DBG_STOP == 1:
            P.emit()
            return nc
        for tt in range(4):
            t = 4 * s + tt
            ci = cnt["n"] % 2
            cnt["n"] += 1
            for ch, (c0, c1) in enumerate(chunks):
                pi = cnt["pp"] % 4
                cnt["pp"] += 1
                w_ = c1 - c0
                for kc in range(8):
                    P.mm(pp[pi][:, 0:w_], nT[b][:, kc, tt * 128:(tt + 1) * 128], w_in[:, kc, c0:c1],
                         kc == 0, kc == 7, r=[r_nT[b], r_w], w=[r_pp[pi]])
                src = pp[pi]
                rp = r_pp[pi]
                if ch == 0:
                    rmsnorm_tile(C, src[:, 0:256], g_q[:], cqn[ci][:, 0:256], 256, scr2, rp, r_w, r_cqn[ci], "q")
                    rmsnorm_tile(C, src[:, 256:384], g_kv[:], cqn[ci][:, 256:384], 128, scr2, rp, r_w, r_cqn[ci], "kv")
                    x1 = src[:, 384:400]
                    x2 = src[:, 400:416]
                    co = cos_t[:, t, :]
                    si = sin_t[:, t, :]
                    ta, tb = ropet[0][:, 0, :], ropet[1][:, 0, :]
                    P.tt("dve", ta, x1, co, ALU.mult, r=[rp, r_pos], w=[r_rope[0]])
                    P.tt("dve", tb, x2, si, ALU.mult, r=[rp, r_pos], w=[r_rope[1]])
                    P.tt("dve", krt[:, 0:16], ta, tb, ALU.subtract, r=[r_rope[0], r_rope[1]], w=[r_kr])
                    P.tt("dve", ta, x1, si, ALU.mult, r=[rp, r_pos], w=[r_rope[0]])
                    P.tt("dve", tb, x2, co, ALU.mult, r=[rp, r_pos], w=[r_rope[1]])
                    P.tt("dve", krt[:, 16:32], ta, tb, ALU.add, r=[r_rope[0], r_rope[1]], pw=[r_kr])
                    for hh in range(6):
                        P.cp("pool", stKA[b][:, hh, tt, 64:96], krt[:], r=[r_kr], pw=[rst])
                    ti = cnt["ptr"] % 2
                    cnt["ptr"] += 1
                    for kc in range(3):
                        P.tr(ptr[ti][:, kc * 128:(kc + 1) * 128], cqn[ci][:, kc * 128:(kc + 1) * 128], ident[:],
                             r=[r_cqn[ci], r_w], w=[r_ptr[ti]])
                    evac(cT[b][:, :, tt * 128:(tt + 1) * 128],
                         ptr[ti][:, 0:384].rearrange("p (k c) -> p k c", k=3), r=[r_ptr[ti]], pw=[r_cT[b]])
                elif ch == 1:
                    evac(stQD[b][:, :, tt, 0:32], src[:, 0:384].rearrange("p (m c) -> p m c", m=12), r=[rp], pw=[rst])
                elif ch == 2:
                    evac(stKD[b][:, :, tt, 0:32], src[:, 0:384].rearrange("p (m c) -> p m c", m=12), r=[rp], pw=[rst])
                elif ch == 3:
                    evac(stVD[b][:, :, tt, 0:64], src[:, 0:384].rearrange("p (m c) -> p m c", m=6), r=[rp], pw=[rst])
                elif ch == 4:
                    evac(stQM[b][:, :, tt, 0:64], src[:, 0:256].rearrange("p (m c) -> p m c", m=4), r=[rp], pw=[rst])
                    evac(stKM[b][:, :, tt, 0:64], src[:, 256:512].rearrange("p (m c) -> p m c", m=4), r=[rp], pw=[rst])
                else:
                    evac(stVM[b][:, :, tt, 0:64], src[:, 0:256].rearrange("p (m c) -> p m c", m=4), r=[rp], pw=[rst])
        if DBG_STOP == 2:
            P.emit()
            return nc
        for tt in range(4):
            t = 4 * s + tt
            co = cos_t[:, t, :]
            si = sin_t[:, t, :]
            for half in range(2):
                qi = cnt["pq"] % 2
                cnt["pq"] += 1
                for kc in range(2):
                    P.mm(pq[qi][:, 0:288], cT[b][:, kc, tt * 128:(tt + 1) * 128], w_uq[:, kc, half * 288:(half + 1) * 288],
                         kc == 0, kc == 1, r=[r_cT[b], r_w], w=[r_pq[qi]])
                v3 = pq[qi][:, 0:288].rearrange("p (m c) -> p m c", m=3)
                hs = slice(3 * half, 3 * half + 3)
                evac(stQA[b][:, hs, tt, 0:64], v3[:, :, 0:64], r=[r_pq[qi]], pw=[rst])
                for m in range(3):
                    hh = 3 * half + m
                    x1 = pq[qi][:, m * 96 + 64:m * 96 + 80]
                    x2 = pq[qi][:, m * 96 + 80:m * 96 + 96]
                    ta, tb, tc, td = (ropet[i][:, hh, :] for i in range(4))
                    rr_ = r_rope[hh]
                    P.tt("dve", ta, x1, co, ALU.mult, r=[r_pq[qi], r_pos], w=[rr_])
                    P.tt("dve", tb, x2, si, ALU.mult, r=[r_pq[qi], r_pos], pw=[rr_])
                    P.tt("dve", tc, x1, si, ALU.mult, r=[r_pq[qi], r_pos], pw=[rr_])
                    P.tt("dve", td, x2, co, ALU.mult, r=[r_pq[qi], r_pos], pw=[rr_])
                    P.tt("pool", stQA[b][:, hh, tt, 64:80], ta, tb, ALU.subtract, r=[rr_], pw=[rst])
                    P.tt("pool", stQA[b][:, hh, tt, 80:96], tc, td, ALU.add, r=[rr_], pw=[rst])
                qi = cnt["pq"] % 2
                cnt["pq"] += 1
                P.mm(pq[qi][:, 0:384], cT[b][:, 2, tt * 128:(tt + 1) * 128], w_ukv[:, half * 384:(half + 1) * 384],
                     True, True, r=[r_cT[b], r_w], w=[r_pq[qi]])
                v3 = pq[qi][:, 0:384].rearrange("p (m c) -> p m c", m=3)
                evac(stKA[b][:, hs, tt, 0:64], v3[:, :, 0:64], r=[r_pq[qi]], pw=[rst])
                evac(stVA[b][:, hs, tt, 0:64], v3[:, :, 64:128], r=[r_pq[qi]], pw=[rst])
        if DBG_STOP == 3:
            P.emit()
            return nc
        for (st, dd, nm) in ((stKA, kA_d, 6), (stQA, qA_d, 6), (stVA, vA_d, 6), (stKD, kD_d, 12), (stQD, qD_d, 12),
                             (stVD, vD_d, 6), (stKM, kM_d, 4), (stQM, qM_d, 4), (stVM, vM_d, 4)):
            for m in range(nm):
                P.dma("sp", dd[m, :, tsl, :], st[b][:, m, :, :], r=[rst], is_output=True)
    P.emit()
    return nc


def own_qtiles(S, parity):
    nqt = S // 512
    out = []
    for p in range(nqt // 2):
        out.append(2 * p + parity if p % 2 == 0 else 2 * p + 1 - parity)
    return out


def own_tiles(S, parity):
    return [4 * q + i for q in own_qtiles(S, parity) for i in range(4)]


def const_inputs_A(S, parity):
    tiles = own_tiles(S, parity)
    TT = len(tiles)
    onehot = np.zeros((128, TT, 32), np.float32)
    for i, g in enumerate(tiles):
        onehot[:, i, g // 2] = 1.0
    invf = (np.float32(10000.0) ** (-np.arange(16, dtype=np.float32) / np.float32(16))).astype(np.float32)
    return {"onehot": onehot.astype(NPBF), "invf": invf.reshape(1, 16),
            "ident": np.eye(128, dtype=np.float32)}


def tok_rows(S, parity):
    return np.concatenate([np.arange(g * 128, (g + 1) * 128) for g in own_tiles(S, parity)])


def const_inputs_B(S, parity):
    tiles = own_tiles(S, parity)
    TT = len(tiles)
    kk = np.arange(128)[:, None]
    qq = np.arange(512)[None, :]
    diag = [np.where(d * 128 + kk <= qq, 0.0, NEGM).astype(np.float32) for d in range(4)]
    full = np.full((128, 512), NEGM, np.float32)
    zero = np.zeros((128, 512), np.float32)
    role_min = np.stack(diag + [full] * 4)
    role_max = np.stack([zero] * 4 + diag)
    m_even = role_min if parity == 0 else role_max
    m_odd = role_max if parity == 0 else role_min
    pastmask = np.zeros((128, TT, 32), np.float32)
    isown = np.zeros((128, TT, 32), np.float32)
    for i, g in enumerate(tiles):
        ob = g // 2
        pastmask[:, i, ob:] = -1e30
        if ob < 32:
            isown[:, i, ob] = 1.0
    return {"m_even": m_even.astype(NPBF), "m_odd": m_odd.astype(NPBF), "pastmask": pastmask, "isown": isown,
            "ident": np.eye(128, dtype=np.float32)}


def build_B(S, lam_init):
    TO = S // 2
    TT = TO // 128
    NSLOT = TT // 4
    NKT = S // 128
    NBLK = S // 256
    nc = bass.Bass("TRN2", target_bir_lowering=False)
    C = Ctx(nc)
    P = C.P
    kA_d = C.dram("kA", [6, 128, NKT, KA_A], BF16)
    qA_d = C.dram("qA", [6, 128, TT, KA_A], BF16)
    vA_d = C.dram("vA", [6, 128, NKT, 65], BF16)
    kD_d = C.dram("kD", [12, 128, NKT, KA_D], BF16)
    qD_d = C.dram("qD", [12, 128, TT, KA_D], BF16)
    vD_d = C.dram("vD", [6, 128, NKT, 65], BF16)
    kM_d = C.dram("kM", [4, 128, NKT, KA_M], BF16)
    qM_d = C.dram("qM", [4, 128, TT, KA_M], BF16)
    vM_d = C.dram("vM", [4, 128, NKT, 65], BF16)
    m_even_d = C.dram("m_even", [8, 128, 512], BF16)
    m_odd_d = C.dram("m_odd", [8, 128, 512], BF16)
    pastmask_d = C.dram("pastmask", [128, TT, 32], F32)
    isown_d = C.dram("isown", [128, TT, 32], F32)
    ident_d = C.dram("ident", [128, 128], F32)
    lam_d = [C.dram(n, [1, 32], F32) for n in ("lq1", "lk1", "lq2", "lk2")]
    gsub_d = C.dram("sub_norm", [1, 64], F32)
    o_d = C.dram("o", [128, TT, D], BF16, out=True)

    r_c = Res("consts")
    ident = C.sb([128, 128], BF16)
    masks = [C.sb([128, 8, 512], BF16) for _ in range(2)]
    pastmask = C.sb([128, TT, 32], F32)
    isown = C.sb([128, TT, 32], F32)
    lamt = [C.sb([128, 32], F32) for _ in range(4)]
    gsub = C.sb([128, 64], F32)
    neghalf = C.sb([128, 4], F32)
    P.memset("pool", neghalf[:], -0.5, pw=[r_c])
    P.dma("pool", ident[:], ident_d, pw=[r_c])
    for i, md in enumerate((m_even_d, m_odd_d)):
        for j in range(8):
            P.dma("sp", masks[i][:, j, :], md[j], pw=[r_c])
    P.dma("sp", pastmask[:], pastmask_d, pw=[r_c])
    P.dma("sp", isown[:], isown_d, pw=[r_c])
    for i in range(4):
        P.dma("sp", lamt[i][:], lam_d[i][0, :].partition_broadcast(128), pw=[r_c])
    P.dma("sp", gsub[:], gsub_d[0, :].partition_broadcast(128), pw=[r_c])
    r_lam = Res("lam")
    lprod = C.sb([128, 32], F32)
    lsum = C.sb([128, 2], F32)
    neg_lam = C.sb([128, 1], F32)
    for i in range(2):
        P.tt("dve", lprod[:], lamt[2 * i][:], lamt[2 * i + 1][:], ALU.mult, r=[r_c], w=[r_lam])
        P.op("dve", lambda e, i=i: e.reduce_sum(out=lsum[:, i:i + 1], in_=lprod[:], axis=AX.X), r=[r_lam], w=[r_lam])
    P.act(lsum[:], lsum[:], AF.Exp, r=[r_lam], w=[r_lam])
    P.stt(neg_lam[:], lsum[:, 1:2], -float(lam_init), lsum[:, 0:1], ALU.add, ALU.subtract, r=[r_lam], w=[r_lam])
    P.ts("dve", gsub[:], gsub[:], 1.0 - float(lam_init), ALU.mult, r=[r_c], w=[r_c])

    KT = [C.sb([128, 2, S], BF16) for _ in range(2)]
    QT = [C.sb([128, 2, TO], BF16) for _ in range(2)]
    V = [C.sb([128, NKT, 65], BF16) for _ in range(2)]
    r_KT = [Res("KT%d" % i) for i in range(2)]
    r_QT = [Res("QT%d" % i) for i in range(2)]
    r_V = [Res("V%d" % i) for i in range(2)]
    CH = 16 if NKT >= 16 else NKT
    stg = [C.sb([128, CH, KA_M], BF16) for _ in range(3)]
    r_stg = [Res("stg%d" % i) for i in range(3)]
    PT = [C.sb([128, 512], BF16) for _ in range(4)]
    r_PT = [Res("PT%d" % i) for i in range(4)]
    osl = [C.sb([128, 4, 64], BF16) for _ in range(2)]
    r_osl = [Res("osl%d" % i) for i in range(2)]
    pS4 = C.ps([128, 4, 512], F32)
    pS = [pS4[:, i, :] for i in range(4)]
    r_pS = [Res("pS%d" % i, psum=True) for i in range(4)]
    pO = [C.ps([128, 512], F32) for _ in range(3)]
    r_pO = [Res("pO%d" % i, psum=True) for i in range(3)]
    pT = [C.ps([128, 1024], BF16) for _ in range(1)]
    r_pT = [Res("pT%d" % i, psum=True) for i in range(1)]
    pG = pS4[:, 0, :]
    r_pG = r_pS[0]
    PT2 = [C.sb([128, 2, 512], BF16) for _ in range(3)]
    r_PT2 = [Res("PT2_%d" % i) for i in range(3)]
    kmT = C.sb([64, 32], F32)
    kmT_bf = C.sb([64, 32], BF16)
    gm = C.sb([128, 32], F32)
    m8 = C.sb([128, 8], F32)
    msel = C.sb([128, 4, 32], BF16)
    r_g = Res("gate")
    r_ms = Res("msel")
    rz = C.sb([128, 2, 4], F32)
    t1 = C.sb([128, 4, 64], F32)
    t2 = C.sb([128, 4, 64], F32)
    sq = C.sb([128, 4, 64], F32)
    ssd = C.sb([128, 4], F32)
    r_nrm = Res("nrm")
    cnt = {"stg": 0, "pT": 0, "pS": 0, "pO": 0, "PT": 0, "osl": 0}

    units = []
    for h in range(6):
        units.append(dict(kind="A", maps=[(kA_d[h], qA_d[h], KA_A)], v=vA_d[h], scale=96 ** -0.5, col=h * 64))
    for h in range(6):
        units.append(dict(kind="D", maps=[(kD_d[2 * h + i], qD_d[2 * h + i], KA_D) for i in range(2)], v=vD_d[h],
                          scale=32 ** -0.5, col=384 + h * 64))
    for h in range(4):
        units.append(dict(kind="M", maps=[(kM_d[h], qM_d[h], KA_M)], v=vM_d[h], scale=64 ** -0.5, col=768 + h * 64))

    for si in range(3):
        P.memset("pool", stg[si][:], 0.0, w=[r_stg[si]])

    def transpose_in(src_d, ntiles, dst, mi, KA, r_dst, src2=None):
        for c0 in range(0, ntiles, CH):
            n = min(CH, ntiles - c0)
            si = cnt["stg"] % 3
            cnt["stg"] += 1
            if src2 is None:
                P.dma("sp", stg[si][:, 0:n, 0:KA], src_d[:, c0:c0 + n, :], w=[r_stg[si]])
            else:
                P.dma("sp", stg[si][:, 0:n, 0:KA_D], src_d[:, c0:c0 + n, :], w=[r_stg[si]])
                P.dma("sp", stg[si][:, 0:n, 64:64 + KA_D], src2[:, c0:c0 + n, :], pw=[r_stg[si]])
            for g0 in range(0, n, 4):
                gn = min(4, n - g0)
                pi = cnt["pT"] % len(pT)
                cnt["pT"] += 1
                for t in range(gn):
                    P.tr(pT[pi][0:KA, t * 128:(t + 1) * 128], stg[si][:, g0 + t, 0:KA], ident[:],
                         r=[r_stg[si], r_c], pw=[r_pT[pi]])
                c = (c0 + g0) * 128
                P.cp("dve", dst[0:KA, mi, c:c + gn * 128], pT[pi][0:KA, 0:gn * 128], r=[r_pT[pi]], pw=[r_dst])

    def prep(u):
        U = units[u]
        ub = u % 2
        for c0 in range(0, NKT, 32):
            n = min(32, NKT - c0)
            P.dma("sp", V[ub][:, c0:c0 + n, :], U["v"][:, c0:c0 + n, :], pw=[r_V[ub]])
        if U["kind"] == "D":
            (k0, q0, _), (k1, q1, _) = U["maps"]
            transpose_in(k0, NKT, KT[ub], 0, KA_M, r_KT[ub], src2=k1)
            transpose_in(q0, TT, QT[ub], 0, KA_M, r_QT[ub], src2=q1)
        else:
            for mi, (kd, qd, KA) in enumerate(U["maps"]):
                transpose_in(kd, NKT, KT[ub], mi, KA, r_KT[ub])
                transpose_in(qd, TT, QT[ub], mi, KA, r_QT[ub])
        if U["kind"] == "M":
            P.memset("dve", kmT[:], 0.0, w=[r_g])
            P.op("dve", lambda e: e.tensor_reduce(out=kmT[:, 0:NBLK],
                                                  in_=KT[ub][0:64, 0, :].rearrange("p (n k) -> p n k", k=256),
                                                  axis=AX.X, op=ALU.add), r=[r_KT[ub]], w=[r_g])
            P.ts("dve", kmT_bf[:], kmT[:], 1.0 / 256.0, ALU.mult, r=[r_g], w=[r_g])
            for g0 in range(0, TT, 4):
                for t4 in range(4):
                    t = g0 + t4
                    P.mm(pG[:, 0:32], QT[ub][0:64, 0, t * 128:(t + 1) * 128], kmT_bf[:, :], True, True,
                         r=[r_QT[ub], r_g], w=[r_pG])
                    P.tt("dve", gm[:], pG[:, 0:32], pastmask[:, t, :], ALU.add, r=[r_pG, r_c], w=[r_g])
                    P.op("dve", lambda e: e.max(out=m8[:], in_=gm[:]), r=[r_g], w=[r_g])
                    P.ts("dve", gm[:], gm[:], m8[:, 2:3], ALU.is_ge, r=[r_g], w=[r_g])
                    P.tt("dve", gm[:], gm[:], isown[:, t, :], ALU.max, r=[r_g, r_c], w=[r_g])
                    P.ts("dve", msel[:, t4, :], gm[:], -1.0, ALU.add, -NEGM, ALU.mult, r=[r_g],
                         **({"w": [r_ms]} if t4 == 0 else {"pw": [r_ms]}))
                pi = cnt["pT"] % len(pT)
                cnt["pT"] += 1
                for t4 in range(4):
                    P.tr(pT[pi][0:32, t4 * 128:(t4 + 1) * 128], msel[:, t4, :], ident[:], r=[r_ms, r_c], pw=[r_pT[pi]])
                P.cp("act", QT[ub][64:96, 0, g0 * 128:(g0 + 4) * 128], pT[pi][0:32, 0:512], r=[r_pT[pi]], pw=[r_QT[ub]])

    def attention(u):
        U = units[u]
        ub = u % 2
        scale = float(U["scale"])
        dual = U["kind"] == "D"
        for p in range(NSLOT):
            nkt = 8 * (p + 1)
            mk = masks[p % 2]
            obanks = []
            if not dual:
                KA = U["maps"][0][2]
                oi = cnt["pO"] % 3
                cnt["pO"] += 1
                obanks.append(oi)
                q_ap = QT[ub][0:KA, 0, p * 512:(p + 1) * 512]
                sbank = {}

                def qkp(sidx):
                    pr = cnt["pS"] % 2
                    cnt["pS"] += 1
                    sbank[sidx] = pr
                    for m in range(2):
                        j = 2 * sidx + m
                        si = 2 * pr + m
                        band = j >= 8 * p
                        P.mm(pS[si], KT[ub][0:KA, 0, j * 128:(j + 1) * 128], q_ap, True, not band,
                             r=[r_KT[ub], r_QT[ub]], w=[r_pS[si]])
                        if band:
                            P.mm(pS[si], ident[:, :], mk[:, j - 8 * p, :], False, True, r=[r_c], pw=[r_pS[si]])

                nst = nkt // 2
                qkp(0)
                first = True
                for sidx in range(nst):
                    pr = sbank[sidx]
                    pi = cnt["PT"] % 3
                    cnt["PT"] += 1
                    P.act(PT2[pi][:], pS4[:, 2 * pr:2 * pr + 2, :], AF.Exp, scale=scale,
                          r=[r_pS[2 * pr], r_pS[2 * pr + 1]], w=[r_PT2[pi]])
                    if sidx + 1 < nst:
                        qkp(sidx + 1)
                    for m in range(2):
                        j = 2 * sidx + m
                        d2 = j - (8 * p + 4)
                        for u4 in range(4):
                            if d2 >= 0 and u4 < d2:
                                continue
                            P.mm(pO[oi][:, u4 * 65:(u4 + 1) * 65], PT2[pi][:, m, u4 * 128:(u4 + 1) * 128], V[ub][:, j, :],
                                 first, j == nkt - 1, r=[r_PT2[pi], r_V[ub]],
                                 **({"w": [r_pO[oi]]} if first else {"pw": [r_pO[oi]]}))
                            first = False
            else:
                ois = []
                for _ in range(2):
                    ois.append(cnt["pO"] % 3)
                    cnt["pO"] += 1
                obanks.extend(ois)
                sbank = {}

                def qk2(j):
                    pr = cnt["pS"] % 2
                    cnt["pS"] += 1
                    sbank[j] = pr
                    band = j >= 8 * p
                    for m, base in enumerate((0, 64)):
                        si = 2 * pr + m
                        P.mm(pS[si], KT[ub][base:base + KA_D, 0, j * 128:(j + 1) * 128],
                             QT[ub][base:base + KA_D, 0, p * 512:(p + 1) * 512], True, not band,
                             r=[r_KT[ub], r_QT[ub]], w=[r_pS[si]])
                    if band:
                        for m in range(2):
                            si = 2 * pr + m
                            P.mm(pS[si], ident[:, :], mk[:, j - 8 * p, :], False, True, r=[r_c], pw=[r_pS[si]])

                qk2(0)
                firsts = [True, True]
                for j in range(nkt):
                    pr = sbank[j]
                    pi = cnt["PT"] % 3
                    cnt["PT"] += 1
                    P.act(PT2[pi][:], pS4[:, 2 * pr:2 * pr + 2, :], AF.Exp, scale=scale,
                          r=[r_pS[2 * pr], r_pS[2 * pr + 1]], w=[r_PT2[pi]])
                    if j + 1 < nkt:
                        qk2(j + 1)
                    d2 = j - (8 * p + 4)
                    for m in range(2):
                        oi = ois[m]
                        for u4 in range(4):
                            if d2 >= 0 and u4 < d2:
                                continue
                            P.mm(pO[oi][:, u4 * 65:(u4 + 1) * 65], PT2[pi][:, m, u4 * 128:(u4 + 1) * 128], V[ub][:, j, :],
                                 firsts[m], j == nkt - 1, r=[r_PT2[pi], r_V[ub]],
                                 **({"w": [r_pO[oi]]} if firsts[m] else {"pw": [r_pO[oi]]}))
                            firsts[m] = False
            oi0 = obanks[0]
            O0 = pO[oi0][:, 0:260].rearrange("p (u c) -> p u c", c=65)
            so = cnt["osl"] % 2
            cnt["osl"] += 1
            P.op("dve", lambda e, O0=O0: e.reciprocal(out=rz[:, 0, :], in_=O0[:, :, 64]), r=[r_pO[oi0]], w=[r_nrm])
            if U["kind"] != "D":
                for u4 in range(4):
                    P.ts("dve", osl[so][:, u4, :], O0[:, u4, 0:64], rz[:, 0, u4:u4 + 1], ALU.mult,
                         r=[r_pO[oi0], r_nrm], **({"w": [r_osl[so]]} if u4 == 0 else {"pw": [r_osl[so]]}))
            else:
                oi1 = obanks[1]
                O1 = pO[oi1][:, 0:260].rearrange("p (u c) -> p u c", c=65)
                P.op("dve", lambda e, O1=O1: e.reciprocal(out=rz[:, 1, :], in_=O1[:, :, 64]), r=[r_pO[oi1]], pw=[r_nrm])
                for u4 in range(4):
                    P.ts("dve", t1[:, u4, :], O0[:, u4, 0:64], rz[:, 0, u4:u4 + 1], ALU.mult,
                         r=[r_pO[oi0], r_nrm], pw=[r_nrm])
                    P.ts("dve", t2[:, u4, :], O1[:, u4, 0:64], rz[:, 1, u4:u4 + 1], ALU.mult, neg_lam[:, 0:1], ALU.mult,
                         r=[r_pO[oi1], r_nrm, r_lam], pw=[r_nrm])
                P.tt("dve", t1[:], t1[:], t2[:], ALU.add, r=[r_nrm], w=[r_nrm])
                P.tt("dve", sq[:], t1[:], t1[:], ALU.mult, r=[r_nrm], w=[r_nrm])
                P.op("dve", lambda e: e.reduce_sum(out=ssd[:], in_=sq[:], axis=AX.X), r=[r_nrm], w=[r_nrm])
                P.ts("dve", ssd[:], ssd[:], 1.0 / 64.0, ALU.mult, EPS, ALU.add, r=[r_nrm], w=[r_nrm])
                P.tt("pool", ssd[:], ssd[:], neghalf[:], ALU.pow, r=[r_nrm, r_c], w=[r_nrm])
                for u4 in range(4):
                    P.stt(osl[so][:, u4, :], t1[:, u4, :], ssd[:, u4:u4 + 1], gsub[:], ALU.mult, ALU.mult,
                          r=[r_nrm, r_c], **({"w": [r_osl[so]]} if u4 == 0 else {"pw": [r_osl[so]]}))
            P.dma("sp", o_d[:, 4 * p:4 * p + 4, U["col"]:U["col"] + 64], osl[so][:], r=[r_osl[so]], is_output=True)

    prep(0)
    for u in range(len(units)):
        if u + 1 < len(units):
            prep(u + 1)
        attention(u)
    P.emit()
    return nc


def assemble_kv(outA_pair, S):
    res = {}
    pos_of = {}
    for par in range(2):
        for i, g in enumerate(own_tiles(S, par)):
            pos_of[g] = (par, i)
    NKT = S // 128
    for name in ("kA", "vA", "kD", "vD", "kM", "vM"):
        a0, a1 = outA_pair[0][name], outA_pair[1][name]
        full = np.empty((a0.shape[0], 128, NKT, a0.shape[3]), dtype=a0.dtype)
        for g in range(NKT):
            par, i = pos_of[g]
            full[:, :, g, :] = (a0 if par == 0 else a1)[:, :, i, :]
        res[name] = full
    return res


_NC_CACHE = {}


def _get_nc(key, fn):
    if key not in _NC_CACHE:
        _NC_CACHE[key] = fn()
    return _NC_CACHE[key]


def _run(nc, in_maps):
    res = run_bass_kernel_spmd(nc, in_maps, core_ids=list(range(8)))
    return res.results


def launch_A(S, h_own, positions, W, l):
    nc = _get_nc(("A", S), lambda: build_A(S))
    in_maps = []
    for c in range(8):
        b, par = c // 2, c % 2
        rows = tok_rows(S, par)
        TT = len(rows) // 128
        m = dict(const_inputs_A(S, par))
        m["h"] = np.ascontiguousarray(h_own[c])
        m["pos"] = np.ascontiguousarray(positions[b][rows].reshape(TT, 128).T)
        m["pos0"] = np.ascontiguousarray(positions[b, 0:1].reshape(1, 1))
        m["attn_norm"] = W["attn_norm"][l].reshape(1, -1)
        m["w_in"] = W["w_in"][l]
        m["q_norm"] = W["mla_q_norm"][l].reshape(1, -1)
        m["w_uq"] = W["mla_w_uq"][l]
        m["kv_norm"] = W["mla_kv_norm"][l].reshape(1, -1)
        m["w_ukv"] = W["mla_w_ukv"][l]
        in_maps.append(m)
    return _run(nc, in_maps)


def launch_B(S, outA, W, l):
    lam_init = 0.8 - 0.6 * math.exp(-0.3 * l)
    nc = _get_nc(("B", S, l), lambda: build_B(S, lam_init))
    in_maps = []
    for b in range(4):
        kv = assemble_kv([outA[2 * b], outA[2 * b + 1]], S)
        for par in range(2):
            m = dict(const_inputs_B(S, par))
            m.update(kv)
            for nm in ("qA", "qD", "qM"):
                m[nm] = outA[2 * b + par][nm]
            m["lq1"] = W["diff_lambda_q1"][l].reshape(1, -1)
            m["lk1"] = W["diff_lambda_k1"][l].reshape(1, -1)
            m["lq2"] = W["diff_lambda_q2"][l].reshape(1, -1)
            m["lk2"] = W["diff_lambda_k2"][l].reshape(1, -1)
            m["sub_norm"] = W["diff_sub_norm"][l].reshape(1, -1)
            in_maps.append(m)
    return _run(nc, in_maps)


def transpose_tile(C, src_bf, dstT, col0, ident, pT, r_pT, r_src, r_dst, r_id, cnt, nchunk=8):
    P = C.P
    pi = cnt["pT"] % len(pT)
    cnt["pT"] += 1
    for kc in range(nchunk):
        P.tr(pT[pi][:, kc * 128:(kc + 1) * 128], src_bf[:, kc * 128:(kc + 1) * 128], ident[:],
             r=[r_src, r_id], pw=[r_pT[pi]])
    cnt["ev"] += 1
    P.cp("act" if cnt["ev"] % 2 else "dve", dstT[:, 0:nchunk, col0:col0 + 128],
         pT[pi][:, 0:nchunk * 128].rearrange("p (k c) -> p k c", k=nchunk), r=[r_pT[pi]], pw=[r_dst])


def build_C1(S):
    TO = S // 2
    TT = TO // 128
    NSLOT = TT // 4
    nc = bass.Bass("TRN2", target_bir_lowering=False)
    C = Ctx(nc)
    P = C.P
    h_d = C.dram("h", [TO, D], F32)
    o_d = C.dram("o", [128, TT, D], BF16)
    mem_d = C.dram("mem", [256, D], F32)
    ident_d = C.dram("ident", [128, 128], F32)
    w_out_d = C.dram("w_out", [D, D], F32)
    g_cross_d = C.dram("cross_norm", [1, D], F32)
    g_mem_d = C.dram("mem_norm", [1, D], F32)
    wq_d = C.dram("wq", [D, D], F32)
    wkv_d = C.dram("wkv", [D, 2 * D], F32)
    wo_d = C.dram("wo", [D, D], F32)
    hout_d = C.dram("h_out", [TO, D], F32, out=True)

    r_w = Res("w")
    ident = C.sb([128, 128], BF16)
    w_out = C.sb([128, 8, D], BF16)
    wq = C.sb([128, 8, D], BF16)
    wo = C.sb([128, 8, D], BF16)
    wkv = C.sb([128, 8, 2 * D], BF16)
    g_cross = C.sb([128, D], F32)
    g_mem = C.sb([128, D], F32)
    P.dma("pool", ident[:], ident_d, pw=[r_w])
    P.dma("sp", g_cross[:], g_cross_d[0, :].partition_broadcast(128), pw=[r_w])
    P.dma("sp", g_mem[:], g_mem_d[0, :].partition_broadcast(128), pw=[r_w])
    load_w_bf16(C, wkv, wkv_d, D, 2 * D, r_w)
    load_w_bf16(C, w_out, w_out_d, D, D, r_w)
    load_w_bf16(C, wq, wq_d, D, D, r_w)
    load_w_bf16(C, wo, wo_d, D, D, r_w)

    hb = [C.sb([128, D], F32) for _ in range(4)]
    r_hb = [Res("hb%d" % i) for i in range(4)]
    nbf = [C.sb([128, D], BF16) for _ in range(2)]
    r_nbf = [Res("nbf%d" % i) for i in range(2)]
    xT = [C.sb([128, 8, 512], BF16) for _ in range(2)]
    r_xT = [Res("xT%d" % i) for i in range(2)]
    osb = C.sb([128, 4, D], BF16)
    r_osb = Res("osb")
    cqT = C.sb([128, 8, 512], BF16)
    r_cqT = Res("cqT")
    oc = C.sb([128, 4, D], BF16)
    r_oc = Res("oc")
    PTc = [C.sb([128, 512], BF16) for _ in range(2)]
    r_PTc = [Res("PTc%d" % i) for i in range(2)]
    kmemT = C.sb([128, 8, 256], BF16)
    vmem = C.sb([128, 2, 4, 257], BF16)
    r_kv = Res("memkv")
    rzc = C.sb([128, 1], F32)
    r_rz = Res("rzc")
    scr = {"junk": C.sb([128, D], BF16), "ss": C.sb([128, 1], F32), "rstd": C.sb([128, 1], F32), "res": Res("scr")}
    pT = [C.ps([128, D], BF16) for _ in range(2)]
    r_pT = [Res("pT%d" % i, psum=True) for i in range(2)]
    pP = [C.ps([128, 512], F32) for _ in range(2)]
    r_pP = [Res("pP%d" % i, psum=True) for i in range(2)]
    pS = [C.ps([128, 512], F32) for _ in range(2)]
    r_pS = [Res("pS%d" % i, psum=True) for i in range(2)]
    pO = [C.ps([128, 512], F32) for _ in range(2)]
    r_pO = [Res("pO%d" % i, psum=True) for i in range(2)]
    cnt = {"pT": 0, "ev": 0, "pP": 0, "pS": 0, "pO": 0, "n": 0, "x": 0}

    def evac(out, in_, r, w=(), pw=()):
        cnt["ev"] += 1
        P.cp("act" if cnt["ev"] % 2 else "dve", out, in_, r=r, w=w, pw=pw)

    memT = xT[1]
    for mt in range(2):
        hi = mt
        P.dma("sp", hb[hi][:], mem_d[mt * 128:(mt + 1) * 128, :], w=[r_hb[hi]])
        ni = cnt["n"] % 2
        cnt["n"] += 1
        rmsnorm_tile(C, hb[hi][:], g_mem[:], nbf[ni][:], D, scr, r_hb[hi], r_w, r_nbf[ni], "m")
        transpose_tile(C, nbf[ni], memT, mt * 128, ident, pT, r_pT, r_nbf[ni], r_xT[1], r_w, cnt)
    for fc in range(8):
        pi = cnt["pP"] % 2
        cnt["pP"] += 1
        for kc in range(8):
            P.mm(pP[pi][:, 0:256], wkv[:, kc, fc * 128:(fc + 1) * 128], memT[:, kc, 0:256], kc == 0, kc == 7,
                 r=[r_w, r_xT[1]], w=[r_pP[pi]])
        evac(kmemT[:, fc, :], pP[pi][:, 0:256], r=[r_pP[pi]], pw=[r_kv])
    P.memset("pool", vmem[:, :, :, 256], 1.0, pw=[r_kv])
    for mt in range(2):
        for half in range(2):
            pi = cnt["pP"] % 2
            cnt["pP"] += 1
            for kc in range(8):
                P.mm(pP[pi][:, :], memT[:, kc, mt * 128:(mt + 1) * 128], wkv[:, kc, D + half * 512:D + (half + 1) * 512],
                     kc == 0, kc == 7, r=[r_w, r_xT[1]], w=[r_pP[pi]])
            evac(vmem[:, mt, 2 * half:2 * half + 2, 0:256], pP[pi][:, :].rearrange("p (h c) -> p h c", h=2),
                 r=[r_pP[pi]], pw=[r_kv])

    def proj_add(srcT, r_srcT, w_sb, tt):
        for half in range(2):
            pi = cnt["pP"] % 2
            cnt["pP"] += 1
            for kc in range(8):
                P.mm(pP[pi][:, :], srcT[:, kc, tt * 128:(tt + 1) * 128], w_sb[:, kc, half * 512:(half + 1) * 512],
                     kc == 0, kc == 7, r=[r_srcT, r_w], w=[r_pP[pi]])
            P.tt("dve", hb[tt][:, half * 512:(half + 1) * 512], hb[tt][:, half * 512:(half + 1) * 512], pP[pi][:, :],
                 ALU.add, r=[r_pP[pi]], w=[r_hb[tt]])

    for p in range(NSLOT):
        P.dma("sp", osb[:], o_d[:, 4 * p:4 * p + 4, :], w=[r_osb])
        for tt in range(4):
            t = 4 * p + tt
            P.dma("sp", hb[tt][:], h_d[t * 128:(t + 1) * 128, :], w=[r_hb[tt]])
        xa = cnt["x"] % 2
        cnt["x"] += 1
        for tt in range(4):
            transpose_tile(C, osb[:, tt, :], xT[xa], tt * 128, ident, pT, r_pT, r_osb, r_xT[xa], r_w, cnt)
        for tt in range(4):
            proj_add(xT[xa], r_xT[xa], w_out, tt)
        xb = cnt["x"] % 2
        cnt["x"] += 1
        for tt in range(4):
            ni = cnt["n"] % 2
            cnt["n"] += 1
            rmsnorm_tile(C, hb[tt][:], g_cross[:], nbf[ni][:], D, scr, r_hb[tt], r_w, r_nbf[ni], "c")
            transpose_tile(C, nbf[ni], xT[xb], tt * 128, ident, pT, r_pT, r_nbf[ni], r_xT[xb], r_w, cnt)
        for fc in range(8):
            pi = cnt["pP"] % 2
            cnt["pP"] += 1
            for kc in range(8):
                P.mm(pP[pi][:, :], wq[:, kc, fc * 128:(fc + 1) * 128], xT[xb][:, kc, :], kc == 0, kc == 7,
                     r=[r_w, r_xT[xb]], w=[r_pP[pi]])
            evac(cqT[:, fc, :], pP[pi][:, :], r=[r_pP[pi]], pw=[r_cqT])
        for hh in range(4):
            for mt in range(2):
                si = cnt["pS"] % 2
                cnt["pS"] += 1
                for dc in range(2):
                    P.mm(pS[si][:, :], kmemT[:, hh * 2 + dc, mt * 128:(mt + 1) * 128], cqT[:, hh * 2 + dc, :],
                         dc == 0, dc == 1, r=[r_kv, r_cqT], w=[r_pS[si]])
                P.act(PTc[mt][:], pS[si][:, :], AF.Exp, scale=1.0 / 16.0, r=[r_pS[si]], w=[r_PTc[mt]])
            for tt in range(4):
                oi = cnt["pO"] % 2
                cnt["pO"] += 1
                for mt in range(2):
                    P.mm(pO[oi][:, 0:257], PTc[mt][:, tt * 128:(tt + 1) * 128], vmem[:, mt, hh, :], mt == 0, mt == 1,
                         r=[r_PTc[mt], r_kv], w=[r_pO[oi]])
                P.op("dve", lambda e, oi=oi: e.reciprocal(out=rzc[:], in_=pO[oi][:, 256:257]), r=[r_pO[oi]], w=[r_rz])
                P.ts("dve", oc[:, tt, hh * 256:(hh + 1) * 256], pO[oi][:, 0:256], rzc[:, 0:1], ALU.mult,
                     r=[r_pO[oi], r_rz], pw=[r_oc])
        xc = cnt["x"] % 2
        cnt["x"] += 1
        for tt in range(4):
            transpose_tile(C, oc[:, tt, :], xT[xc], tt * 128, ident, pT, r_pT, r_oc, r_xT[xc], r_w, cnt)
        for tt in range(4):
            t = 4 * p + tt
            proj_add(xT[xc], r_xT[xc], wo, tt)
            P.dma("sp", hout_d[t * 128:(t + 1) * 128, :], hb[tt][:], r=[r_hb[tt]], is_output=True)
    P.emit()
    return nc


def build_C2(S, final):
    TO = S // 2
    TT = TO // 128
    nc = bass.Bass("TRN2", target_bir_lowering=False)
    C = Ctx(nc)
    P = C.P
    h_d = C.dram("h", [TO, D], F32)
    ident_d = C.dram("ident", [128, 128], F32)
    g_mlp_d = C.dram("mlp_norm", [1, D], F32)
    w1_d = C.dram("w1", [D, 4 * D], F32)
    w2_d = C.dram("w2", [4 * D, D], F32)
    g_fin_d = C.dram("final_norm", [1, D], F32)
    hout_d = C.dram("h_out", [TO, D], F32, out=True)
    r_w = Res("w")
    ident = C.sb([128, 128], BF16)
    w1 = C.sb([128, 8, 4 * D], BF16)
    w2 = C.sb([128, 32, D], BF16)
    g_mlp = C.sb([128, D], F32)
    g_fin = C.sb([128, D], F32)
    P.dma("pool", ident[:], ident_d, pw=[r_w])
    P.dma("sp", g_mlp[:], g_mlp_d[0, :].partition_broadcast(128), pw=[r_w])
    P.dma("sp", g_fin[:], g_fin_d[0, :].partition_broadcast(128), pw=[r_w])
    load_w_bf16(C, w1, w1_d, D, 4 * D, r_w)
    load_w_bf16(C, w2, w2_d, 4 * D, D, r_w)
    NT = 2
    W = NT * 128
    hb = [C.sb([128, D], F32) for _ in range(2 * NT)]
    r_hb = [Res("hb%d" % i) for i in range(2 * NT)]
    nbf = [C.sb([128, D], BF16) for _ in range(2)]
    r_nbf = [Res("nbf%d" % i) for i in range(2)]
    xT = [C.sb([128, 8, W], BF16) for _ in range(2)]
    r_xT = [Res("xT%d" % i) for i in range(2)]
    hidT = C.sb([128, 32, W], BF16)
    r_hid = Res("hidT")
    rl = [C.sb([128, W], F32) for _ in range(2)]
    r_rl = [Res("rl%d" % i) for i in range(2)]
    fo = [C.sb([128, D], F32) for _ in range(2)]
    r_fo = [Res("fo%d" % i) for i in range(2)]
    scr = {"junk": C.sb([128, D], BF16), "ss": C.sb([128, 1], F32), "rstd": C.sb([128, 1], F32), "res": Res("scr")}
    pT = [C.ps([128, D], BF16) for _ in range(2)]
    r_pT = [Res("pT%d" % i, psum=True) for i in range(2)]
    pH = [C.ps([128, 512], F32) for _ in range(3)]
    r_pH = [Res("pH%d" % i, psum=True) for i in range(3)]
    pP = [C.ps([128, 512], F32) for _ in range(3)]
    r_pP = [Res("pP%d" % i, psum=True) for i in range(3)]
    cnt = {"pT": 0, "ev": 0, "pP": 0, "pH": 0, "n": 0, "x": 0, "rl": 0, "hb": 0, "fo": 0}
    for g in range(TT // NT):
        hs = []
        xa = cnt["x"] % 2
        cnt["x"] += 1
        for tt in range(NT):
            t = g * NT + tt
            hi = cnt["hb"] % (2 * NT)
            cnt["hb"] += 1
            hs.append(hi)
            P.dma("sp", hb[hi][:], h_d[t * 128:(t + 1) * 128, :], w=[r_hb[hi]])
            ni = cnt["n"] % 2
            cnt["n"] += 1
            rmsnorm_tile(C, hb[hi][:], g_mlp[:], nbf[ni][:], D, scr, r_hb[hi], r_w, r_nbf[ni], "m")
            transpose_tile(C, nbf[ni], xT[xa], tt * 128, ident, pT, r_pT, r_nbf[ni], r_xT[xa], r_w, cnt)
        for fc in range(32):
            pi = cnt["pH"] % 3
            cnt["pH"] += 1
            for kc in range(8):
                P.mm(pH[pi][:, 0:W], w1[:, kc, fc * 128:(fc + 1) * 128], xT[xa][:, kc, :], kc == 0, kc == 7,
                     r=[r_w, r_xT[xa]], w=[r_pH[pi]])
            ri = cnt["rl"] % 2
            cnt["rl"] += 1
            P.act(rl[ri][:], pH[pi][:, 0:W], AF.Relu, r=[r_pH[pi]], w=[r_rl[ri]])
            P.tt("pool", hidT[:, fc, :], rl[ri][:], rl[ri][:], ALU.mult, r=[r_rl[ri]], pw=[r_hid])
        for tt in range(NT):
            t = g * NT + tt
            hi = hs[tt]
            for half in range(2):
                pi = cnt["pP"] % 3
                cnt["pP"] += 1
                for fc in range(32):
                    P.mm(pP[pi][:, :], hidT[:, fc, tt * 128:(tt + 1) * 128], w2[:, fc, half * 512:(half + 1) * 512],
                         fc == 0, fc == 31, r=[r_hid, r_w], w=[r_pP[pi]])
                P.tt("dve", hb[hi][:, half * 512:(half + 1) * 512], hb[hi][:, half * 512:(half + 1) * 512], pP[pi][:, :],
                     ALU.add, r=[r_pP[pi]], w=[r_hb[hi]])
            if final:
                fi = cnt["fo"] % 2
                cnt["fo"] += 1
                rmsnorm_tile(C, hb[hi][:], g_fin[:], fo[fi][:], D, scr, r_hb[hi], r_w, r_fo[fi], "f")
                P.dma("sp", hout_d[t * 128:(t + 1) * 128, :], fo[fi][:], r=[r_fo[fi]], is_output=True)
            else:
                P.dma("sp", hout_d[t * 128:(t + 1) * 128, :], hb[hi][:], r=[r_hb[hi]], is_output=True)
    P.emit()
    return nc


def launch_C1(S, h_own, outB, mem, W, l):
    nc = _get_nc(("C1", S), lambda: build_C1(S))
    in_maps = []
    for c in range(8):
        b = c // 2
        m = {"h": np.ascontiguousarray(h_own[c]), "o": outB[c]["o"], "mem": np.ascontiguousarray(mem[b]),
             "ident": np.eye(128, dtype=np.float32), "w_out": W["w_out"][l],
             "cross_norm": W["cross_norm"][l].reshape(1, -1), "mem_norm": W["mem_norm"][l].reshape(1, -1),
             "wq": W["cross_wq"][l], "wkv": W["cross_wkv"][l], "wo": W["cross_wo"][l]}
        in_maps.append(m)
    return [r["h_out"] for r in _run(nc, in_maps)]


def launch_C2(S, h_own, W, l, final):
    nc = _get_nc(("C2", S, final), lambda: build_C2(S, final))
    in_maps = []
    for c in range(8):
        m = {"h": np.ascontiguousarray(h_own[c]), "ident": np.eye(128, dtype=np.float32),
             "mlp_norm": W["mlp_norm"][l].reshape(1, -1), "w1": W["mlp_w1"][l], "w2": W["mlp_w2"][l],
             "final_norm": W["final_norm"].reshape(1, -1)}
        in_maps.append(m)
    return [r["h_out"] for r in _run(nc, in_maps)]


def forward(S, inputs, depth=2):
    x = np.asarray(inputs["x"])
    positions = np.asarray(inputs["positions"])
    mem = np.asarray(inputs["mem"])
    W = {k: np.asarray(v) for k, v in inputs.items()}
    h_own = [np.ascontiguousarray(x[c // 2][tok_rows(S, c % 2)]) for c in range(8)]
    for l in range(depth):
        outA = launch_A(S, h_own, positions, W, l)
        outB = launch_B(S, outA, W, l)
        h_own = launch_C1(S, h_own, outB, mem, W, l)
        h_own = launch_C2(S, h_own, W, l, final=(l == depth - 1))
    out = np.empty((4, S, D), np.float32)
    for c in range(8):
        out[c // 2][tok_rows(S, c % 2)] = h_own[c]
    return out


def kernel(**inputs):
    return forward(8192, inputs)
```
